# Optimizing a Trainium2 kernel written in Bass

```python
import jax, jax.numpy as jnp
from jax import lax
import numpy as np

D_MODEL = 2048
BATCH = 8
SEQ = 4096
DEPTH = 1

CHUNK = 64
D_MIX = D_MODEL
D_CONV = D_MIX // 2
CONV_WIDTH = 31
D_MLSTM = D_MIX - D_CONV
MLSTM_HEADS = 4
DV = D_MLSTM // MLSTM_HEADS
DK = DV // 2
N_EXPERTS = 32
TOP_K = 4
D_EXPERT = D_MODEL
SWIGLU_ALPHA = 1.702
SWIGLU_LIMIT = 7.0
MOE_BLOCK = 256
LN_EPS = 1e-5
DEEPNORM_ALPHA = (2 * DEPTH) ** 0.25
DEEPNORM_BETA = (8 * DEPTH) ** -0.25
IN_SIZES = (D_CONV, D_CONV, MLSTM_HEADS * DK, MLSTM_HEADS * DK, D_MLSTM, D_MLSTM, MLSTM_HEADS, MLSTM_HEADS)
D_IN = sum(IN_SIZES)
SPLIT_POINTS = tuple(int(s) for s in np.cumsum(IN_SIZES)[:-1])

kernel_name = 'hymba_conformer_mlstm_moe_deepnorm'


def layer_norm(x, gain=None, bias=None):
    xf = x.astype(jnp.float32)
    mu = jnp.mean(xf, axis=-1, keepdims=True)
    var = jnp.mean(jnp.square(xf - mu), axis=-1, keepdims=True)
    y = (xf - mu) * lax.rsqrt(var + LN_EPS)
    if gain is not None:
        y = y * gain.astype(jnp.float32) + bias.astype(jnp.float32)
    return y.astype(x.dtype)


def conformer_conv(val, gate, dw_w, dw_b, ln_g, ln_b):
    u = val * jax.nn.sigmoid(gate)
    y = lax.conv_general_dilated(
        u, dw_w[:, None, :].astype(u.dtype), window_strides=(1,),
        padding=[(CONV_WIDTH - 1, 0)],
        dimension_numbers=('NWC', 'WIO', 'NWC'), feature_group_count=D_CONV)
    y = y + dw_b
    return jax.nn.silu(layer_norm(y, ln_g, ln_b))


def mlstm_chunkwise(q, k, v, i_pre, f_pre):
    bsz, seq = q.shape[:2]
    n_chunks = seq // CHUNK

    def to_chunks(z):
        z = z.astype(jnp.float32).reshape(bsz, n_chunks, CHUNK, MLSTM_HEADS, -1)
        return z.transpose(1, 0, 3, 2, 4)

    qc = to_chunks(q)
    kc = to_chunks(k) * (DK ** -0.5)
    vc = to_chunks(v)
    ic = to_chunks(i_pre[..., None])[..., 0]
    lfc = jax.nn.log_sigmoid(to_chunks(f_pre[..., None])[..., 0])
    causal = jnp.tril(jnp.ones((CHUNK, CHUNK), dtype=bool))

    def step(carry, inp):
        c_mat, n_vec, m = carry
        q_, k_, v_, ig, lf = inp
        b = jnp.cumsum(lf, axis=-1)
        dmat = jnp.where(causal, b[..., :, None] - b[..., None, :] + ig[..., None, :], -jnp.inf)
        inter = b + m[..., None]
        m_q = jnp.maximum(inter, jnp.max(dmat, axis=-1))
        s = jnp.einsum('bhld,bhsd->bhls', q_, k_) * jnp.exp(dmat - m_q[..., None])
        w_inter = jnp.exp(inter - m_q)
        num = jnp.einsum('bhls,bhsv->bhlv', s, v_) + w_inter[..., None] * jnp.einsum('bhld,bhdv->bhlv', q_, c_mat)
        den = jnp.sum(s, axis=-1) + w_inter * jnp.einsum('bhld,bhd->bhl', q_, n_vec)
        h = num / jnp.maximum(jnp.abs(den), jnp.exp(-m_q))[..., None]
        b_last = b[..., -1]
        gain = b_last[..., None] - b + ig
        m_new = jnp.maximum(b_last + m, jnp.max(gain, axis=-1))
        wk = jnp.exp(gain - m_new[..., None])
        decay = jnp.exp(b_last + m - m_new)
        c_mat = decay[..., None, None] * c_mat + jnp.einsum('bhs,bhsd,bhsv->bhdv', wk, k_, v_)
        n_vec = decay[..., None] * n_vec + jnp.einsum('bhs,bhsd->bhd', wk, k_)
        return (c_mat, n_vec, m_new), h

    init = (jnp.zeros((bsz, MLSTM_HEADS, DK, DV), jnp.float32),
            jnp.zeros((bsz, MLSTM_HEADS, DK), jnp.float32),
            jnp.zeros((bsz, MLSTM_HEADS), jnp.float32))
    _, hc = lax.scan(step, init, (qc, kc, vc, ic, lfc))
    return hc.transpose(1, 0, 3, 2, 4).reshape(bsz, seq, MLSTM_HEADS, DV)


def clamped_swiglu(a, u):
    a = jnp.minimum(a, SWIGLU_LIMIT)
    u = jnp.clip(u, -SWIGLU_LIMIT, SWIGLU_LIMIT)
    return a * jax.nn.sigmoid(SWIGLU_ALPHA * a) * (u + 1.0)


def moe_ffn(h, router_w, router_b, w_gate, b_gate, w_up, b_up, w_down, b_down):
    bsz, seq, d = h.shape
    n_tok = bsz * seq
    n_assign = n_tok * TOP_K
    n_blocks = -(-n_assign // MOE_BLOCK) + N_EXPERTS
    xt = h.reshape(n_tok, d)
    logits = (xt @ router_w + router_b).astype(jnp.float32)
    top_logit, top_idx = lax.top_k(logits, TOP_K)
    top_w = jax.nn.softmax(top_logit, axis=-1)
    e_flat = top_idx.reshape(n_assign)
    t_flat = jnp.arange(n_assign, dtype=jnp.int32) // TOP_K
    w_flat = top_w.reshape(n_assign)
    order = jnp.argsort(e_flat)
    e_sorted = e_flat[order]
    counts = jnp.bincount(e_flat, length=N_EXPERTS)
    starts = jnp.cumsum(counts) - counts
    padded = (counts + MOE_BLOCK - 1) // MOE_BLOCK * MOE_BLOCK
    pad_end = jnp.cumsum(padded)
    pad_start = pad_end - padded
    dest = pad_start[e_sorted] + jnp.arange(n_assign, dtype=jnp.int32) - starts[e_sorted]
    row_tok = jnp.full((n_blocks * MOE_BLOCK,), n_tok, jnp.int32).at[dest].set(t_flat[order])
    row_w = jnp.zeros((n_blocks * MOE_BLOCK,), jnp.float32).at[dest].set(w_flat[order])
    block_expert = jnp.minimum(
        jnp.searchsorted(pad_end, jnp.arange(n_blocks, dtype=jnp.int32) * MOE_BLOCK, side='right'),
        N_EXPERTS - 1)
    x_pad = jnp.concatenate([xt, jnp.zeros((1, d), xt.dtype)], axis=0)

    def expert_block(acc, blk):
        tok, wt, e = blk
        xb = x_pad[tok]
        act = clamped_swiglu(xb @ w_gate[e] + b_gate[e], xb @ w_up[e] + b_up[e])
        yb = act @ w_down[e] + b_down[e]
        return acc.at[tok].add(wt[:, None].astype(yb.dtype) * yb), None

    acc, _ = lax.scan(expert_block, jnp.zeros((n_tok + 1, d), xt.dtype),
                      (row_tok.reshape(n_blocks, MOE_BLOCK), row_w.reshape(n_blocks, MOE_BLOCK), block_expert))
    return acc[:n_tok].reshape(bsz, seq, d)


def hybrid_layer(x, c, w_ada, b_ada, w_in, b_in, dw_w, dw_b, conv_ln_g, conv_ln_b, mh_g, w_out,
                 ln1_g, ln1_b, router_w, router_b, w_gate, b_gate, w_up, b_up, w_down, b_down, ln2_g, ln2_b):
    bsz, seq, _ = x.shape
    mod = jax.nn.silu(c) @ w_ada + b_ada
    sh1, sc1, g1, sh2, sc2, g2 = jnp.split(mod[:, None, :], 6, axis=-1)
    h = layer_norm(x) * (1.0 + sc1) + sh1
    z = h @ w_in + b_in
    a_c, g_c, q, k, v, o, ig, fg = jnp.split(z, SPLIT_POINTS, axis=-1)
    y_conv = conformer_conv(a_c, g_c, dw_w, dw_b, conv_ln_g, conv_ln_b)
    hm = mlstm_chunkwise(q.reshape(bsz, seq, MLSTM_HEADS, DK), k.reshape(bsz, seq, MLSTM_HEADS, DK),
                         v.reshape(bsz, seq, MLSTM_HEADS, DV), ig, fg)
    hm = hm * lax.rsqrt(jnp.mean(jnp.square(hm), axis=-1, keepdims=True) + LN_EPS)
    hm = hm * mh_g.reshape(MLSTM_HEADS, DV).astype(jnp.float32)
    y_mlstm = (hm.reshape(bsz, seq, D_MLSTM) * jax.nn.sigmoid(o.astype(jnp.float32))).astype(x.dtype)
    y_mix = jnp.concatenate([y_conv, y_mlstm], axis=-1) @ w_out
    x = layer_norm(DEEPNORM_ALPHA * x + g1 * y_mix, ln1_g, ln1_b)
    h2 = layer_norm(x) * (1.0 + sc2) + sh2
    y_moe = moe_ffn(h2, router_w, router_b, w_gate, b_gate, w_up, b_up, w_down, b_down)
    return layer_norm(DEEPNORM_ALPHA * x + g2 * y_moe, ln2_g, ln2_b)


def setup_inputs(seed: int = 0) -> dict:
    key = jax.random.key(seed)
    ks = jax.random.split(key, 24)

    def nrm(k, shape, scale):
        return jax.random.normal(k, shape, jnp.float32) * scale

    b_in = nrm(ks[5], (DEPTH, D_IN), 0.01)
    b_in = b_in.at[:, D_IN - MLSTM_HEADS:].add(jnp.linspace(3.0, 6.0, MLSTM_HEADS))
    return {
        'x': nrm(ks[0], (BATCH, SEQ, D_MODEL), 1.0),
        'c': nrm(ks[1], (BATCH, D_MODEL), 1.0),
        'w_ada': nrm(ks[2], (DEPTH, D_MODEL, 6 * D_MODEL), D_MODEL ** -0.5),
        'b_ada': nrm(ks[3], (DEPTH, 6 * D_MODEL), 0.01),
        'w_in': nrm(ks[4], (DEPTH, D_MODEL, D_IN), D_MODEL ** -0.5),
        'b_in': b_in,
        'dw_w': nrm(ks[6], (DEPTH, CONV_WIDTH, D_CONV), CONV_WIDTH ** -0.5),
        'dw_b': nrm(ks[7], (DEPTH, D_CONV), 0.01),
        'conv_ln_g': 1.0 + nrm(ks[8], (DEPTH, D_CONV), 0.01),
        'conv_ln_b': nrm(ks[9], (DEPTH, D_CONV), 0.01),
        'mh_g': 1.0 + nrm(ks[10], (DEPTH, D_MLSTM), 0.01),
        'w_out': nrm(ks[11], (DEPTH, D_MIX, D_MODEL), D_MIX ** -0.5 * DEEPNORM_BETA),
        'ln1_g': 1.0 + nrm(ks[12], (DEPTH, D_MODEL), 0.01),
        'ln1_b': nrm(ks[13], (DEPTH, D_MODEL), 0.01),
        'router_w': nrm(ks[14], (DEPTH, D_MODEL, N_EXPERTS), D_MODEL ** -0.5),
        'router_b': nrm(ks[15], (DEPTH, N_EXPERTS), 0.01),
        'w_gate': nrm(ks[16], (DEPTH, N_EXPERTS, D_MODEL, D_EXPERT), D_MODEL ** -0.5),
        'b_gate': nrm(ks[17], (DEPTH, N_EXPERTS, D_EXPERT), 0.01),
        'w_up': nrm(ks[18], (DEPTH, N_EXPERTS, D_MODEL, D_EXPERT), D_MODEL ** -0.5),
        'b_up': nrm(ks[19], (DEPTH, N_EXPERTS, D_EXPERT), 0.01),
        'w_down': nrm(ks[20], (DEPTH, N_EXPERTS, D_EXPERT, D_MODEL), D_EXPERT ** -0.5 * DEEPNORM_BETA),
        'b_down': nrm(ks[21], (DEPTH, N_EXPERTS, D_MODEL), 0.01 * DEEPNORM_BETA),
        'ln2_g': 1.0 + nrm(ks[22], (DEPTH, D_MODEL), 0.01),
        'ln2_b': nrm(ks[23], (DEPTH, D_MODEL), 0.01),
    }


def reference(x, c, w_ada, b_ada, w_in, b_in, dw_w, dw_b, conv_ln_g, conv_ln_b, mh_g, w_out,
              ln1_g, ln1_b, router_w, router_b, w_gate, b_gate, w_up, b_up, w_down, b_down, ln2_g, ln2_b):
    for l in range(DEPTH):
        x = hybrid_layer(x, c, w_ada[l], b_ada[l], w_in[l], b_in[l], dw_w[l], dw_b[l], conv_ln_g[l], conv_ln_b[l],
                         mh_g[l], w_out[l], ln1_g[l], ln1_b[l], router_w[l], router_b[l], w_gate[l], b_gate[l],
                         w_up[l], b_up[l], w_down[l], b_down[l], ln2_g[l], ln2_b[l])
    return x
```

```python
import numpy as np
from contextlib import ExitStack
import concourse.bass as bass
import concourse.mybir as mybir
from concourse.bass_utils import run_bass_kernel_spmd

F32 = mybir.dt.float32
BF16 = mybir.dt.bfloat16
I32 = mybir.dt.int32
AF = mybir.ActivationFunctionType
ALU = mybir.AluOpType
AX = mybir.AxisListType

NRING = 8
MIXSTOP = 0
EVAC_DVE = 1
B1STOP = 0


class Buf:
    __slots__ = ("name", "w", "rs")

    def __init__(self, name=""):
        self.name = name
        self.w = None
        self.rs = {}


class _Op:
    __slots__ = ("waits", "fn", "signal", "is_dma", "dma_sem")


class _Eng:
    def __init__(self, name):
        self.name = name
        self.ops = []
        self.waited = {}
        self.ndma = 0
        self.ring_tok = [None] * NRING


class Sched:
    def __init__(self, nc):
        self.nc = nc
        self.engs = {n: _Eng(n) for n in ("pe", "act", "dve", "pool", "sp")}
        self.order = []

    def _add_wait(self, eng, op, tok):
        if tok is None:
            return
        if tok[0] == "c":
            if tok[1] == eng.name and eng.name == "pe":
                return
            key = ("c", tok[1])
            v = tok[2]
        else:
            key = ("d", tok[1], tok[2])
            v = tok[3]
        if eng.waited.get(key, -1) >= v:
            return
        eng.waited[key] = v
        op.waits.append(tok)
        if tok[0] == "c":
            self.engs[tok[1]].ops[tok[2]].signal = True

    def _deps(self, eng, op, reads, writes):
        for b in reads:
            self._add_wait(eng, op, b.w)
        for b in writes:
            self._add_wait(eng, op, b.w)
            for t in list(b.rs.values()):
                self._add_wait(eng, op, t)

    def _commit(self, tok, reads, writes):
        key = tok[:2] if tok[0] == "c" else tok[:3]
        for b in reads:
            b.rs[key] = tok
        for b in writes:
            b.w = tok
            b.rs = {}

    def _new(self, engname, fn, is_dma):
        eng = self.engs[engname]
        o = _Op()
        o.waits = []
        o.fn = fn
        o.signal = False
        o.is_dma = is_dma
        o.dma_sem = None
        return eng, o

    def op(self, engname, fn, reads=(), writes=()):
        eng, o = self._new(engname, fn, False)
        self._deps(eng, o, reads, writes)
        idx = len(eng.ops)
        eng.ops.append(o)
        self.order.append((engname, idx))
        tok = ("c", engname, idx)
        self._commit(tok, reads, writes)
        return tok

    def dma(self, engname, fn, reads=(), writes=()):
        eng, o = self._new(engname, fn, True)
        slot = eng.ndma % NRING
        val = 16 * (eng.ndma // NRING + 1)
        eng.ndma += 1
        self._add_wait(eng, o, eng.ring_tok[slot])
        self._deps(eng, o, reads, writes)
        o.dma_sem = slot
        idx = len(eng.ops)
        eng.ops.append(o)
        self.order.append((engname, idx))
        tok = ("d", engname, slot, val)
        eng.ring_tok[slot] = tok
        self._commit(tok, reads, writes)
        return tok

    def all_tokens(self):
        toks = []
        for n, e in self.engs.items():
            if n != "sp":
                for i in range(len(e.ops) - 1, -1, -1):
                    if (not e.ops[i].is_dma) and e.ops[i].fn is not None:
                        toks.append(("c", n, i))
                        break
            toks.extend(t for t in e.ring_tok if t is not None)
        return toks

    def barrier(self):
        toks = self.all_tokens()
        for n in self.engs:
            eng, o = self._new(n, None, False)
            for t in toks:
                self._add_wait(eng, o, t)
            idx = len(eng.ops)
            eng.ops.append(o)
            self.order.append((n, idx))

    def emit(self, stack):
        nc = self.nc
        csem = {n: stack.enter_context(nc.semaphore("c_" + n)) for n in self.engs}
        dsem = {}
        for n in ("sp", "pool"):
            for s in range(NRING):
                dsem[(n, s)] = stack.enter_context(nc.semaphore("d_%s_%d" % (n, s)))
        pref = {}
        for n, e in self.engs.items():
            c = 0
            arr = []
            for o in e.ops:
                if o.signal and not o.is_dma and o.fn is not None:
                    c += 1
                arr.append(c)
            pref[n] = arr
        hs = {"pe": nc.tensor, "act": nc.scalar, "dve": nc.vector, "pool": nc.gpsimd, "sp": nc.sync}
        for (n, i) in self.order:
            o = self.engs[n].ops[i]
            h = hs[n]
            for t in o.waits:
                if t[0] == "c":
                    h.wait_ge(csem[t[1]], pref[t[1]][t[2]])
                else:
                    h.wait_ge(dsem[(t[1], t[2])], t[3])
            if o.fn is None:
                continue
            inst = o.fn(h)
            if o.is_dma:
                inst.then_inc(dsem[(n, o.dma_sem)], 16)
            elif o.signal:
                inst.then_inc(csem[n], 1)


S_TOK = 4096
D = 2048
NT = 32
ST = 256
NST = S_TOK // ST
TPS = ST // 128
CPS = ST // 64
DIN = 5128
NE = 32
BLK = 512
NBLK = 63
NSLOT = NBLK * BLK
ALPHA = 2.0 ** 0.25
EPS = 1e-5
QS = 128.0 ** -0.5

C_ID = 0
C_MASK = 128
C_IOTA = 192
C_SELI = 193
C_SELF = 197
C_NEGI = 201
C_SELB = 205
C_ONE = 717
C_IOE = 1229
C_IO16 = 1292
CF_W = 1296
B_ID = 0
B_ONE = 128
B_TRI = 640
CB_W = 768


def make_consts():
    import ml_dtypes
    cf = np.zeros((128, CF_W), np.float32)
    cf[:, C_ID:C_ID + 128] = np.eye(128, dtype=np.float32)
    p = np.arange(128)
    cf[:, C_MASK:C_MASK + 64] = ((p % 64)[:, None] <= np.arange(64)[None, :]).astype(np.float32)
    cf[:, C_IOTA] = p
    for h in range(4):
        cf[h, C_SELI + h] = 1.0
        cf[h + 4, C_SELF + h] = 1.0
        cf[h, C_NEGI + h] = -1.0
        cf[h, C_SELB + h * 128:C_SELB + (h + 1) * 128] = 1.0
    cf[:, C_ONE:C_ONE + 512] = 1.0
    cf[:, C_IOE:C_IOE + 63] = np.arange(63, dtype=np.float32)[None, :]
    cf[:, C_IO16:C_IO16 + 4] = (np.arange(4, dtype=np.float32) * 128.0)[None, :]
    cb = np.zeros((128, CB_W), np.float32)
    cb[:, B_ID:B_ID + 128] = np.eye(128)
    cb[:, B_ONE:B_ONE + 512] = 1.0
    cb[:, B_TRI:B_TRI + 128] = (p[:, None] < p[None, :]).astype(np.float32)
    return cf, cb.astype(ml_dtypes.bfloat16)


def build(dbg=False, phases=("A", "B", "C", "D", "E")):
    nc = bass.Bass("TRN2", target_bir_lowering=False)

    def din(name, shape, dt=F32):
        return nc.dram_tensor(name, list(shape), dt, kind="ExternalInput").ap()

    x = din("x", [S_TOK, D])
    c_col = din("c_col", [128, 16])
    w_ada = din("w_ada", [D, 6 * D])
    b_ada = din("b_ada", [1, 6 * D])
    w_in = din("w_in", [D, DIN])
    b_in_col = din("b_in_col", [128, 41])
    b_kv_bc = din("b_kv_bc", [128, 1536])
    dw_col = din("dw_col", [128, 8 * 31])
    cv_col = din("cv_col", [128, 32])
    w_out = din("w_out", [D, D])
    ln_bc = din("ln_bc", [128, 4 * D])
    router_w = din("router_w", [D, NE])
    rb_bc = din("rb_bc", [128, NE])
    if "C" in phases:
        wg_l = din("wg_l", [16384, 8192])
        wu_l = din("wu_l", [16384, 8192])
        wd_l = din("wd_l", [16384, 8192])
    else:
        wg_l = wu_l = wd_l = None
    bgu = din("bgu", [NE, 2 * D])
    b_down = din("b_down", [NE, D])
    cf = din("cf", [128, CF_W])
    cb = din("cb", [128, CB_W], BF16)
    out = nc.dram_tensor("out", [S_TOK, D], F32, kind="ExternalOutput").ap()
    ikind = "ExternalOutput" if dbg else "Internal"
    x1_d = nc.dram_tensor("x1_d", [S_TOK, D], F32, kind=ikind).ap()
    h2_d = nc.dram_tensor("h2_d", [S_TOK, D], BF16, kind=ikind).ap()
    Xs = nc.dram_tensor("Xs", [NSLOT, D], BF16, kind="Internal").ap()
    Ys = nc.dram_tensor("Ys", [NSLOT, D], F32, kind="Internal").ap()
    if dbg:
        d_ymix = nc.dram_tensor("d_ymix", [128, 16 * ST], BF16, kind="ExternalOutput").ap()
        d_hT = nc.dram_tensor("d_hT", [128, 16 * ST], BF16, kind="ExternalOutput").ap()
        d_rt = nc.dram_tensor("d_rt", [128, NT * 40], F32, kind="ExternalOutput").ap()

    gs = ExitStack()
    S = Sched(nc)

    def sb(name, shape, dt, st=None):
        return (st or gs).enter_context(nc.sbuf_tensor(name, list(shape), dt))

    def MM(o, lhsT, rhs, start, stop, reads, writes, **kw):
        S.op("pe", lambda h: h.matmul(o, lhsT, rhs, start=start, stop=stop, **kw), reads, writes)

    def TR(o, in_, ident, reads, writes):
        S.op("pe", lambda h: h.transpose(o, in_, ident), reads, writes)

    def ACT(o, in_, func, reads, writes, bias=None, scale=None, eng="act"):
        kw = {}
        if bias is not None:
            kw["bias"] = bias
        if scale is not None:
            kw["scale"] = scale
        S.op("act", lambda h: h.activation(out=o, in_=in_, func=func, **kw), reads, writes)

    def TT(eng, o, a, b, op, reads, writes):
        S.op(eng, lambda h: h.tensor_tensor(out=o, in0=a, in1=b, op=op), reads, writes)

    def TS(eng, o, a, s1, op0, reads, writes, s2=None, op1=None):
        if op1 is None:
            S.op(eng, lambda h: h.tensor_scalar(out=o, in0=a, scalar1=s1, scalar2=None, op0=op0), reads, writes)
        else:
            S.op(eng, lambda h: h.tensor_scalar(out=o, in0=a, scalar1=s1, scalar2=s2, op0=op0, op1=op1), reads, writes)

    def STT(eng, o, a, sc, b, op0, op1, reads, writes):
        S.op(eng, lambda h: h.scalar_tensor_tensor(out=o, in0=a, scalar=sc, in1=b, op0=op0, op1=op1), reads, writes)

    def CP(eng, o, a, reads, writes):
        if eng == "act":
            S.op("act", lambda h: h.activation(out=o, in_=a, func=AF.Copy), reads, writes)
        else:
            S.op(eng, lambda h: h.tensor_copy(o, a), reads, writes)

    def DMA(q, o, a, reads, writes):
        return S.dma(q, lambda h: h.dma_start(out=o, in_=a), reads, writes)

    def RED(o, a, op, reads, writes):
        S.op("dve", lambda h: h.tensor_reduce(out=o, in_=a, axis=AX.X, op=op), reads, writes)

    PB = [gs.enter_context(nc.psum_tensor("pb%d" % i, [128, 512], F32)) for i in range(6)]
    PBb = [Buf("pb%d" % i) for i in range(6)]
    PTS = [gs.enter_context(nc.psum_tensor("pt%d" % i, [128, 1024], BF16)) for i in range(2)]
    PT = PTS
    PTb = [Buf("pt0"), Buf("pt1")]
    bigc = [0]

    def nextbank():
        i = bigc[0] % 3
        bigc[0] += 1
        return PB[i], PBb[i]

    cf_t = sb("cf_t", [128, CF_W], F32)
    cb_t = sb("cb_t", [128, CB_W], BF16)
    bcol = sb("bcol", [128, 41], F32)
    bqs = sb("bqs", [128, 4], F32)
    dwc = sb("dwc", [128, 248], F32)
    cvc = sb("cvc", [128, 32], F32)
    mod_col = sb("mod_col", [128, 96], F32)
    sc1p = sb("sc1p", [128, 16], F32)
    g2_bc = sb("g2_bc", [128, D], F32)
    b_c = Buf("consts")
    b_g2 = Buf("g2")
    DMA("sp", cf_t[:], cf[:], [], [b_c])
    DMA("sp", cb_t[:], cb[:], [], [b_c])
    DMA("sp", bcol[:], b_in_col[:], [], [b_c])
    DMA("sp", dwc[:], dw_col[:], [], [b_c])
    DMA("sp", cvc[:], cv_col[:], [], [b_c])
    TS("dve", bqs[:], bcol[:, 16:20], QS, ALU.mult, [b_c], [b_c])
    S.barrier()
    ident_b = cb_t[:, B_ID:B_ID + 128]

    sab = ExitStack()
    g1_bc = sb("g1_bc", [128, D], F32, sab)
    sc2p_bc = sb("sc2p_bc", [128, D], F32, sab)
    sh2_bc = sb("sh2_bc", [128, D], F32, sab)
    b_rows = Buf("rows")

    with ExitStack() as sa:
        mod_row = sb("mod_row", [1, 6 * D], F32, sa)
        b_mr = Buf("mr")
        ccol = sb("ccol", [128, 16], F32, sa)
        scb = sb("scb", [128, 16], BF16, sa)
        b_cc = Buf()
        b_scb = Buf()
        DMA("sp", ccol[:], c_col[:], [], [b_cc])
        ACT(scb[:], ccol[:], AF.Silu, [b_cc], [b_scb])
        wa = [sb("wa%d" % i, [128, 16, 512], BF16, sa) for i in range(3)]
        b_wa = [Buf() for _ in range(3)]
        bar = [sb("bar%d" % i, [1, 512], F32, sa) for i in range(2)]
        b_bar = [Buf() for _ in range(2)]
        w_ada_v = w_ada.rearrange("(k p) n -> p k n", p=128)
        for pc in range(24):
            i = pc % 3
            DMA("pool", wa[i][:], w_ada_v[:, :, pc * 512:(pc + 1) * 512], [], [b_wa[i]])
            DMA("sp", bar[pc % 2][:], b_ada[0:1, pc * 512:(pc + 1) * 512], [], [b_bar[pc % 2]])
            bank, bb = PB[pc % 2], PBb[pc % 2]
            for k in range(16):
                MM(bank[0:1, :], scb[:, k:k + 1], wa[i][:, k, :], k == 0, k == 15, [b_scb, b_wa[i]], [bb])
            TT("dve", mod_row[0:1, pc * 512:(pc + 1) * 512], bank[0:1, :], bar[pc % 2][0:1, :], ALU.add,
               [bb, b_bar[pc % 2]], [b_mr])
        for j in range(96):
            MM(PB[2][:, j:j + 1], mod_row[0:1, j * 128:(j + 1) * 128], cf_t[0:1, C_ONE:C_ONE + 1], True, True,
               [b_mr, b_c], [PBb[2]])
        CP("dve", mod_col[:], PB[2][:, 0:96], [PBb[2]], [b_c])
        TS("dve", sc1p[:], mod_col[:, 16:32], 1.0, ALU.add, [b_c], [b_c])
        for (dst, off, plus1, bdst) in ((g1_bc, 2 * D, False, b_rows), (sh2_bc, 3 * D, False, b_rows),
                                        (sc2p_bc, 4 * D, True, b_rows), (g2_bc, 5 * D, False, b_g2)):
            for i in range(4):
                bank, bb = PB[i % 2], PBb[i % 2]
                MM(bank[:, :], cf_t[0:1, C_ONE:C_ONE + 128], mod_row[0:1, off + i * 512: off + (i + 1) * 512],
                   True, True, [b_mr, b_c], [bb])
                if plus1:
                    ACT(dst[:, i * 512:(i + 1) * 512], bank[:, :], AF.Identity, [bb], [bdst], bias=1.0)
                else:
                    CP("dve", dst[:, i * 512:(i + 1) * 512], bank[:, :], [bb], [bdst])
        S.barrier()
    sh1 = mod_col[:, 0:16]

    if "B" in phases:
        build_mixer(nc, S, locals())
    S.barrier()
    sab.close()

    if "C" in phases:
        build_moe(nc, S, locals())

    S.barrier()
    S.emit(gs)
    gs.close()
    return nc


def build_mixer(nc, S, L):
    (MM, TR, ACT, TT, TS, STT, CP, DMA, RED, sb, PB, PBb, PT, PTb, nextbank) = (
        L[k] for k in ("MM", "TR", "ACT", "TT", "TS", "STT", "CP", "DMA", "RED", "sb", "PB", "PBb", "PT", "PTb",
                       "nextbank"))
    cf_t, cb_t, bcol, bqs, dwc, cvc, mod_col, sc1p, sh1, b_c = (
        L[k] for k in ("cf_t", "cb_t", "bcol", "bqs", "dwc", "cvc", "mod_col", "sc1p", "sh1", "b_c"))
    g1_bc, sc2p_bc, sh2_bc, b_rows = (L[k] for k in ("g1_bc", "sc2p_bc", "sh2_bc", "b_rows"))
    x, w_in, w_out, b_kv_bc, ln_bc, x1_d, h2_d = (L[k] for k in ("x", "w_in", "w_out", "b_kv_bc", "ln_bc", "x1_d", "h2_d"))
    dbg = L["dbg"]
    ident_b = cb_t[:, B_ID:B_ID + 128]
    sm = ExitStack()

    def T(name, shape, dt):
        return sb(name, shape, dt, sm)

    xall = T("xall", [128, TPS, D], F32)
    b_x = [Buf() for _ in range(TPS)]
    xn = T("xn", [128, D], BF16)
    b_xn = Buf()
    st6 = T("st6", [128, 24], F32)
    mv = T("mv", [128, 2], F32)
    rstd = T("rstd", [128, 1], F32)
    nmr = T("nmr", [128, 1], F32)
    b_stat = Buf()
    hT = T("hT", [128, 16, ST], BF16)
    b_hT = Buf()
    wgb = [T("wgb%d" % i, [128, 16, 512], BF16) for i in range(2)]
    b_wg = [Buf() for _ in range(2)]
    wgt = T("wgt", [128, 16, 8], BF16)
    b_wgt = Buf()
    sg = T("sg", [128, 8, ST], BF16)
    b_sg = Buf()
    uT = T("uT", [128, 8, 30 + ST], BF16)
    b_uT = Buf()
    qT = T("qT", [128, 4, ST], BF16)
    kT = T("kT", [128, 4, ST], BF16)
    b_qk = Buf()
    sgo = T("sgo", [128, 8, ST], BF16)
    b_sgo = Buf()
    ktok = T("ktok", [64, CPS, 512], BF16)
    vtok = T("vtok", [64, CPS, 1024], BF16)
    b_kv = Buf()
    bkv = T("bkv", [128, 1536], F32)
    ymixT = T("ymixT", [128, 16, ST], BF16)
    b_ym = Buf()
    ycv = T("ycv", [128, 8, ST], F32)
    b_ycv = Buf()
    diag = T("diag", [128, 31, 128], BF16)
    b_diag = Buf()
    ybq = T("ybq", [128, 2 * ST], BF16)
    b_yb = Buf()
    mean = T("mean", [128, ST], F32)
    rstc = T("rstc", [128, ST], F32)
    msq = T("msq", [128, ST], F32)
    b_cst = Buf()
    ctmp = T("ctmp", [128, ST], F32)
    b_ctmp = Buf()
    gsb = T("gsb", [8, ST], F32)
    expg = T("expg", [8, ST], F32)
    lfn = T("lfn", [8, ST], F32)
    bneg = T("bneg", [8, ST], F32)
    A_sb = T("A_sb", [4, ST], F32)
    G_sb = T("G_sb", [4, ST], F32)
    E_sb = T("E_sb", [4, ST], F32)
    b_gt = Buf()
    bcar = T("bcar", [8, 1], F32)
    gcar = T("gcar", [4, 1], F32)
    G_bc = T("G_bc", [128, 4, ST], F32)
    gprev = T("gprev", [128, 4], F32)
    b_gbc = Buf()
    colsc = T("colsc", [64, CPS, 8], F32)
    b_cols = Buf()
    Cst = T("Cst", [128, 4, 256], F32)
    nst = T("nst", [128, 4], F32)
    Cbf = T("Cbf", [128, 4, 256], BF16)
    nbf = T("nbf", [128, 4], BF16)
    b_C = Buf()
    b_Cbf = Buf()
    DTt = [T("DT%d" % i, [64, 64], F32) for i in range(2)]
    Dm = [T("Dm%d" % i, [64, 64], F32) for i in range(2)]
    b_DT = [Buf() for _ in range(2)]
    b_Dm = [Buf() for _ in range(2)]
    sT = T("sT", [64, 4, 64], BF16)
    b_sT = [Buf() for _ in range(4)]
    wi = [T("wi%d" % i, [128, 64], F32) for i in range(2)]
    b_wi = [Buf() for _ in range(2)]
    qs = T("qs", [128, 4, 64], BF16)
    b_qs = [Buf() for _ in range(4)]
    kw = T("kw", [64, 4, 128], BF16)
    b_kw = [Buf() for _ in range(4)]
    wkc = T("wkc", [64, 4], F32)
    dec = T("dec", [128, 4], F32)
    b_sm = Buf()
    dabs = T("dabs", [64, 4], F32)
    dd = T("dd", [64, 4], F32)
    rr = T("rr", [64, 4], F32)
    ssq = T("ssq", [64, 4], F32)
    tcol = T("tcol", [64, 4], F32)
    fcol = T("fcol", [64, 4], F32)
    b_nrm = Buf()
    sqt = T("sqt", [64, 256], F32)
    b_sqt = Buf()
    hn = T("hn", [64, CPS, 4, 256], BF16)
    b_hn = Buf()
    tmpw = T("tmpw", [128, 512], F32)
    b_tmpw = Buf()
    x1t = ycv[:].rearrange("p c n -> p (c n)")
    b_x1t = b_ycv
    h2b = xn[:]
    b_h2b = b_xn
    ln1g = hT[:].bitcast(F32).rearrange("p k n -> p (k n)")
    ln1b = ymixT[:].bitcast(F32).rearrange("p k n -> p (k n)")
    b_dx = Buf()
    b_dh = Buf()

    DMA("sp", bkv[:], b_kv_bc[:], [], [b_c])
    S.op("pool", lambda h: h.memset(uT[:, :, 0:30], 0.0), [], [b_uT])
    S.op("pool", lambda h: h.memset(Cst[:], 0.0), [], [b_C])
    S.op("pool", lambda h: h.memset(nst[:], 0.0), [], [b_C])
    S.op("pool", lambda h: h.memset(Cbf[:], 0.0), [], [b_Cbf])
    S.op("pool", lambda h: h.memset(nbf[:], 0.0), [], [b_Cbf])
    S.op("pool", lambda h: h.memset(bcar[:], 0.0), [], [b_gt])
    S.op("pool", lambda h: h.memset(gcar[:], 0.0), [], [b_gt])
    S.op("pool", lambda h: h.memset(gprev[:], 0.0), [], [b_gbc])
    S.barrier()

    w_in_v = w_in.rearrange("(k p) n -> p k n", p=128)
    w_out_v = w_out.rearrange("(k p) n -> p k n", p=128)
    wcnt = [0]

    def load_w(src):
        i = wcnt[0] % 2
        wcnt[0] += 1
        DMA("pool", wgb[i][:], src, [], [b_wg[i]])
        return wgb[i], b_wg[i]

    def ln_stats(src, rd):
        for c4 in range(4):
            S.op("dve", (lambda c4: lambda h: h.bn_stats(st6[:, c4 * 6:(c4 + 1) * 6], src[:, c4 * 512:(c4 + 1) * 512]))(c4),
                 rd, [b_stat])
        S.op("dve", lambda h: h.bn_aggr(mv[:], st6[:]), [b_stat], [b_stat])
        ACT(rstd[:], mv[:, 1:2], AF.Sqrt, [b_stat], [b_stat], bias=EPS)
        S.op("dve", lambda h: h.reciprocal(rstd[:], rstd[:]), [b_stat], [b_stat])
        STT("dve", nmr[:], mv[:, 0:1], -1.0, rstd[:], ALU.mult, ALU.mult, [b_stat], [b_stat])

    for st in range(NST):
        t0 = st * ST
        for i in range(TPS):
            DMA("sp", xall[:, i, :], x[t0 + i * 128: t0 + (i + 1) * 128, :], [], [b_x[i]])
            ln_stats(xall[:, i, :], [b_x[i]])
            if B1STOP == 1:
                continue
            ACT(xn[:], xall[:, i, :], AF.Identity, [b_x[i], b_stat], [b_xn], bias=nmr[:], scale=rstd[:])
            if B1STOP == 2:
                continue
            for kg in range(4):
                hf = kg % 2
                for kk in range(4):
                    k = kg * 4 + kk
                    TR(PT[hf][:, kk * 128:(kk + 1) * 128], xn[:, k * 128:(k + 1) * 128], ident_b,
                       [b_xn, b_c], [PTb[hf]])
                for kk in range(4):
                    if B1STOP == 3:
                        continue
                    k = kg * 4 + kk
                    if EVAC_DVE:
                        TS("dve", hT[:, k, i * 128:(i + 1) * 128], PT[hf][:, kk * 128:(kk + 1) * 128],
                           sc1p[:, k:k + 1], ALU.mult, [PTb[hf], b_c], [b_hT], s2=sh1[:, k:k + 1], op1=ALU.add)
                    else:
                        ACT(hT[:, k, i * 128:(i + 1) * 128], PT[hf][:, kk * 128:(kk + 1) * 128],
                            AF.Identity, [PTb[hf], b_c], [b_hT], bias=sh1[:, k:k + 1], scale=sc1p[:, k:k + 1])
        if dbg and st == 0:
            DMA("sp", L["d_hT"][:], hT[:].rearrange("p k n -> p (k n)"), [b_hT], [Buf()])

        if MIXSTOP == 1:
            break
        def fm_chunk(wt, bw, c, evac):
            bank, bb = nextbank()
            for k in range(16):
                MM(bank[:, 0:ST], wt[:, k, c * 128:(c + 1) * 128], hT[:, k, :], k == 0, k == 15, [bw, b_hT], [bb])
            evac(bank[:, 0:ST], bb)

        for g in range(2):
            wt, bw = load_w(w_in_v[:, :, 1024 + g * 512: 1024 + (g + 1) * 512])
            for c in range(4):
                cc = g * 4 + c
                fm_chunk(wt, bw, c, (lambda cc: lambda ps, bb: ACT(sg[:, cc, :], ps, AF.Sigmoid, [bb, b_c], [b_sg],
                                                                 bias=bcol[:, 8 + cc: 9 + cc]))(cc))
        for g in range(2):
            wt, bw = load_w(w_in_v[:, :, g * 512:(g + 1) * 512])
            for c in range(4):
                cc = g * 4 + c
                fm_chunk(wt, bw, c, (lambda cc: lambda ps, bb: STT("dve", uT[:, cc, 30:30 + ST], ps, bcol[:, cc:cc + 1],
                                                                 sg[:, cc, :], ALU.add, ALU.mult, [bb, b_sg, b_c],
                                                                 [b_uT]))(cc))
        wt, bw = load_w(w_in_v[:, :, 2048:2560])
        for c in range(4):
            fm_chunk(wt, bw, c, (lambda c: lambda ps, bb: ACT(qT[:, c, :], ps, AF.Identity, [bb, b_c], [b_qk],
                                                            bias=bqs[:, c:c + 1], scale=QS))(c))
        wt, bw = load_w(w_in_v[:, :, 2560:3072])
        for c in range(4):
            fm_chunk(wt, bw, c, (lambda c: lambda ps, bb: ACT(kT[:, c, :], ps, AF.Identity, [bb, b_c], [b_qk],
                                                            bias=bcol[:, 20 + c: 21 + c]))(c))
        for ch in range(CPS):
            bank, bb = nextbank()
            for k in range(16):
                MM(bank[0:64, :], hT[:, k, ch * 64:(ch + 1) * 64], wt[:, k, :], k == 0, k == 15, [bw, b_hT], [bb])
            TT("dve", ktok[:, ch, :], bank[0:64, :], bkv[0:64, 0:512], ALU.add, [bb, b_c], [b_kv])
        for g in range(2):
            wt, bw = load_w(w_in_v[:, :, 3072 + g * 512: 3072 + (g + 1) * 512])
            for ch in range(CPS):
                bank, bb = nextbank()
                for k in range(16):
                    MM(bank[0:64, :], hT[:, k, ch * 64:(ch + 1) * 64], wt[:, k, :], k == 0, k == 15, [bw, b_hT], [bb])
                TT("dve", vtok[:, ch, g * 512:(g + 1) * 512], bank[0:64, :], bkv[0:64, 512 + g * 512: 1024 + g * 512],
                   ALU.add, [bb, b_c], [b_kv])
        for g in range(2):
            wt, bw = load_w(w_in_v[:, :, 4096 + g * 512: 4096 + (g + 1) * 512])
            for c in range(4):
                cc = g * 4 + c
                fm_chunk(wt, bw, c, (lambda cc: lambda ps, bb: ACT(sgo[:, cc, :], ps, AF.Sigmoid, [bb, b_c], [b_sgo],
                                                                 bias=bcol[:, 32 + cc: 33 + cc]))(cc))
        DMA("pool", wgt[:], w_in_v[:, :, 5120:5128], [], [b_wgt])
        for k in range(16):
            MM(PB[3][0:8, 0:ST], wgt[:, k, :], hT[:, k, :], k == 0, k == 15, [b_wgt, b_hT], [PBb[3]])
        ACT(gsb[:], PB[3][0:8, 0:ST], AF.Identity, [PBb[3], b_c], [b_gt], bias=bcol[0:8, 40:41])

        if MIXSTOP == 2:
            break
        ACT(expg[:], gsb[:], AF.Exp, [b_gt], [b_gt], scale=-1.0)
        ACT(lfn[:], expg[:], AF.Ln, [b_gt], [b_gt], bias=1.0)
        S.op("dve", lambda h: h.tensor_tensor_scan(out=bneg[:], data0=cf_t[0:8, C_ONE:C_ONE + ST], data1=lfn[:],
                                                  initial=bcar[:, 0:1], op0=ALU.mult, op1=ALU.add), [b_gt, b_c], [b_gt])
        CP("dve", bcar[:], bneg[:, ST - 1:ST], [b_gt], [b_gt])
        MM(PB[4][0:4, 0:ST], cf_t[0:8, C_SELI:C_SELI + 4], gsb[:], True, False, [b_gt, b_c], [PBb[4]])
        MM(PB[4][0:4, 0:ST], cf_t[0:8, C_SELF:C_SELF + 4], bneg[:], False, True, [b_gt, b_c], [PBb[4]])
        CP("dve", A_sb[:], PB[4][0:4, 0:ST], [PBb[4]], [b_gt])
        S.op("dve", lambda h: h.tensor_tensor_scan(out=G_sb[:], data0=cf_t[0:4, C_ONE:C_ONE + ST], data1=A_sb[:],
                                                  initial=gcar[:, 0:1], op0=ALU.mult, op1=ALU.max), [b_gt, b_c], [b_gt])
        CP("dve", gcar[:], G_sb[:, ST - 1:ST], [b_gt], [b_gt])
        MM(PB[4][0:4, 0:ST], cf_t[0:8, C_SELF:C_SELF + 4], bneg[:], True, False, [b_gt, b_c], [PBb[4]])
        MM(PB[4][0:4, 0:ST], cf_t[0:4, C_NEGI:C_NEGI + 4], G_sb[:], False, True, [b_gt, b_c], [PBb[4]])
        ACT(E_sb[:], PB[4][0:4, 0:ST], AF.Exp, [PBb[4]], [b_gt])
        if st > 0:
            CP("dve", gprev[:], G_bc[:, :, ST - 1], [b_gbc], [b_gbc])
        for h_ in range(4):
            MM(PB[3][:, 0:ST], cf_t[0:4, C_SELB + h_ * 128: C_SELB + (h_ + 1) * 128], G_sb[:], True, True,
               [b_gt, b_c], [PBb[3]])
            CP("dve", G_bc[:, h_, :], PB[3][:, 0:ST], [PBb[3]], [b_gbc])
        for ch in range(CPS):
            S.op("pe", (lambda ch: lambda h: h.transpose(PB[4][0:64, 0:4], A_sb[0:4, ch * 64:(ch + 1) * 64],
                                                         cf_t[0:4, C_ID:C_ID + 4]))(ch), [b_gt, b_c], [PBb[4]])
            S.op("pe", (lambda ch: lambda h: h.transpose(PB[4][0:64, 4:8], E_sb[0:4, ch * 64:(ch + 1) * 64],
                                                         cf_t[0:4, C_ID:C_ID + 4]))(ch), [b_gt, b_c], [PBb[4]])
            CP("dve", colsc[:, ch, :], PB[4][0:64, 0:8], [PBb[4]], [b_cols])

        if MIXSTOP == 3:
            break
        MM_sum, bsum = PB[5], PBb[5]
        MM_sq, bsq = PB[5], PBb[5]
        for c in range(8):
            for j in range(31):
                TS("pool", diag[:, j, :], ident_b, dwc[:, c * 31 + j: c * 31 + j + 1], ALU.mult, [b_c], [b_diag])
            bank, bb = nextbank()
            for j in range(31):
                MM(bank[:, 0:ST], diag[:, j, :], uT[:, c, j:j + ST], j == 0, j == 30, [b_diag, b_uT], [bb])
            ACT(ycv[:, c, :], bank[:, 0:ST], AF.Identity, [bb, b_c], [b_ycv], bias=cvc[:, c:c + 1])
            ACT(ybq[:, 0:ST], bank[:, 0:ST], AF.Identity, [bb, b_c], [b_yb], bias=cvc[:, c:c + 1])
            ACT(ybq[:, ST:2 * ST], bank[:, 0:ST], AF.Square, [bb, b_c], [b_yb], bias=cvc[:, c:c + 1])
            MM(MM_sum[:, 0:2 * ST], cb_t[:, B_ONE:B_ONE + 128], ybq[:], c == 0, c == 7, [b_yb, b_c], [bsum])
        CP("pool", uT[:, :, 0:30], uT[:, :, ST:ST + 30], [b_uT], [b_uT])
        TS("dve", mean[:], MM_sum[:, 0:ST], 1.0 / 1024, ALU.mult, [bsum], [b_cst])
        TT("dve", msq[:], mean[:], mean[:], ALU.mult, [b_cst], [b_cst])
        STT("dve", rstc[:], MM_sq[:, ST:2 * ST], 1.0 / 1024, msq[:], ALU.mult, ALU.subtract, [bsq, b_cst], [b_cst])
        ACT(rstc[:], rstc[:], AF.Sqrt, [b_cst], [b_cst], bias=EPS)
        S.op("dve", lambda h: h.reciprocal(rstc[:], rstc[:]), [b_cst], [b_cst])
        for c in range(8):
            TT("dve", ctmp[:], ycv[:, c, :], mean[:], ALU.subtract, [b_ycv, b_cst], [b_ctmp])
            TT("dve", ctmp[:], ctmp[:], rstc[:], ALU.mult, [b_ctmp, b_cst], [b_ctmp])
            ACT(ymixT[:, c, :], ctmp[:], AF.Silu, [b_ctmp, b_c], [b_ym], bias=cvc[:, 16 + c: 17 + c],
                scale=cvc[:, 8 + c: 9 + c])

        if MIXSTOP == 4:
            break
        for ch in range(CPS):
            cs = slice(ch * 64, (ch + 1) * 64)
            last = ch * 64 + 63
            for h_ in range(4):
                gp = G_bc[:, h_, ch * 64 - 1: ch * 64] if ch > 0 else gprev[:, h_:h_ + 1]
                i2 = h_ % 2
                MM(PB[4][0:64, h_ * 64:(h_ + 1) * 64], kT[:, h_, cs], qT[:, h_, cs], True, True, [b_qk], [PBb[4]])
                ACT(DTt[i2][:], G_bc[0:64, h_, cs], AF.Exp, [b_gbc, b_cols], [b_DT[i2]], bias=colsc[:, ch, h_:h_ + 1],
                    scale=-1.0)
                TT("pool", Dm[i2][:], DTt[i2][:], cf_t[0:64, C_MASK:C_MASK + 64], ALU.mult, [b_DT[i2], b_c], [b_Dm[i2]])
                TT("dve", sT[:, h_, :], PB[4][0:64, h_ * 64:(h_ + 1) * 64], Dm[i2][:], ALU.mult, [PBb[4], b_Dm[i2]],
                   [b_sT[h_]])
                ACT(wi[i2][:], G_bc[:, h_, cs], AF.Exp, [b_gbc], [b_wi[i2]], bias=gp, scale=-1.0)
                TT("pool", qs[:, h_, :], qT[:, h_, cs], wi[i2][:], ALU.mult, [b_qk, b_wi[i2]], [b_qs[h_]])
                ACT(wkc[:, h_:h_ + 1], G_bc[0:64, h_, last:last + 1], AF.Exp, [b_gbc, b_cols], [b_sm],
                    bias=colsc[:, ch, h_:h_ + 1], scale=-1.0)
                ACT(dec[:, h_:h_ + 1], G_bc[:, h_, last:last + 1], AF.Exp, [b_gbc], [b_sm], bias=gp, scale=-1.0)
                TS("pool", kw[:, h_, :], ktok[:, ch, h_ * 128:(h_ + 1) * 128], wkc[:, h_:h_ + 1], ALU.mult,
                   [b_kv, b_sm], [b_kw[h_]])
            nbanks = []
            for pr in range(2):
                bank, bb = nextbank()
                nbanks.append((bank, bb))
                for hh in range(2):
                    h_ = pr * 2 + hh
                    MM(bank[0:64, hh * 256:(hh + 1) * 256], sT[:, h_, :], vtok[:, ch, h_ * 256:(h_ + 1) * 256], True, False,
                       [b_sT[h_], b_kv], [bb])
                    MM(bank[0:64, hh * 256:(hh + 1) * 256], qs[:, h_, :], Cbf[:, h_, :], False, True, [b_qs[h_], b_Cbf], [bb])
            for h_ in range(4):
                MM(PB[3][0:64, h_:h_ + 1], sT[:, h_, :], cb_t[0:64, B_ONE:B_ONE + 1], True, False, [b_sT[h_], b_c], [PBb[3]])
                MM(PB[3][0:64, h_:h_ + 1], qs[:, h_, :], nbf[:, h_:h_ + 1], False, True, [b_qs[h_], b_Cbf], [PBb[3]])
            CP("dve", dd[:], PB[3][0:64, 0:4], [PBb[3]], [b_nrm])
            STT("dve", dabs[:], dd[:], -1.0, dd[:], ALU.mult, ALU.max, [b_nrm], [b_nrm])
            TT("dve", dd[:], dabs[:], colsc[:, ch, 4:8], ALU.max, [b_nrm, b_cols], [b_nrm])
            S.op("dve", lambda h: h.reciprocal(rr[:], dd[:]), [b_nrm], [b_nrm])
            for h_ in range(4):
                bank, bb = nbanks[h_ // 2]
                hh = h_ % 2
                ACT(sqt[:], bank[0:64, hh * 256:(hh + 1) * 256], AF.Square, [bb], [b_sqt])
                RED(ssq[:, h_:h_ + 1], sqt[:], ALU.add, [b_sqt], [b_nrm])
            TT("dve", tcol[:], rr[:], rr[:], ALU.mult, [b_nrm], [b_nrm])
            TT("dve", tcol[:], tcol[:], ssq[:], ALU.mult, [b_nrm], [b_nrm])
            TS("dve", tcol[:], tcol[:], 1.0 / 256, ALU.mult, [b_nrm], [b_nrm], s2=EPS, op1=ALU.add)
            ACT(tcol[:], tcol[:], AF.Sqrt, [b_nrm], [b_nrm])
            S.op("dve", lambda h: h.reciprocal(tcol[:], tcol[:]), [b_nrm], [b_nrm])
            TT("dve", fcol[:], rr[:], tcol[:], ALU.mult, [b_nrm], [b_nrm])
            for h_ in range(4):
                bank, bb = nbanks[h_ // 2]
                hh = h_ % 2
                ACT(hn[:, ch, h_, :], bank[0:64, hh * 256:(hh + 1) * 256], AF.Identity, [bb, b_nrm], [b_hn],
                    scale=fcol[:, h_:h_ + 1])
            for pr in range(2):
                bank, bb = nextbank()
                for hh in range(2):
                    h_ = pr * 2 + hh
                    MM(bank[:, hh * 256:(hh + 1) * 256], kw[:, h_, :], vtok[:, ch, h_ * 256:(h_ + 1) * 256], True, True,
                       [b_kw[h_], b_kv], [bb])
                    MM(PB[3][:, 8 + h_: 9 + h_], kw[:, h_, :], cb_t[0:64, B_ONE:B_ONE + 1], True, True, [b_kw[h_], b_c],
                       [PBb[3]])
                for hh in range(2):
                    h_ = pr * 2 + hh
                    STT("dve", Cst[:, h_, :], Cst[:, h_, :], dec[:, h_:h_ + 1], bank[:, hh * 256:(hh + 1) * 256],
                        ALU.mult, ALU.add, [bb, b_sm, b_C], [b_C])
                    STT("dve", nst[:, h_:h_ + 1], nst[:, h_:h_ + 1], dec[:, h_:h_ + 1], PB[3][:, 8 + h_: 9 + h_],
                        ALU.mult, ALU.add, [PBb[3], b_sm, b_C], [b_C])
            CP("act", Cbf[:].rearrange("p h v -> p (h v)"), Cst[:].rearrange("p h v -> p (h v)"), [b_C], [b_Cbf])
            CP("act", nbf[:], nst[:], [b_C], [b_Cbf])
            hf = ch % 2
            for idx in range(8):
                h_, half = idx // 2, idx % 2
                TR(PT[hf][:, idx * 64:(idx + 1) * 64], hn[:, ch, h_, half * 128:(half + 1) * 128],
                   cb_t[0:64, B_ID:B_ID + 64], [b_hn, b_c], [PTb[hf]])
            for idx in range(8):
                STT("dve", ymixT[:, 8 + idx, cs], PT[hf][:, idx * 64:(idx + 1) * 64],
                    cvc[:, 24 + idx: 25 + idx], sgo[:, idx, cs], ALU.mult, ALU.mult, [PTb[hf], b_sgo, b_c], [b_ym])
        if dbg and st == 0:
            DMA("sp", L["d_ymix"][:], ymixT[:].rearrange("p k n -> p (k n)"), [b_ym], [Buf()])

        if MIXSTOP == 5:
            break
        for pc in range(4):
            wt, bw = load_w(w_out_v[:, :, pc * 512:(pc + 1) * 512])
            for i in range(TPS):
                bank, bb = nextbank()
                for k in range(16):
                    MM(bank[:, :], ymixT[:, k, i * 128:(i + 1) * 128], wt[:, k, :], k == 0, k == 15, [bw, b_ym], [bb])
                TT("dve", tmpw[:], bank[:, :], g1_bc[:, pc * 512:(pc + 1) * 512], ALU.mult, [bb, b_rows], [b_tmpw])
                STT("dve", xall[:, i, pc * 512:(pc + 1) * 512], xall[:, i, pc * 512:(pc + 1) * 512], ALPHA, tmpw[:],
                    ALU.mult, ALU.add, [b_tmpw, b_x[i]], [b_x[i]])
        if MIXSTOP == 6:
            break
        DMA("sp", ln1g, ln_bc[:, 0:D], [], [b_hT])
        DMA("sp", ln1b, ln_bc[:, D:2 * D], [], [b_ym])
        for i in range(TPS):
            rows = slice(t0 + i * 128, t0 + (i + 1) * 128)
            ln_stats(xall[:, i, :], [b_x[i]])
            ACT(x1t, xall[:, i, :], AF.Identity, [b_x[i], b_stat], [b_x1t], bias=nmr[:], scale=rstd[:])
            TT("pool", x1t, x1t, ln1g, ALU.mult, [b_x1t, b_hT], [b_x1t])
            TT("pool", x1t, x1t, ln1b, ALU.add, [b_x1t, b_ym], [b_x1t])
            DMA("sp", x1_d[rows, :], x1t, [b_x1t], [b_dx])
            ln_stats(x1t, [b_x1t])
            ACT(xall[:, i, :], x1t, AF.Identity, [b_x1t, b_stat], [b_x[i]], bias=nmr[:], scale=rstd[:])
            TT("dve", xall[:, i, :], xall[:, i, :], sc2p_bc[:], ALU.mult, [b_x[i], b_rows], [b_x[i]])
            TT("dve", h2b, xall[:, i, :], sh2_bc[:], ALU.add, [b_x[i], b_rows], [b_h2b])
            DMA("sp", h2_d[rows, :], h2b, [b_h2b], [b_dh])
    S.barrier()
    sm.close()


def build_moe(nc, S, L):
    (MM, TR, ACT, TT, TS, STT, CP, DMA, RED, sb, PB, PBb, PT, PTb) = (
        L[k] for k in ("MM", "TR", "ACT", "TT", "TS", "STT", "CP", "DMA", "RED", "sb", "PB", "PBb", "PT", "PTb"))
    cf_t, cb_t, b_c, g2_bc, b_g2 = (L[k] for k in ("cf_t", "cb_t", "b_c", "g2_bc", "b_g2"))
    x1_d, h2_d, Xs, Ys, out, ln_bc = (L[k] for k in ("x1_d", "h2_d", "Xs", "Ys", "out", "ln_bc"))
    router_w, rb_bc, wg_l, wu_l, wd_l, bgu, b_down = (L[k] for k in ("router_w", "rb_bc", "wg_l", "wu_l", "wd_l", "bgu",
                                                                      "b_down"))
    dbg = L["dbg"]
    phases = L["phases"]
    ident_b = cb_t[:, B_ID:B_ID + 128]
    sp_ = ExitStack()

    def P(name, shape, dt):
        return sb(name, shape, dt, sp_)

    dest_i = P("dest_i", [128, NT * 4], I32)
    wcol = P("wcol", [128, NT * 4], F32)
    WtT = P("WtT", [32, S_TOK], BF16)
    idxW_i = P("idxW_i", [128, NBLK * 4], I32)
    ej_i = P("ej_i", [128, NBLK], I32)
    bd_bf = P("bd_bf", [32, D], BF16)
    b_rt = Buf("routing")
    DMA("pool", bd_bf[:], b_down[:, :], [], [b_rt])

    with ExitStack() as sc:
        def T(name, shape, dt):
            return sb(name, shape, dt, sc)

        h2all = T("h2all", [128, NT, D], BF16)
        b_h2 = [Buf() for _ in range(NT)]
        rwb = T("rwb", [128, 16, NE], BF16)
        rbb = T("rbb", [128, NE], F32)
        h2T = [T("h2T%d" % i, [128, 16, 128], BF16) for i in range(2)]
        b_h2T = [Buf() for _ in range(2)]
        logit_all = T("logit_all", [128, NT, NE], F32)
        top8_all = T("top8_all", [128, NT, 8], F32)
        Wt_all = T("Wt_all", [128, NT, NE], F32)
        pos_all = T("pos_all", [128, NT, NE], F32)
        b_la = Buf()
        maskf = T("maskf", [128, NE], F32)
        mask_bf = T("mask_bf", [128, NE], BF16)
        nmx = T("nmx", [128, 1], F32)
        ex = T("ex", [128, NE], F32)
        rs = T("rs", [128, 1], F32)
        b_tl = Buf()
        cnt = T("cnt", [128, NE], F32)
        b_cnt = Buf()
        zero1 = T("zero1", [128, 1], F32)
        yv = T("yv", [128, NE], F32)
        fr = T("fr", [128, NE], F32)
        nb = T("nb", [128, NE], F32)
        bend = T("bend", [128, NE], F32)
        base = T("base", [128, NE], F32)
        destf = T("destf", [128, NE], F32)
        oh = T("oh", [128, NE], F32)
        prod = T("prod", [128, NE], F32)
        dcol = T("dcol", [128, NT * 4], F32)
        ej = T("ej", [128, NBLK], F32)
        t1 = T("t1", [128, NBLK], F32)
        idxf = T("idxf", [128, NBLK * 4], F32)
        b_g = Buf()

        rw_v = router_w.rearrange("(k p) e -> p k e", p=128)
        DMA("pool", rwb[:], rw_v, [], [b_rt])
        DMA("sp", rbb[:], rb_bc[:], [], [b_rt])
        S.op("pool", lambda h: h.memset(cnt[:], 0.0), [], [b_cnt])
        S.op("pool", lambda h: h.memset(zero1[:], 0.0), [], [b_cnt])
        zt = T("zt", [128, 4, D], BF16)
        S.op("pool", lambda h: h.memset(zt[:], 0.0), [], [b_cnt])
        for t in range(NT):
            DMA("sp", h2all[:, t, :], h2_d[t * 128:(t + 1) * 128, :], [], [b_h2[t]])
        for t in range(NT):
            j = t % 2
            for kg in range(4):
                hf = kg % 2
                for kk in range(4):
                    k = kg * 4 + kk
                    TR(PT[hf][:, kk * 128:(kk + 1) * 128], h2all[:, t, k * 128:(k + 1) * 128], ident_b,
                       [b_h2[t], b_c], [PTb[hf]])
                CP("act" if kg % 2 else "dve", h2T[j][:, kg * 4:(kg + 1) * 4, :].rearrange("p k n -> p (k n)"),
                   PT[hf][:, 0:512], [PTb[hf]], [b_h2T[j]])
            for k in range(16):
                MM(PB[3][:, 0:NE], h2T[j][:, k, :], rwb[:, k, :], k == 0, k == 15, [b_h2T[j], b_rt], [PBb[3]])
            lg = logit_all[:, t, :]
            TT("dve", lg, PB[3][:, 0:NE], rbb[:], ALU.add, [PBb[3], b_rt], [b_la])
            S.op("dve", (lambda t: lambda h: h.max(out=top8_all[:, t, :], in_=logit_all[:, t, :]))(t), [b_la], [b_la])
            TS("dve", maskf[:], lg, top8_all[:, t, 3:4], ALU.is_ge, [b_la], [b_tl])
            TS("dve", nmx[:], top8_all[:, t, 0:1], -1.0, ALU.mult, [b_la], [b_tl])
            ACT(ex[:], lg, AF.Exp, [b_la, b_tl], [b_tl], bias=nmx[:])
            TT("dve", ex[:], ex[:], maskf[:], ALU.mult, [b_tl], [b_tl])
            RED(rs[:], ex[:], ALU.add, [b_tl], [b_tl])
            S.op("dve", lambda h: h.reciprocal(rs[:], rs[:]), [b_tl], [b_tl])
            TS("dve", Wt_all[:, t, :], ex[:], rs[:], ALU.mult, [b_tl], [b_la])
            CP("dve", mask_bf[:], maskf[:], [b_tl], [b_tl])
            MM(PB[4][:, 0:NE], cb_t[:, B_TRI:B_TRI + 128], mask_bf[:], True, True, [b_tl, b_c], [PBb[4]])
            MM(PB[4][:, NE:2 * NE], cb_t[:, B_ONE:B_ONE + 128], mask_bf[:], True, True, [b_tl, b_c], [PBb[4]])
            TT("dve", pos_all[:, t, :], PB[4][:, 0:NE], cnt[:], ALU.add, [PBb[4], b_cnt], [b_la])
            TT("dve", cnt[:], cnt[:], PB[4][:, NE:2 * NE], ALU.add, [PBb[4], b_cnt], [b_cnt])
            TR(PB[5][0:32, 0:128], Wt_all[:, t, :], cf_t[:, C_ID:C_ID + 128], [b_la, b_c], [PBb[5]])
            CP("act", WtT[:, t * 128:(t + 1) * 128], PB[5][0:32, 0:128], [PBb[5]], [b_rt])
        TS("dve", nb[:], cnt[:], 0.0, ALU.is_gt, [b_cnt], [b_g])
        for m_ in range(1, 8):
            TS("dve", fr[:], cnt[:], float(m_ * BLK), ALU.is_gt, [b_cnt, b_g], [b_g])
            TT("dve", nb[:], nb[:], fr[:], ALU.add, [b_g], [b_g])
        S.op("dve", lambda h: h.tensor_tensor_scan(out=bend[:], data0=cf_t[:, C_ONE:C_ONE + NE], data1=nb[:],
                                                  initial=zero1[:, 0:1], op0=ALU.mult, op1=ALU.add), [b_g, b_c, b_cnt], [b_g])
        TT("dve", base[:], bend[:], nb[:], ALU.subtract, [b_g], [b_g])
        TS("dve", base[:], base[:], 512.0, ALU.mult, [b_g], [b_g])
        for t in range(NT):
            TT("dve", destf[:], pos_all[:, t, :], base[:], ALU.add, [b_la, b_g], [b_g])
            for k in range(4):
                TS("dve", oh[:], logit_all[:, t, :], top8_all[:, t, k:k + 1], ALU.is_equal, [b_la, b_g], [b_g])
                TT("dve", prod[:], oh[:], destf[:], ALU.mult, [b_g], [b_g])
                RED(dcol[:, t * 4 + k: t * 4 + k + 1], prod[:], ALU.add, [b_g], [b_g])
                TT("dve", prod[:], oh[:], Wt_all[:, t, :], ALU.mult, [b_g, b_la], [b_g])
                RED(wcol[:, t * 4 + k: t * 4 + k + 1], prod[:], ALU.add, [b_g], [b_rt])
        CP("dve", dest_i[:], dcol[:], [b_g], [b_rt])
        for j in range(NBLK):
            TS("dve", oh[:], bend[:], float(j), ALU.is_le, [b_g], [b_g])
            RED(ej[:, j:j + 1], oh[:], ALU.add, [b_g], [b_g])
        TS("dve", ej[:], ej[:], float(NE - 1), ALU.min, [b_g], [b_g])
        CP("dve", ej_i[:], ej[:], [b_g], [b_rt])
        TS("dve", t1[:], ej[:], 512.0, ALU.mult, [b_g, b_c], [b_g], s2=cf_t[:, C_IOTA:C_IOTA + 1], op1=ALU.add)
        idxv = idxf[:].rearrange("p (j q) -> p j q", q=4)
        for q in range(4):
            TS("dve", idxv[:, :, q], t1[:], float(q * 128), ALU.add, [b_g], [b_g])
        CP("dve", idxW_i[:], idxf[:], [b_g], [b_rt])
        if dbg:
            dr = L["d_rt"]
            DMA("sp", dr[:, 0:NT * 32], logit_all[:].rearrange("p t e -> p (t e)"), [b_la], [Buf()])
            DMA("sp", dr[:, NT * 32:NT * 36], dcol[:], [b_g], [Buf()])
            DMA("sp", dr[:, NT * 36:NT * 40], wcol[:], [b_rt], [Buf()])
        b_xs = Buf("Xs")
        for j in range(NBLK):
            DMA("sp", Xs[j * BLK:(j + 1) * BLK, :].rearrange("(i p) d -> p i d", p=128), zt[:], [b_cnt], [b_xs])
        for t in range(NT):
            for k in range(4):
                S.dma("pool", (lambda t, k: lambda h: h.indirect_dma_start(
                    out=Xs[:, :], out_offset=bass.IndirectOffsetOnAxis(ap=dest_i[:, t * 4 + k: t * 4 + k + 1], axis=0),
                    in_=h2all[:, t, :], in_offset=None))(t, k), [b_rt, b_h2[t]], [b_xs])
        S.barrier()

    b_ys = Buf("Ys")
    if "D" in phases:
        with ExitStack() as sd:
            def T(name, shape, dt):
                return sb(name, shape, dt, sd)

            Xb = T("Xb", [128, 4, D], BF16)
            b_Xb = Buf()
            XbT = [T("XbT%d" % i, [128, 16, BLK], BF16) for i in range(2)]
            b_XbT = [Buf() for _ in range(2)]
            NWQ = 4
            wq = [T("wq%d" % i, [128, 8192], BF16) for i in range(NWQ)]
            b_wq = [Buf() for _ in range(NWQ)]
            actT = T("actT", [128, 16, BLK], BF16)
            b_act = Buf()
            ystage = [T("ystage%d" % i, [128, 4, 512], F32) for i in range(2)]
            b_yst = [Buf() for _ in range(2)]
            bgub = T("bgub", [128, 2 * D], BF16)
            b_bgu = Buf()
            a_t = [T("a_t%d" % i, [128, BLK], F32) for i in range(2)]
            sg_t = [T("sg_t%d" % i, [128, BLK], F32) for i in range(2)]
            u_t = [T("u_t%d" % i, [128, BLK], F32) for i in range(2)]
            b_sw = [Buf() for _ in range(2)]
            wqc = [0]
            swc = [0]

            def gather_w(src, col):
                i = wqc[0] % NWQ
                wqc[0] += 1
                S.dma("pool", lambda h: h.indirect_dma_start(
                    out=wq[i][:, :], out_offset=None, in_=src[:, :],
                    in_offset=bass.IndirectOffsetOnAxis(ap=idxW_i[:, col:col + 1], axis=0)), [b_rt], [b_wq[i]])
                return wq[i], b_wq[i]

            gu_banks = [(PB[0], PBb[0], PB[1], PBb[1]), (PB[2], PBb[2], PB[3], PBb[3])]
            dn_banks = [(PB[4], PBb[4]), (PB[5], PBb[5])]
            guc = [0]
            dnc = [0]
            for j in range(NBLK):
                xt = XbT[j % 2]
                bxt = b_XbT[j % 2]
                DMA("sp", Xb[:], Xs[j * BLK:(j + 1) * BLK, :].rearrange("(i p) d -> p i d", p=128), [b_xs], [b_Xb])
                S.dma("pool", (lambda j: lambda h: h.indirect_dma_start(
                    out=bgub[:, :], out_offset=None, in_=bgu[:, :],
                    in_offset=bass.IndirectOffsetOnAxis(ap=ej_i[:, j:j + 1], axis=0)))(j), [b_rt], [b_bgu])
                for k in range(16):
                    hf = k % 2
                    for i in range(4):
                        TR(PT[hf][:, i * 128:(i + 1) * 128], Xb[:, i, k * 128:(k + 1) * 128], ident_b,
                           [b_Xb, b_c], [PTb[hf]])
                    CP("act" if k % 2 else "dve", xt[:, k, :], PT[hf][:, 0:512], [PTb[hf]], [bxt])
                for q in range(4):
                    wg, bwg = gather_w(wg_l, j * 4 + q)
                    wu, bwu = gather_w(wu_l, j * 4 + q)
                    for fc in range(4):
                        f0 = (q * 4 + fc) * 128
                        pg, bpg, pu, bpu = gu_banks[guc[0] % 2]
                        guc[0] += 1
                        MM(pg[:, :], bgub[0:1, f0:f0 + 128], cb_t[0:1, B_ONE:B_ONE + 512], True, False, [b_bgu, b_c], [bpg])
                        for k in range(16):
                            MM(pg[:, :], wg[:, k * 512 + fc * 128: k * 512 + (fc + 1) * 128], xt[:, k, :], False, k == 15,
                               [bwg, bxt], [bpg])
                        MM(pu[:, :], bgub[0:1, D + f0: D + f0 + 128], cb_t[0:1, B_ONE:B_ONE + 512], True, False,
                           [b_bgu, b_c], [bpu])
                        for k in range(16):
                            MM(pu[:, :], wu[:, k * 512 + fc * 128: k * 512 + (fc + 1) * 128], xt[:, k, :], False, k == 15,
                               [bwu, bxt], [bpu])
                        s_ = swc[0] % 2
                        swc[0] += 1
                        TS("dve", a_t[s_][:], pg[:, :], 7.0, ALU.min, [bpg], [b_sw[s_]])
                        ACT(sg_t[s_][:], a_t[s_][:], AF.Sigmoid, [b_sw[s_]], [b_sw[s_]], scale=1.702)
                        TS("dve", u_t[s_][:], pu[:, :], 7.0, ALU.min, [bpu], [b_sw[s_]], s2=-7.0, op1=ALU.max)
                        TT("pool", a_t[s_][:], a_t[s_][:], sg_t[s_][:], ALU.mult, [b_sw[s_]], [b_sw[s_]])
                        STT("dve", actT[:, q * 4 + fc, :], u_t[s_][:], 1.0, a_t[s_][:], ALU.add, ALU.mult, [b_sw[s_]],
                            [b_act])
                for q in range(4):
                    wd, bwd = gather_w(wd_l, j * 4 + q)
                    for i in range(4):
                        pd, bpd = dn_banks[dnc[0] % 2]
                        dnc[0] += 1
                        for fc in range(16):
                            MM(pd[:, :], actT[:, fc, i * 128:(i + 1) * 128], wd[:, fc * 512:(fc + 1) * 512], fc == 0,
                               fc == 15, [b_act, bwd], [bpd])
                        CP("act" if (i % 2) else "dve", ystage[q % 2][:, i, :], pd[:, :], [bpd], [b_yst[q % 2]])
                    DMA("sp", Ys[j * BLK:(j + 1) * BLK, q * 512:(q + 1) * 512].rearrange("(i p) d -> p i d", p=128),
                        ystage[q % 2][:], [b_yst[q % 2]], [b_ys])
            S.barrier()

    if "E" in phases:
        with ExitStack() as se:
            def T(name, shape, dt):
                return sb(name, shape, dt, se)

            yk = [T("yk%d" % i, [128, D], F32) for i in range(4)]
            b_yk = [Buf() for _ in range(4)]
            acc = T("acc", [128, D], F32)
            b_acc = Buf()
            x1t = T("x1e", [128, D], F32)
            b_x1 = Buf()
            outt = T("outt", [128, D], F32)
            b_ot = Buf()
            ln2g = T("ln2g", [128, D], F32)
            ln2b = T("ln2b", [128, D], F32)
            st6 = T("st6e", [128, 24], F32)
            mv = T("mve", [128, 2], F32)
            rstd = T("rstde", [128, 1], F32)
            nmr = T("nmre", [128, 1], F32)
            b_stat = Buf()
            b_out = Buf()
            DMA("sp", ln2g[:], ln_bc[:, 2 * D:3 * D], [], [b_c])
            DMA("sp", ln2b[:], ln_bc[:, 3 * D:4 * D], [], [b_c])
            for t in range(NT):
                rows = slice(t * 128, (t + 1) * 128)
                for k in range(4):
                    S.dma("pool", (lambda t, k: lambda h: h.indirect_dma_start(
                        out=yk[k][:, :], out_offset=None, in_=Ys[:, :],
                        in_offset=bass.IndirectOffsetOnAxis(ap=dest_i[:, t * 4 + k: t * 4 + k + 1], axis=0)))(t, k),
                        [b_rt, b_ys], [b_yk[k]])
                DMA("sp", x1t[:], x1_d[rows, :], [], [b_x1])
                TS("dve", acc[:], yk[0][:], wcol[:, t * 4: t * 4 + 1], ALU.mult, [b_yk[0], b_rt], [b_acc])
                for k in range(1, 4):
                    STT("dve", acc[:], yk[k][:], wcol[:, t * 4 + k: t * 4 + k + 1], acc[:], ALU.mult, ALU.add,
                        [b_yk[k], b_rt, b_acc], [b_acc])
                for pc in range(4):
                    bank, bb = PB[pc % 2], PBb[pc % 2]
                    MM(bank[:, :], WtT[:, t * 128:(t + 1) * 128], bd_bf[:, pc * 512:(pc + 1) * 512], True, True, [b_rt], [bb])
                    TT("dve", acc[:, pc * 512:(pc + 1) * 512], acc[:, pc * 512:(pc + 1) * 512], bank[:, :], ALU.add,
                       [bb, b_acc], [b_acc])
                TT("pool", acc[:], acc[:], g2_bc[:], ALU.mult, [b_acc, b_g2], [b_acc])
                STT("dve", acc[:], x1t[:], ALPHA, acc[:], ALU.mult, ALU.add, [b_x1, b_acc], [b_acc])
                for c4 in range(4):
                    S.op("dve", (lambda c4: lambda h: h.bn_stats(st6[:, c4 * 6:(c4 + 1) * 6], acc[:, c4 * 512:(c4 + 1) * 512]))(c4),
                         [b_acc], [b_stat])
                S.op("dve", lambda h: h.bn_aggr(mv[:], st6[:]), [b_stat], [b_stat])
                ACT(rstd[:], mv[:, 1:2], AF.Sqrt, [b_stat], [b_stat], bias=EPS)
                S.op("dve", lambda h: h.reciprocal(rstd[:], rstd[:]), [b_stat], [b_stat])
                STT("dve", nmr[:], mv[:, 0:1], -1.0, rstd[:], ALU.mult, ALU.mult, [b_stat], [b_stat])
                ACT(outt[:], acc[:], AF.Identity, [b_acc, b_stat], [b_ot], bias=nmr[:], scale=rstd[:])
                TT("pool", outt[:], outt[:], ln2g[:], ALU.mult, [b_ot, b_c], [b_ot])
                TT("pool", outt[:], outt[:], ln2b[:], ALU.add, [b_ot, b_c], [b_ot])
                DMA("sp", out[rows, :], outt[:], [b_ot], [b_out])
            S.barrier()
    sp_.close()


def prep_shared(inp):
    f = lambda a: np.ascontiguousarray(a, dtype=np.float32)
    b_in = np.asarray(inp["b_in"][0], np.float32)
    pad = np.zeros(41 * 128, np.float32)
    pad[:DIN] = b_in
    col8 = lambda v: np.asarray(v, np.float32).reshape(8, 128).T

    def relayout(w):
        w = np.asarray(w, np.float32).reshape(NE, 16, 128, 4, 512)
        return np.ascontiguousarray(w.transpose(0, 3, 2, 1, 4)).reshape(NE * 4 * 128, 16 * 512)

    cfc, cbc = make_consts()
    sh = {
        "w_ada": f(inp["w_ada"][0]),
        "b_ada": f(inp["b_ada"][0][None, :]),
        "w_in": f(inp["w_in"][0]),
        "b_in_col": f(pad.reshape(41, 128).T),
        "b_kv_bc": f(np.tile(b_in[2560:4096][None, :], (128, 1))),
        "dw_col": f(np.asarray(inp["dw_w"][0], np.float32).reshape(31, 8, 128).transpose(2, 1, 0).reshape(128, 248)),
        "cv_col": f(np.concatenate([col8(inp["dw_b"][0]), col8(inp["conv_ln_g"][0]), col8(inp["conv_ln_b"][0]),
                                    col8(inp["mh_g"][0])], axis=1)),
        "w_out": f(inp["w_out"][0]),
        "ln_bc": f(np.tile(np.concatenate([inp["ln1_g"][0], inp["ln1_b"][0], inp["ln2_g"][0], inp["ln2_b"][0]])[None, :],
                           (128, 1))),
        "router_w": f(inp["router_w"][0]),
        "rb_bc": f(np.tile(np.asarray(inp["router_b"][0], np.float32)[None, :], (128, 1))),
        "wg_l": relayout(inp["w_gate"][0]),
        "wu_l": relayout(inp["w_up"][0]),
        "wd_l": relayout(inp["w_down"][0]),
        "bgu": f(np.concatenate([inp["b_gate"][0], inp["b_up"][0]], axis=1)),
        "b_down": f(inp["b_down"][0]),
        "cf": cfc,
        "cb": cbc,
    }
    return sh


def core_inputs(inp, sh, b):
    m = dict(sh)
    m["x"] = np.ascontiguousarray(inp["x"][b], dtype=np.float32)
    m["c_col"] = np.ascontiguousarray(np.asarray(inp["c"][b], np.float32).reshape(16, 128).T)
    return m


_NC_CACHE = {}


def kernel(**inputs):
    sh = prep_shared(inputs)
    if "nc" not in _NC_CACHE:
        _NC_CACHE["nc"] = build()
    nc = _NC_CACHE["nc"]
    in_maps = [core_inputs(inputs, sh, b) for b in range(8)]
    res = run_bass_kernel_spmd(nc, in_maps, core_ids=list(range(8)))
    return np.stack([np.asarray(r["out"], dtype=np.float32) for r in res.results], axis=0)
```

```python
import numpy as np
from contextlib import ExitStack
import concourse.bass as bass
import concourse.mybir as mybir
from concourse.bass_utils import run_bass_kernel_spmd

F32 = mybir.dt.float32
BF16 = mybir.dt.bfloat16
I32 = mybir.dt.int32
AF = mybir.ActivationFunctionType
ALU = mybir.AluOpType
AX = mybir.AxisListType

NRING = 8
MIXSTOP = 0
EVAC_DVE = 1
B1STOP = 0


class Buf:
    __slots__ = ("name", "w", "rs")

    def __init__(self, name=""):
        self.name = name
        self.w = None
        self.rs = {}


class _Op:
    __slots__ = ("waits", "fn", "signal", "is_dma", "dma_sem")


class _Eng:
    def __init__(self, name):
        self.name = name
        self.ops = []
        self.waited = {}
        self.ndma = 0
        self.ring_tok = [None] * NRING


class Sched:
    def __init__(self, nc):
        self.nc = nc
        self.engs = {n: _Eng(n) for n in ("pe", "act", "dve", "pool", "sp")}
        self.order = []

    def _add_wait(self, eng, op, tok):
        if tok is None:
            return
        if tok[0] == "c":
            if tok[1] == eng.name and eng.name == "pe":
                return
            key = ("c", tok[1])
            v = tok[2]
        else:
            key = ("d", tok[1], tok[2])
            v = tok[3]
        if eng.waited.get(key, -1) >= v:
            return
        eng.waited[key] = v
        op.waits.append(tok)
        if tok[0] == "c":
            self.engs[tok[1]].ops[tok[2]].signal = True

    def _deps(self, eng, op, reads, writes):
        for b in reads:
            self._add_wait(eng, op, b.w)
        for b in writes:
            self._add_wait(eng, op, b.w)
            for t in list(b.rs.values()):
                self._add_wait(eng, op, t)

    def _commit(self, tok, reads, writes):
        key = tok[:2] if tok[0] == "c" else tok[:3]
        for b in reads:
            b.rs[key] = tok
        for b in writes:
            b.w = tok
            b.rs = {}

    def _new(self, engname, fn, is_dma):
        eng = self.engs[engname]
        o = _Op()
        o.waits = []
        o.fn = fn
        o.signal = False
        o.is_dma = is_dma
        o.dma_sem = None
        return eng, o

    def op(self, engname, fn, reads=(), writes=()):
        eng, o = self._new(engname, fn, False)
        self._deps(eng, o, reads, writes)
        idx = len(eng.ops)
        eng.ops.append(o)
        self.order.append((engname, idx))
        tok = ("c", engname, idx)
        self._commit(tok, reads, writes)
        return tok

    def dma(self, engname, fn, reads=(), writes=()):
        eng, o = self._new(engname, fn, True)
        slot = eng.ndma % NRING
        val = 16 * (eng.ndma // NRING + 1)
        eng.ndma += 1
        self._add_wait(eng, o, eng.ring_tok[slot])
        self._deps(eng, o, reads, writes)
        o.dma_sem = slot
        idx = len(eng.ops)
        eng.ops.append(o)
        self.order.append((engname, idx))
        tok = ("d", engname, slot, val)
        eng.ring_tok[slot] = tok
        self._commit(tok, reads, writes)
        return tok

    def all_tokens(self):
        toks = []
        for n, e in self.engs.items():
            if n != "sp":
                for i in range(len(e.ops) - 1, -1, -1):
                    if (not e.ops[i].is_dma) and e.ops[i].fn is not None:
                        toks.append(("c", n, i))
                        break
            toks.extend(t for t in e.ring_tok if t is not None)
        return toks

    def barrier(self):
        toks = self.all_tokens()
        for n in self.engs:
            eng, o = self._new(n, None, False)
            for t in toks:
                self._add_wait(eng, o, t)
            idx = len(eng.ops)
            eng.ops.append(o)
            self.order.append((n, idx))

    def emit(self, stack):
        nc = self.nc
        csem = {n: stack.enter_context(nc.semaphore("c_" + n)) for n in self.engs}
        dsem = {}
        for n in ("sp", "pool"):
            for s in range(NRING):
                dsem[(n, s)] = stack.enter_context(nc.semaphore("d_%s_%d" % (n, s)))
        pref = {}
        for n, e in self.engs.items():
            c = 0
            arr = []
            for o in e.ops:
                if o.signal and not o.is_dma and o.fn is not None:
                    c += 1
                arr.append(c)
            pref[n] = arr
        hs = {"pe": nc.tensor, "act": nc.scalar, "dve": nc.vector, "pool": nc.gpsimd, "sp": nc.sync}
        for (n, i) in self.order:
            o = self.engs[n].ops[i]
            h = hs[n]
            for t in o.waits:
                if t[0] == "c":
                    h.wait_ge(csem[t[1]], pref[t[1]][t[2]])
                else:
                    h.wait_ge(dsem[(t[1], t[2])], t[3])
            if o.fn is None:
                continue
            inst = o.fn(h)
            if o.is_dma:
                inst.then_inc(dsem[(n, o.dma_sem)], 16)
            elif o.signal:
                inst.then_inc(csem[n], 1)


S_TOK = 4096
D = 2048
NT = 32
ST = 256
NST = S_TOK // ST
TPS = ST // 128
CPS = ST // 64
DIN = 5128
NE = 32
BLK = 512
NBLK = 63
NSLOT = NBLK * BLK
ALPHA = 2.0 ** 0.25
EPS = 1e-5
QS = 128.0 ** -0.5

C_ID = 0
C_MASK = 128
C_IOTA = 192
C_SELI = 193
C_SELF = 197
C_NEGI = 201
C_SELB = 205
C_ONE = 717
C_IOE = 1229
C_IO16 = 1292
CF_W = 1296
B_ID = 0
B_ONE = 128
B_TRI = 640
CB_W = 768


def make_consts():
    import ml_dtypes
    cf = np.zeros((128, CF_W), np.float32)
    cf[:, C_ID:C_ID + 128] = np.eye(128, dtype=np.float32)
    p = np.arange(128)
    cf[:, C_MASK:C_MASK + 64] = ((p % 64)[:, None] <= np.arange(64)[None, :]).astype(np.float32)
    cf[:, C_IOTA] = p
    for h in range(4):
        cf[h, C_SELI + h] = 1.0
        cf[h + 4, C_SELF + h] = 1.0
        cf[h, C_NEGI + h] = -1.0
        cf[h, C_SELB + h * 128:C_SELB + (h + 1) * 128] = 1.0
    cf[:, C_ONE:C_ONE + 512] = 1.0
    cf[:, C_IOE:C_IOE + 63] = np.arange(63, dtype=np.float32)[None, :]
    cf[:, C_IO16:C_IO16 + 4] = (np.arange(4, dtype=np.float32) * 128.0)[None, :]
    cb = np.zeros((128, CB_W), np.float32)
    cb[:, B_ID:B_ID + 128] = np.eye(128)
    cb[:, B_ONE:B_ONE + 512] = 1.0
    cb[:, B_TRI:B_TRI + 128] = (p[:, None] < p[None, :]).astype(np.float32)
    return cf, cb.astype(ml_dtypes.bfloat16)


def build(dbg=False, phases=("A", "B", "C", "D", "E")):
    nc = bass.Bass("TRN2", target_bir_lowering=False)

    def din(name, shape, dt=F32):
        return nc.dram_tensor(name, list(shape), dt, kind="ExternalInput").ap()

    x = din("x", [S_TOK, D])
    c_col = din("c_col", [128, 16])
    w_ada = din("w_ada", [D, 6 * D])
    b_ada = din("b_ada", [1, 6 * D])
    w_in = din("w_in", [D, DIN])
    b_in_col = din("b_in_col", [128, 41])
    b_kv_bc = din("b_kv_bc", [128, 1536])
    dw_col = din("dw_col", [128, 8 * 31])
    cv_col = din("cv_col", [128, 32])
    w_out = din("w_out", [D, D])
    ln_bc = din("ln_bc", [128, 4 * D])
    router_w = din("router_w", [D, NE])
    rb_bc = din("rb_bc", [128, NE])
    if "C" in phases:
        wg_l = din("wg_l", [16384, 8192])
        wu_l = din("wu_l", [16384, 8192])
        wd_l = din("wd_l", [16384, 8192])
    else:
        wg_l = wu_l = wd_l = None
    bgu = din("bgu", [NE, 2 * D])
    b_down = din("b_down", [NE, D])
    cf = din("cf", [128, CF_W])
    cb = din("cb", [128, CB_W], BF16)
    out = nc.dram_tensor("out", [S_TOK, D], F32, kind="ExternalOutput").ap()
    ikind = "ExternalOutput" if dbg else "Internal"
    x1_d = nc.dram_tensor("x1_d", [S_TOK, D], F32, kind=ikind).ap()
    h2_d = nc.dram_tensor("h2_d", [S_TOK, D], BF16, kind=ikind).ap()
    Xs = nc.dram_tensor("Xs", [NSLOT, D], BF16, kind="Internal").ap()
    Ys = nc.dram_tensor("Ys", [NSLOT, D], F32, kind="Internal").ap()
    if dbg:
        d_ymix = nc.dram_tensor("d_ymix", [128, 16 * ST], BF16, kind="ExternalOutput").ap()
        d_hT = nc.dram_tensor("d_hT", [128, 16 * ST], BF16, kind="ExternalOutput").ap()
        d_rt = nc.dram_tensor("d_rt", [128, NT * 40], F32, kind="ExternalOutput").ap()

    gs = ExitStack()
    S = Sched(nc)

    def sb(name, shape, dt, st=None):
        return (st or gs).enter_context(nc.sbuf_tensor(name, list(shape), dt))

    def MM(o, lhsT, rhs, start, stop, reads, writes, **kw):
        S.op("pe", lambda h: h.matmul(o, lhsT, rhs, start=start, stop=stop, **kw), reads, writes)

    def TR(o, in_, ident, reads, writes):
        S.op("pe", lambda h: h.transpose(o, in_, ident), reads, writes)

    def ACT(o, in_, func, reads, writes, bias=None, scale=None, eng="act"):
        kw = {}
        if bias is not None:
            kw["bias"] = bias
        if scale is not None:
            kw["scale"] = scale
        S.op("act", lambda h: h.activation(out=o, in_=in_, func=func, **kw), reads, writes)

    def TT(eng, o, a, b, op, reads, writes):
        S.op(eng, lambda h: h.tensor_tensor(out=o, in0=a, in1=b, op=op), reads, writes)

    def TS(eng, o, a, s1, op0, reads, writes, s2=None, op1=None):
        if op1 is None:
            S.op(eng, lambda h: h.tensor_scalar(out=o, in0=a, scalar1=s1, scalar2=None, op0=op0), reads, writes)
        else:
            S.op(eng, lambda h: h.tensor_scalar(out=o, in0=a, scalar1=s1, scalar2=s2, op0=op0, op1=op1), reads, writes)

    def STT(eng, o, a, sc, b, op0, op1, reads, writes):
        S.op(eng, lambda h: h.scalar_tensor_tensor(out=o, in0=a, scalar=sc, in1=b, op0=op0, op1=op1), reads, writes)

    def CP(eng, o, a, reads, writes):
        if eng == "act":
            S.op("act", lambda h: h.activation(out=o, in_=a, func=AF.Copy), reads, writes)
        else:
            S.op(eng, lambda h: h.tensor_copy(o, a), reads, writes)

    def DMA(q, o, a, reads, writes):
        return S.dma(q, lambda h: h.dma_start(out=o, in_=a), reads, writes)

    def RED(o, a, op, reads, writes):
        S.op("dve", lambda h: h.tensor_reduce(out=o, in_=a, axis=AX.X, op=op), reads, writes)

    PB = [gs.enter_context(nc.psum_tensor("pb%d" % i, [128, 512], F32)) for i in range(6)]
    PBb = [Buf("pb%d" % i) for i in range(6)]
    PTS = [gs.enter_context(nc.psum_tensor("pt%d" % i, [128, 1024], BF16)) for i in range(2)]
    PT = PTS
    PTb = [Buf("pt0"), Buf("pt1")]
    bigc = [0]

    def nextbank():
        i = bigc[0] % 3
        bigc[0] += 1
        return PB[i], PBb[i]

    xc = [0]
    yc = [0]

    def xbank():
        i = (0, 1)[xc[0] % 2]
        xc[0] += 1
        return PB[i], PBb[i]

    def ybank():
        i = (2, 4)[yc[0] % 2]
        yc[0] += 1
        return PB[i], PBb[i]

    cf_t = sb("cf_t", [128, CF_W], F32)
    cb_t = sb("cb_t", [128, CB_W], BF16)
    bcol = sb("bcol", [128, 41], F32)
    bqs = sb("bqs", [128, 4], F32)
    dwc = sb("dwc", [128, 248], F32)
    cvc = sb("cvc", [128, 32], F32)
    mod_col = sb("mod_col", [128, 96], F32)
    sc1p = sb("sc1p", [128, 16], F32)
    g2_bc = sb("g2_bc", [128, D], F32)
    b_c = Buf("consts")
    b_g2 = Buf("g2")
    DMA("sp", cf_t[:], cf[:], [], [b_c])
    DMA("sp", cb_t[:], cb[:], [], [b_c])
    DMA("sp", bcol[:], b_in_col[:], [], [b_c])
    DMA("sp", dwc[:], dw_col[:], [], [b_c])
    DMA("sp", cvc[:], cv_col[:], [], [b_c])
    TS("dve", bqs[:], bcol[:, 16:20], QS, ALU.mult, [b_c], [b_c])
    S.barrier()
    ident_b = cb_t[:, B_ID:B_ID + 128]

    sab = ExitStack()
    g1_bc = sb("g1_bc", [128, D], F32, sab)
    sc2p_bc = sb("sc2p_bc", [128, D], F32, sab)
    sh2_bc = sb("sh2_bc", [128, D], F32, sab)
    b_rows = Buf("rows")

    with ExitStack() as sa:
        mod_row = sb("mod_row", [1, 6 * D], F32, sa)
        b_mr = Buf("mr")
        ccol = sb("ccol", [128, 16], F32, sa)
        scb = sb("scb", [128, 16], BF16, sa)
        b_cc = Buf()
        b_scb = Buf()
        DMA("sp", ccol[:], c_col[:], [], [b_cc])
        ACT(scb[:], ccol[:], AF.Silu, [b_cc], [b_scb])
        wa = [sb("wa%d" % i, [128, 16, 512], BF16, sa) for i in range(3)]
        b_wa = [Buf() for _ in range(3)]
        bar = [sb("bar%d" % i, [1, 512], F32, sa) for i in range(2)]
        b_bar = [Buf() for _ in range(2)]
        w_ada_v = w_ada.rearrange("(k p) n -> p k n", p=128)
        for pc in range(24):
            i = pc % 3
            DMA("pool", wa[i][:], w_ada_v[:, :, pc * 512:(pc + 1) * 512], [], [b_wa[i]])
            DMA("sp", bar[pc % 2][:], b_ada[0:1, pc * 512:(pc + 1) * 512], [], [b_bar[pc % 2]])
            bank, bb = PB[pc % 2], PBb[pc % 2]
            for k in range(16):
                MM(bank[0:1, :], scb[:, k:k + 1], wa[i][:, k, :], k == 0, k == 15, [b_scb, b_wa[i]], [bb])
            TT("dve", mod_row[0:1, pc * 512:(pc + 1) * 512], bank[0:1, :], bar[pc % 2][0:1, :], ALU.add,
               [bb, b_bar[pc % 2]], [b_mr])
        for j in range(96):
            MM(PB[2][:, j:j + 1], mod_row[0:1, j * 128:(j + 1) * 128], cf_t[0:1, C_ONE:C_ONE + 1], True, True,
               [b_mr, b_c], [PBb[2]])
        CP("dve", mod_col[:], PB[2][:, 0:96], [PBb[2]], [b_c])
        TS("dve", sc1p[:], mod_col[:, 16:32], 1.0, ALU.add, [b_c], [b_c])
        for (dst, off, plus1, bdst) in ((g1_bc, 2 * D, False, b_rows), (sh2_bc, 3 * D, False, b_rows),
                                        (sc2p_bc, 4 * D, True, b_rows), (g2_bc, 5 * D, False, b_g2)):
            for i in range(4):
                bank, bb = PB[i % 2], PBb[i % 2]
                MM(bank[:, :], cf_t[0:1, C_ONE:C_ONE + 128], mod_row[0:1, off + i * 512: off + (i + 1) * 512],
                   True, True, [b_mr, b_c], [bb])
                if plus1:
                    ACT(dst[:, i * 512:(i + 1) * 512], bank[:, :], AF.Identity, [bb], [bdst], bias=1.0)
                else:
                    CP("dve", dst[:, i * 512:(i + 1) * 512], bank[:, :], [bb], [bdst])
        S.barrier()
    sh1 = mod_col[:, 0:16]

    if "B" in phases:
        build_mixer(nc, S, locals())
    S.barrier()
    sab.close()

    if "C" in phases:
        build_moe(nc, S, locals())

    S.barrier()
    S.emit(gs)
    gs.close()
    return nc


def build_mixer(nc, S, L):
    (MM, TR, ACT, TT, TS, STT, CP, DMA, RED, sb, PB, PBb, PT, PTb, nextbank) = (
        L[k] for k in ("MM", "TR", "ACT", "TT", "TS", "STT", "CP", "DMA", "RED", "sb", "PB", "PBb", "PT", "PTb",
                       "nextbank"))
    xbank, ybank = L["xbank"], L["ybank"]
    cf_t, cb_t, bcol, bqs, dwc, cvc, mod_col, sc1p, sh1, b_c = (
        L[k] for k in ("cf_t", "cb_t", "bcol", "bqs", "dwc", "cvc", "mod_col", "sc1p", "sh1", "b_c"))
    g1_bc, sc2p_bc, sh2_bc, b_rows = (L[k] for k in ("g1_bc", "sc2p_bc", "sh2_bc", "b_rows"))
    x, w_in, w_out, b_kv_bc, ln_bc, x1_d, h2_d = (L[k] for k in ("x", "w_in", "w_out", "b_kv_bc", "ln_bc", "x1_d", "h2_d"))
    dbg = L["dbg"]
    ident_b = cb_t[:, B_ID:B_ID + 128]
    sm = ExitStack()

    def T(name, shape, dt):
        return sb(name, shape, dt, sm)

    xall = T("xall", [128, TPS, D], F32)
    b_x = [Buf() for _ in range(TPS)]
    xn = T("xn", [128, D], BF16)
    b_xn = Buf()
    st6 = T("st6", [128, 24], F32)
    mv = T("mv", [128, 2], F32)
    rstd = T("rstd", [128, 1], F32)
    nmr = T("nmr", [128, 1], F32)
    b_stat = Buf()
    hT = T("hT", [128, 16, ST], BF16)
    b_hT = Buf()
    wgb = [T("wgb%d" % i, [128, 16, 512], BF16) for i in range(2)]
    b_wg = [Buf() for _ in range(2)]
    wgt = T("wgt", [128, 16, 8], BF16)
    b_wgt = Buf()
    sg = T("sg", [128, 8, ST], BF16)
    b_sg = Buf()
    uT = T("uT", [128, 8, 30 + ST], BF16)
    b_uT = Buf()
    qT = T("qT", [128, 4, ST], BF16)
    kT = T("kT", [128, 4, ST], BF16)
    b_qk = Buf()
    sgo = T("sgo", [128, 8, ST], BF16)
    b_sgo = Buf()
    ktok = T("ktok", [64, CPS, 512], BF16)
    vtok = T("vtok", [64, CPS, 1024], BF16)
    b_kv = Buf()
    bkv = T("bkv", [128, 1536], F32)
    ymixT = T("ymixT", [128, 16, ST], BF16)
    b_ym = Buf()
    ycv = T("ycv", [128, 8, ST], F32)
    b_ycv = Buf()
    diag = T("diag", [128, 31, 128], BF16)
    b_diag = Buf()
    ybq = T("ybq", [128, 2 * ST], BF16)
    b_yb = Buf()
    mean = T("mean", [128, ST], F32)
    rstc = T("rstc", [128, ST], F32)
    msq = T("msq", [128, ST], F32)
    b_cst = Buf()
    ctmp = T("ctmp", [128, ST], F32)
    b_ctmp = Buf()
    gsb = T("gsb", [8, ST], F32)
    expg = T("expg", [8, ST], F32)
    lfn = T("lfn", [8, ST], F32)
    bneg = T("bneg", [8, ST], F32)
    A_sb = T("A_sb", [4, ST], F32)
    G_sb = T("G_sb", [4, ST], F32)
    E_sb = T("E_sb", [4, ST], F32)
    b_gt = Buf()
    bcar = T("bcar", [8, 1], F32)
    gcar = T("gcar", [4, 1], F32)
    G_bc = T("G_bc", [128, 4, ST], F32)
    gprev = T("gprev", [128, 4], F32)
    b_gbc = Buf()
    colsc = T("colsc", [64, CPS, 8], F32)
    b_cols = Buf()
    Cst = T("Cst", [128, 4, 256], F32)
    nst = T("nst", [128, 4], F32)
    Cbf = T("Cbf", [128, 4, 256], BF16)
    nbf = T("nbf", [128, 4], BF16)
    b_C = Buf()
    b_Cbf = Buf()
    DTt = [T("DT%d" % i, [64, 64], F32) for i in range(2)]
    Dm = [T("Dm%d" % i, [64, 64], F32) for i in range(2)]
    b_DT = [Buf() for _ in range(2)]
    b_Dm = [Buf() for _ in range(2)]
    sT = T("sT", [64, 4, 64], BF16)
    b_sT = [Buf() for _ in range(4)]
    wi = [T("wi%d" % i, [128, 64], F32) for i in range(2)]
    b_wi = [Buf() for _ in range(2)]
    qs = T("qs", [128, 4, 64], BF16)
    b_qs = [Buf() for _ in range(4)]
    kw = T("kw", [64, 4, 128], BF16)
    b_kw = [Buf() for _ in range(4)]
    wkc = T("wkc", [64, 4], F32)
    dec = T("dec", [128, 4], F32)
    b_sm = Buf()
    dabs = T("dabs", [64, 4], F32)
    dd = T("dd", [64, 4], F32)
    rr = T("rr", [64, 4], F32)
    ssq = T("ssq", [64, 4], F32)
    tcol = T("tcol", [64, 4], F32)
    fcol = T("fcol", [64, 4], F32)
    b_nrm = Buf()
    sqt = T("sqt", [64, 256], F32)
    b_sqt = Buf()
    hn = T("hn", [64, CPS, 4, 256], BF16)
    b_hn = Buf()
    tmpw = T("tmpw", [128, 512], F32)
    b_tmpw = Buf()
    x1t = ycv[:].rearrange("p c n -> p (c n)")
    b_x1t = b_ycv
    h2b = xn[:]
    b_h2b = b_xn
    ln1g = hT[:].bitcast(F32).rearrange("p k n -> p (k n)")
    ln1b = ymixT[:].bitcast(F32).rearrange("p k n -> p (k n)")
    b_dx = Buf()
    b_dh = Buf()

    DMA("sp", bkv[:], b_kv_bc[:], [], [b_c])
    S.op("pool", lambda h: h.memset(uT[:, :, 0:30], 0.0), [], [b_uT])
    S.op("pool", lambda h: h.memset(Cst[:], 0.0), [], [b_C])
    S.op("pool", lambda h: h.memset(nst[:], 0.0), [], [b_C])
    S.op("pool", lambda h: h.memset(Cbf[:], 0.0), [], [b_Cbf])
    S.op("pool", lambda h: h.memset(nbf[:], 0.0), [], [b_Cbf])
    S.op("pool", lambda h: h.memset(bcar[:], 0.0), [], [b_gt])
    S.op("pool", lambda h: h.memset(gcar[:], 0.0), [], [b_gt])
    S.op("pool", lambda h: h.memset(gprev[:], 0.0), [], [b_gbc])
    S.barrier()

    w_in_v = w_in.rearrange("(k p) n -> p k n", p=128)
    w_out_v = w_out.rearrange("(k p) n -> p k n", p=128)
    wcnt = [0]

    def load_w(src):
        i = wcnt[0] % 2
        wcnt[0] += 1
        DMA("pool", wgb[i][:], src, [], [b_wg[i]])
        return wgb[i], b_wg[i]

    def ln_stats(src, rd):
        for c4 in range(4):
            S.op("dve", (lambda c4: lambda h: h.bn_stats(st6[:, c4 * 6:(c4 + 1) * 6], src[:, c4 * 512:(c4 + 1) * 512]))(c4),
                 rd, [b_stat])
        S.op("dve", lambda h: h.bn_aggr(mv[:], st6[:]), [b_stat], [b_stat])
        ACT(rstd[:], mv[:, 1:2], AF.Sqrt, [b_stat], [b_stat], bias=EPS)
        S.op("dve", lambda h: h.reciprocal(rstd[:], rstd[:]), [b_stat], [b_stat])
        STT("dve", nmr[:], mv[:, 0:1], -1.0, rstd[:], ALU.mult, ALU.mult, [b_stat], [b_stat])

    for st in range(NST):
        t0 = st * ST
        for i in range(TPS):
            DMA("sp", xall[:, i, :], x[t0 + i * 128: t0 + (i + 1) * 128, :], [], [b_x[i]])
            ln_stats(xall[:, i, :], [b_x[i]])
            if B1STOP == 1:
                continue
            ACT(xn[:], xall[:, i, :], AF.Identity, [b_x[i], b_stat], [b_xn], bias=nmr[:], scale=rstd[:])
            if B1STOP == 2:
                continue
            for kg in range(4):
                hf = kg % 2
                for kk in range(4):
                    k = kg * 4 + kk
                    TR(PT[hf][:, kk * 128:(kk + 1) * 128], xn[:, k * 128:(k + 1) * 128], ident_b,
                       [b_xn, b_c], [PTb[hf]])
                for kk in range(4):
                    if B1STOP == 3:
                        continue
                    k = kg * 4 + kk
                    if EVAC_DVE:
                        TS("dve", hT[:, k, i * 128:(i + 1) * 128], PT[hf][:, kk * 128:(kk + 1) * 128],
                           sc1p[:, k:k + 1], ALU.mult, [PTb[hf], b_c], [b_hT], s2=sh1[:, k:k + 1], op1=ALU.add)
                    else:
                        ACT(hT[:, k, i * 128:(i + 1) * 128], PT[hf][:, kk * 128:(kk + 1) * 128],
                            AF.Identity, [PTb[hf], b_c], [b_hT], bias=sh1[:, k:k + 1], scale=sc1p[:, k:k + 1])
        if dbg and st == 0:
            DMA("sp", L["d_hT"][:], hT[:].rearrange("p k n -> p (k n)"), [b_hT], [Buf()])

        if MIXSTOP == 1:
            break
        def fm_chunk(wt, bw, c, evac):
            bank, bb = nextbank()
            for k in range(16):
                MM(bank[:, 0:ST], wt[:, k, c * 128:(c + 1) * 128], hT[:, k, :], k == 0, k == 15, [bw, b_hT], [bb])
            evac(bank[:, 0:ST], bb)

        wt, bw = load_w(w_in_v[:, :, 2048:2560])
        for c in range(4):
            fm_chunk(wt, bw, c, (lambda c: lambda ps, bb: ACT(qT[:, c, :], ps, AF.Identity, [bb, b_c], [b_qk],
                                                            bias=bqs[:, c:c + 1], scale=QS))(c))
        wt, bw = load_w(w_in_v[:, :, 2560:3072])
        for c in range(4):
            fm_chunk(wt, bw, c, (lambda c: lambda ps, bb: ACT(kT[:, c, :], ps, AF.Identity, [bb, b_c], [b_qk],
                                                            bias=bcol[:, 20 + c: 21 + c]))(c))
        for ch in range(CPS):
            bank, bb = nextbank()
            for k in range(16):
                MM(bank[0:64, :], hT[:, k, ch * 64:(ch + 1) * 64], wt[:, k, :], k == 0, k == 15, [bw, b_hT], [bb])
            TT("dve", ktok[:, ch, :], bank[0:64, :], bkv[0:64, 0:512], ALU.add, [bb, b_c], [b_kv])
        for g in range(2):
            wt, bw = load_w(w_in_v[:, :, 3072 + g * 512: 3072 + (g + 1) * 512])
            for ch in range(CPS):
                bank, bb = nextbank()
                for k in range(16):
                    MM(bank[0:64, :], hT[:, k, ch * 64:(ch + 1) * 64], wt[:, k, :], k == 0, k == 15, [bw, b_hT], [bb])
                TT("dve", vtok[:, ch, g * 512:(g + 1) * 512], bank[0:64, :], bkv[0:64, 512 + g * 512: 1024 + g * 512],
                   ALU.add, [bb, b_c], [b_kv])
        DMA("pool", wgt[:], w_in_v[:, :, 5120:5128], [], [b_wgt])
        for k in range(16):
            MM(PB[3][0:8, 0:ST], wgt[:, k, :], hT[:, k, :], k == 0, k == 15, [b_wgt, b_hT], [PBb[3]])
        ACT(gsb[:], PB[3][0:8, 0:ST], AF.Identity, [PBb[3], b_c], [b_gt], bias=bcol[0:8, 40:41])

        def streamX():
            def fm_chunk_x(wt, bw, c, evac):
                bank, bb = xbank()
                for k in range(16):
                    MM(bank[:, 0:ST], wt[:, k, c * 128:(c + 1) * 128], hT[:, k, :], k == 0, k == 15, [bw, b_hT], [bb])
                evac(bank[:, 0:ST], bb)
            for g in range(2):
                wt, bw = load_w(w_in_v[:, :, 4096 + g * 512: 4096 + (g + 1) * 512])
                for c in range(4):
                    yield
                    cc = g * 4 + c
                    fm_chunk_x(wt, bw, c, (lambda cc: lambda ps, bb: ACT(sgo[:, cc, :], ps, AF.Sigmoid, [bb, b_c], [b_sgo],
                                                                     bias=bcol[:, 32 + cc: 33 + cc]))(cc))
            for g in range(2):
                wt, bw = load_w(w_in_v[:, :, 1024 + g * 512: 1024 + (g + 1) * 512])
                for c in range(4):
                    yield
                    cc = g * 4 + c
                    fm_chunk_x(wt, bw, c, (lambda cc: lambda ps, bb: ACT(sg[:, cc, :], ps, AF.Sigmoid, [bb, b_c], [b_sg],
                                                                     bias=bcol[:, 8 + cc: 9 + cc]))(cc))
            for g in range(2):
                wt, bw = load_w(w_in_v[:, :, g * 512:(g + 1) * 512])
                for c in range(4):
                    yield
                    cc = g * 4 + c
                    fm_chunk_x(wt, bw, c, (lambda cc: lambda ps, bb: STT("dve", uT[:, cc, 30:30 + ST], ps, bcol[:, cc:cc + 1],
                                                                     sg[:, cc, :], ALU.add, ALU.mult, [bb, b_sg, b_c],
                                                                     [b_uT]))(cc))
            MM_sum, bsum = PB[5], PBb[5]
            MM_sq, bsq = PB[5], PBb[5]
            for c in range(8):
                yield
                for j in range(31):
                    ACT(diag[:, j, :], ident_b, AF.Identity, [b_c], [b_diag], scale=dwc[:, c * 31 + j: c * 31 + j + 1])
                bank, bb = xbank()
                for j in range(31):
                    MM(bank[:, 0:ST], diag[:, j, :], uT[:, c, j:j + ST], j == 0, j == 30, [b_diag, b_uT], [bb])
                ACT(ycv[:, c, :], bank[:, 0:ST], AF.Identity, [bb, b_c], [b_ycv], bias=cvc[:, c:c + 1])
                ACT(ybq[:, 0:ST], bank[:, 0:ST], AF.Identity, [bb, b_c], [b_yb], bias=cvc[:, c:c + 1])
                ACT(ybq[:, ST:2 * ST], bank[:, 0:ST], AF.Square, [bb, b_c], [b_yb], bias=cvc[:, c:c + 1])
                MM(MM_sum[:, 0:2 * ST], cb_t[:, B_ONE:B_ONE + 128], ybq[:], c == 0, c == 7, [b_yb, b_c], [bsum])
            CP("pool", uT[:, :, 0:30], uT[:, :, ST:ST + 30], [b_uT], [b_uT])
            TS("dve", mean[:], MM_sum[:, 0:ST], 1.0 / 1024, ALU.mult, [bsum], [b_cst])
            TT("dve", msq[:], mean[:], mean[:], ALU.mult, [b_cst], [b_cst])
            STT("dve", rstc[:], MM_sq[:, ST:2 * ST], 1.0 / 1024, msq[:], ALU.mult, ALU.subtract, [bsq, b_cst], [b_cst])
            ACT(rstc[:], rstc[:], AF.Sqrt, [b_cst], [b_cst], bias=EPS)
            S.op("dve", lambda h: h.reciprocal(rstc[:], rstc[:]), [b_cst], [b_cst])
            for c in range(8):
                yield
                TT("dve", ctmp[:], ycv[:, c, :], mean[:], ALU.subtract, [b_ycv, b_cst], [b_ctmp])
                TT("dve", ctmp[:], ctmp[:], rstc[:], ALU.mult, [b_ctmp, b_cst], [b_ctmp])
                ACT(ymixT[:, c, :], ctmp[:], AF.Silu, [b_ctmp, b_c], [b_ym], bias=cvc[:, 16 + c: 17 + c],
                    scale=cvc[:, 8 + c: 9 + c])

        def streamY():
            ACT(expg[:], gsb[:], AF.Exp, [b_gt], [b_gt], scale=-1.0)
            ACT(lfn[:], expg[:], AF.Ln, [b_gt], [b_gt], bias=1.0)
            S.op("dve", lambda h: h.tensor_tensor_scan(out=bneg[:], data0=cf_t[0:8, C_ONE:C_ONE + ST], data1=lfn[:],
                                                      initial=bcar[:, 0:1], op0=ALU.mult, op1=ALU.add), [b_gt, b_c], [b_gt])
            CP("dve", bcar[:], bneg[:, ST - 1:ST], [b_gt], [b_gt])
            MM(PB[4][0:4, 0:ST], cf_t[0:8, C_SELI:C_SELI + 4], gsb[:], True, False, [b_gt, b_c], [PBb[4]])
            MM(PB[4][0:4, 0:ST], cf_t[0:8, C_SELF:C_SELF + 4], bneg[:], False, True, [b_gt, b_c], [PBb[4]])
            CP("dve", A_sb[:], PB[4][0:4, 0:ST], [PBb[4]], [b_gt])
            S.op("dve", lambda h: h.tensor_tensor_scan(out=G_sb[:], data0=cf_t[0:4, C_ONE:C_ONE + ST], data1=A_sb[:],
                                                      initial=gcar[:, 0:1], op0=ALU.mult, op1=ALU.max), [b_gt, b_c], [b_gt])
            CP("dve", gcar[:], G_sb[:, ST - 1:ST], [b_gt], [b_gt])
            MM(PB[4][0:4, 0:ST], cf_t[0:8, C_SELF:C_SELF + 4], bneg[:], True, False, [b_gt, b_c], [PBb[4]])
            MM(PB[4][0:4, 0:ST], cf_t[0:4, C_NEGI:C_NEGI + 4], G_sb[:], False, True, [b_gt, b_c], [PBb[4]])
            ACT(E_sb[:], PB[4][0:4, 0:ST], AF.Exp, [PBb[4]], [b_gt])
            if st > 0:
                CP("dve", gprev[:], G_bc[:, :, ST - 1], [b_gbc], [b_gbc])
            for h_ in range(4):
                yield
                MM(PB[3][:, 0:ST], cf_t[0:4, C_SELB + h_ * 128: C_SELB + (h_ + 1) * 128], G_sb[:], True, True,
                   [b_gt, b_c], [PBb[3]])
                CP("dve", G_bc[:, h_, :], PB[3][:, 0:ST], [PBb[3]], [b_gbc])
            for ch in range(CPS):
                S.op("pe", (lambda ch: lambda h: h.transpose(PB[4][0:64, 0:4], A_sb[0:4, ch * 64:(ch + 1) * 64],
                                                             cf_t[0:4, C_ID:C_ID + 4]))(ch), [b_gt, b_c], [PBb[4]])
                S.op("pe", (lambda ch: lambda h: h.transpose(PB[4][0:64, 4:8], E_sb[0:4, ch * 64:(ch + 1) * 64],
                                                             cf_t[0:4, C_ID:C_ID + 4]))(ch), [b_gt, b_c], [PBb[4]])
                CP("dve", colsc[:, ch, :], PB[4][0:64, 0:8], [PBb[4]], [b_cols])

            for ch in range(CPS):
                cs = slice(ch * 64, (ch + 1) * 64)
                last = ch * 64 + 63
                for h_ in range(4):
                    yield
                    gp = G_bc[:, h_, ch * 64 - 1: ch * 64] if ch > 0 else gprev[:, h_:h_ + 1]
                    i2 = h_ % 2
                    MM(PB[3][0:64, 16 + h_ * 64: 16 + (h_ + 1) * 64], kT[:, h_, cs], qT[:, h_, cs], True, True, [b_qk], [PBb[3]])
                    ACT(DTt[i2][:], G_bc[0:64, h_, cs], AF.Exp, [b_gbc, b_cols], [b_DT[i2]], bias=colsc[:, ch, h_:h_ + 1],
                        scale=-1.0)
                    TT("dve", Dm[i2][:], DTt[i2][:], cf_t[0:64, C_MASK:C_MASK + 64], ALU.mult, [b_DT[i2], b_c], [b_Dm[i2]])
                    TT("dve", sT[:, h_, :], PB[3][0:64, 16 + h_ * 64: 16 + (h_ + 1) * 64], Dm[i2][:], ALU.mult, [PBb[3], b_Dm[i2]],
                       [b_sT[h_]])
                    ACT(wi[i2][:], G_bc[:, h_, cs], AF.Exp, [b_gbc], [b_wi[i2]], bias=gp, scale=-1.0)
                    TT("dve", qs[:, h_, :], qT[:, h_, cs], wi[i2][:], ALU.mult, [b_qk, b_wi[i2]], [b_qs[h_]])
                    ACT(wkc[:, h_:h_ + 1], G_bc[0:64, h_, last:last + 1], AF.Exp, [b_gbc, b_cols], [b_sm],
                        bias=colsc[:, ch, h_:h_ + 1], scale=-1.0)
                    ACT(dec[:, h_:h_ + 1], G_bc[:, h_, last:last + 1], AF.Exp, [b_gbc], [b_sm], bias=gp, scale=-1.0)
                    TS("dve", kw[:, h_, :], ktok[:, ch, h_ * 128:(h_ + 1) * 128], wkc[:, h_:h_ + 1], ALU.mult,
                       [b_kv, b_sm], [b_kw[h_]])
                nbanks = []
                for pr in range(2):
                    yield
                    bank, bb = ybank()
                    nbanks.append((bank, bb))
                    for hh in range(2):
                        h_ = pr * 2 + hh
                        MM(bank[0:64, hh * 256:(hh + 1) * 256], sT[:, h_, :], vtok[:, ch, h_ * 256:(h_ + 1) * 256], True, False,
                           [b_sT[h_], b_kv], [bb])
                        MM(bank[0:64, hh * 256:(hh + 1) * 256], qs[:, h_, :], Cbf[:, h_, :], False, True, [b_qs[h_], b_Cbf], [bb])
                for h_ in range(4):
                    yield
                    MM(PB[3][0:64, h_:h_ + 1], sT[:, h_, :], cb_t[0:64, B_ONE:B_ONE + 1], True, False, [b_sT[h_], b_c], [PBb[3]])
                    MM(PB[3][0:64, h_:h_ + 1], qs[:, h_, :], nbf[:, h_:h_ + 1], False, True, [b_qs[h_], b_Cbf], [PBb[3]])
                CP("dve", dd[:], PB[3][0:64, 0:4], [PBb[3]], [b_nrm])
                STT("dve", dabs[:], dd[:], -1.0, dd[:], ALU.mult, ALU.max, [b_nrm], [b_nrm])
                TT("dve", dd[:], dabs[:], colsc[:, ch, 4:8], ALU.max, [b_nrm, b_cols], [b_nrm])
                S.op("dve", lambda h: h.reciprocal(rr[:], dd[:]), [b_nrm], [b_nrm])
                for h_ in range(4):
                    yield
                    bank, bb = nbanks[h_ // 2]
                    hh = h_ % 2
                    ACT(sqt[:], bank[0:64, hh * 256:(hh + 1) * 256], AF.Square, [bb], [b_sqt])
                    RED(ssq[:, h_:h_ + 1], sqt[:], ALU.add, [b_sqt], [b_nrm])
                TT("dve", tcol[:], rr[:], rr[:], ALU.mult, [b_nrm], [b_nrm])
                TT("dve", tcol[:], tcol[:], ssq[:], ALU.mult, [b_nrm], [b_nrm])
                TS("dve", tcol[:], tcol[:], 1.0 / 256, ALU.mult, [b_nrm], [b_nrm], s2=EPS, op1=ALU.add)
                ACT(tcol[:], tcol[:], AF.Sqrt, [b_nrm], [b_nrm])
                S.op("dve", lambda h: h.reciprocal(tcol[:], tcol[:]), [b_nrm], [b_nrm])
                TT("dve", fcol[:], rr[:], tcol[:], ALU.mult, [b_nrm], [b_nrm])
                for h_ in range(4):
                    yield
                    bank, bb = nbanks[h_ // 2]
                    hh = h_ % 2
                    ACT(hn[:, ch, h_, :], bank[0:64, hh * 256:(hh + 1) * 256], AF.Identity, [bb, b_nrm], [b_hn],
                        scale=fcol[:, h_:h_ + 1])
                for pr in range(2):
                    yield
                    bank, bb = ybank()
                    for hh in range(2):
                        h_ = pr * 2 + hh
                        MM(bank[:, hh * 256:(hh + 1) * 256], kw[:, h_, :], vtok[:, ch, h_ * 256:(h_ + 1) * 256], True, True,
                           [b_kw[h_], b_kv], [bb])
                        MM(PB[3][:, 8 + h_: 9 + h_], kw[:, h_, :], cb_t[0:64, B_ONE:B_ONE + 1], True, True, [b_kw[h_], b_c],
                           [PBb[3]])
                    for hh in range(2):
                        h_ = pr * 2 + hh
                        STT("dve", Cst[:, h_, :], Cst[:, h_, :], dec[:, h_:h_ + 1], bank[:, hh * 256:(hh + 1) * 256],
                            ALU.mult, ALU.add, [bb, b_sm, b_C], [b_C])
                        STT("dve", nst[:, h_:h_ + 1], nst[:, h_:h_ + 1], dec[:, h_:h_ + 1], PB[3][:, 8 + h_: 9 + h_],
                            ALU.mult, ALU.add, [PBb[3], b_sm, b_C], [b_C])
                CP("act", Cbf[:].rearrange("p h v -> p (h v)"), Cst[:].rearrange("p h v -> p (h v)"), [b_C], [b_Cbf])
                CP("act", nbf[:], nst[:], [b_C], [b_Cbf])
                hf = ch % 2
                for idx in range(8):
                    h_, half = idx // 2, idx % 2
                    TR(PT[hf][:, idx * 64:(idx + 1) * 64], hn[:, ch, h_, half * 128:(half + 1) * 128],
                       cb_t[0:64, B_ID:B_ID + 64], [b_hn, b_c], [PTb[hf]])
                for idx in range(8):
                    STT("dve", ymixT[:, 8 + idx, cs], PT[hf][:, idx * 64:(idx + 1) * 64],
                        cvc[:, 24 + idx: 25 + idx], sgo[:, idx, cs], ALU.mult, ALU.mult, [PTb[hf], b_sgo, b_c], [b_ym])
        gx, gy = streamX(), streamY()
        alive = [gx, gy]
        while alive:
            for g_ in list(alive):
                try:
                    next(g_)
                except StopIteration:
                    alive.remove(g_)

        if dbg and st == 0:
            DMA("sp", L["d_ymix"][:], ymixT[:].rearrange("p k n -> p (k n)"), [b_ym], [Buf()])

        if MIXSTOP == 5:
            break
        for pc in range(4):
            wt, bw = load_w(w_out_v[:, :, pc * 512:(pc + 1) * 512])
            for i in range(TPS):
                bank, bb = nextbank()
                for k in range(16):
                    MM(bank[:, :], ymixT[:, k, i * 128:(i + 1) * 128], wt[:, k, :], k == 0, k == 15, [bw, b_ym], [bb])
                TT("dve", tmpw[:], bank[:, :], g1_bc[:, pc * 512:(pc + 1) * 512], ALU.mult, [bb, b_rows], [b_tmpw])
                STT("dve", xall[:, i, pc * 512:(pc + 1) * 512], xall[:, i, pc * 512:(pc + 1) * 512], ALPHA, tmpw[:],
                    ALU.mult, ALU.add, [b_tmpw, b_x[i]], [b_x[i]])
        if MIXSTOP == 6:
            break
        DMA("sp", ln1g, ln_bc[:, 0:D], [], [b_hT])
        DMA("sp", ln1b, ln_bc[:, D:2 * D], [], [b_ym])
        for i in range(TPS):
            rows = slice(t0 + i * 128, t0 + (i + 1) * 128)
            ln_stats(xall[:, i, :], [b_x[i]])
            ACT(x1t, xall[:, i, :], AF.Identity, [b_x[i], b_stat], [b_x1t], bias=nmr[:], scale=rstd[:])
            TT("dve", x1t, x1t, ln1g, ALU.mult, [b_x1t, b_hT], [b_x1t])
            TT("dve", x1t, x1t, ln1b, ALU.add, [b_x1t, b_ym], [b_x1t])
            DMA("sp", x1_d[rows, :], x1t, [b_x1t], [b_dx])
            ln_stats(x1t, [b_x1t])
            ACT(xall[:, i, :], x1t, AF.Identity, [b_x1t, b_stat], [b_x[i]], bias=nmr[:], scale=rstd[:])
            TT("dve", xall[:, i, :], xall[:, i, :], sc2p_bc[:], ALU.mult, [b_x[i], b_rows], [b_x[i]])
            TT("dve", h2b, xall[:, i, :], sh2_bc[:], ALU.add, [b_x[i], b_rows], [b_h2b])
            DMA("sp", h2_d[rows, :], h2b, [b_h2b], [b_dh])
    S.barrier()
    sm.close()


def build_moe(nc, S, L):
    (MM, TR, ACT, TT, TS, STT, CP, DMA, RED, sb, PB, PBb, PT, PTb) = (
        L[k] for k in ("MM", "TR", "ACT", "TT", "TS", "STT", "CP", "DMA", "RED", "sb", "PB", "PBb", "PT", "PTb"))
    cf_t, cb_t, b_c, g2_bc, b_g2 = (L[k] for k in ("cf_t", "cb_t", "b_c", "g2_bc", "b_g2"))
    x1_d, h2_d, Xs, Ys, out, ln_bc = (L[k] for k in ("x1_d", "h2_d", "Xs", "Ys", "out", "ln_bc"))
    router_w, rb_bc, wg_l, wu_l, wd_l, bgu, b_down = (L[k] for k in ("router_w", "rb_bc", "wg_l", "wu_l", "wd_l", "bgu",
                                                                      "b_down"))
    dbg = L["dbg"]
    phases = L["phases"]
    ident_b = cb_t[:, B_ID:B_ID + 128]
    sp_ = ExitStack()

    def P(name, shape, dt):
        return sb(name, shape, dt, sp_)

    dest_i = P("dest_i", [128, NT * 4], I32)
    wcol = P("wcol", [128, NT * 4], F32)
    WtT = P("WtT", [32, S_TOK], BF16)
    idxW_i = P("idxW_i", [128, NBLK * 4], I32)
    ej_i = P("ej_i", [128, NBLK], I32)
    bd_bf = P("bd_bf", [32, D], BF16)
    b_rt = Buf("routing")
    DMA("pool", bd_bf[:], b_down[:, :], [], [b_rt])

    with ExitStack() as sc:
        def T(name, shape, dt):
            return sb(name, shape, dt, sc)

        h2all = T("h2all", [128, NT, D], BF16)
        b_h2 = [Buf() for _ in range(NT)]
        rwb = T("rwb", [128, 16, NE], BF16)
        rbb = T("rbb", [128, NE], F32)
        h2T = [T("h2T%d" % i, [128, 16, 128], BF16) for i in range(2)]
        b_h2T = [Buf() for _ in range(2)]
        logit_all = T("logit_all", [128, NT, NE], F32)
        top8_all = T("top8_all", [128, NT, 8], F32)
        Wt_all = T("Wt_all", [128, NT, NE], F32)
        pos_all = T("pos_all", [128, NT, NE], F32)
        b_la = Buf()
        maskf = T("maskf", [128, NE], F32)
        mask_bf = T("mask_bf", [128, NE], BF16)
        nmx = T("nmx", [128, 1], F32)
        ex = T("ex", [128, NE], F32)
        rs = T("rs", [128, 1], F32)
        b_tl = Buf()
        cnt = T("cnt", [128, NE], F32)
        b_cnt = Buf()
        zero1 = T("zero1", [128, 1], F32)
        yv = T("yv", [128, NE], F32)
        fr = T("fr", [128, NE], F32)
        nb = T("nb", [128, NE], F32)
        bend = T("bend", [128, NE], F32)
        base = T("base", [128, NE], F32)
        destf = T("destf", [128, NE], F32)
        oh = T("oh", [128, NE], F32)
        prod = T("prod", [128, NE], F32)
        dcol = T("dcol", [128, NT * 4], F32)
        ej = T("ej", [128, NBLK], F32)
        t1 = T("t1", [128, NBLK], F32)
        idxf = T("idxf", [128, NBLK * 4], F32)
        b_g = Buf()

        rw_v = router_w.rearrange("(k p) e -> p k e", p=128)
        DMA("pool", rwb[:], rw_v, [], [b_rt])
        DMA("sp", rbb[:], rb_bc[:], [], [b_rt])
        S.op("pool", lambda h: h.memset(cnt[:], 0.0), [], [b_cnt])
        S.op("pool", lambda h: h.memset(zero1[:], 0.0), [], [b_cnt])
        zt = T("zt", [128, 4, D], BF16)
        S.op("pool", lambda h: h.memset(zt[:], 0.0), [], [b_cnt])
        for t in range(NT):
            DMA("sp", h2all[:, t, :], h2_d[t * 128:(t + 1) * 128, :], [], [b_h2[t]])
        for t in range(NT):
            j = t % 2
            for kg in range(4):
                hf = kg % 2
                for kk in range(4):
                    k = kg * 4 + kk
                    TR(PT[hf][:, kk * 128:(kk + 1) * 128], h2all[:, t, k * 128:(k + 1) * 128], ident_b,
                       [b_h2[t], b_c], [PTb[hf]])
                CP("act" if kg % 2 else "dve", h2T[j][:, kg * 4:(kg + 1) * 4, :].rearrange("p k n -> p (k n)"),
                   PT[hf][:, 0:512], [PTb[hf]], [b_h2T[j]])
            for k in range(16):
                MM(PB[3][:, 0:NE], h2T[j][:, k, :], rwb[:, k, :], k == 0, k == 15, [b_h2T[j], b_rt], [PBb[3]])
            lg = logit_all[:, t, :]
            TT("dve", lg, PB[3][:, 0:NE], rbb[:], ALU.add, [PBb[3], b_rt], [b_la])
            S.op("dve", (lambda t: lambda h: h.max(out=top8_all[:, t, :], in_=logit_all[:, t, :]))(t), [b_la], [b_la])
            TS("dve", maskf[:], lg, top8_all[:, t, 3:4], ALU.is_ge, [b_la], [b_tl])
            TS("dve", nmx[:], top8_all[:, t, 0:1], -1.0, ALU.mult, [b_la], [b_tl])
            ACT(ex[:], lg, AF.Exp, [b_la, b_tl], [b_tl], bias=nmx[:])
            TT("dve", ex[:], ex[:], maskf[:], ALU.mult, [b_tl], [b_tl])
            RED(rs[:], ex[:], ALU.add, [b_tl], [b_tl])
            S.op("dve", lambda h: h.reciprocal(rs[:], rs[:]), [b_tl], [b_tl])
            TS("dve", Wt_all[:, t, :], ex[:], rs[:], ALU.mult, [b_tl], [b_la])
            CP("dve", mask_bf[:], maskf[:], [b_tl], [b_tl])
            MM(PB[4][:, 0:NE], cb_t[:, B_TRI:B_TRI + 128], mask_bf[:], True, True, [b_tl, b_c], [PBb[4]])
            MM(PB[4][:, NE:2 * NE], cb_t[:, B_ONE:B_ONE + 128], mask_bf[:], True, True, [b_tl, b_c], [PBb[4]])
            TT("dve", pos_all[:, t, :], PB[4][:, 0:NE], cnt[:], ALU.add, [PBb[4], b_cnt], [b_la])
            TT("dve", cnt[:], cnt[:], PB[4][:, NE:2 * NE], ALU.add, [PBb[4], b_cnt], [b_cnt])
            TR(PB[5][0:32, 0:128], Wt_all[:, t, :], cf_t[:, C_ID:C_ID + 128], [b_la, b_c], [PBb[5]])
            CP("act", WtT[:, t * 128:(t + 1) * 128], PB[5][0:32, 0:128], [PBb[5]], [b_rt])
        TS("dve", nb[:], cnt[:], 0.0, ALU.is_gt, [b_cnt], [b_g])
        for m_ in range(1, 8):
            TS("dve", fr[:], cnt[:], float(m_ * BLK), ALU.is_gt, [b_cnt, b_g], [b_g])
            TT("dve", nb[:], nb[:], fr[:], ALU.add, [b_g], [b_g])
        S.op("dve", lambda h: h.tensor_tensor_scan(out=bend[:], data0=cf_t[:, C_ONE:C_ONE + NE], data1=nb[:],
                                                  initial=zero1[:, 0:1], op0=ALU.mult, op1=ALU.add), [b_g, b_c, b_cnt], [b_g])
        TT("dve", base[:], bend[:], nb[:], ALU.subtract, [b_g], [b_g])
        TS("dve", base[:], base[:], 512.0, ALU.mult, [b_g], [b_g])
        for t in range(NT):
            TT("dve", destf[:], pos_all[:, t, :], base[:], ALU.add, [b_la, b_g], [b_g])
            for k in range(4):
                TS("dve", oh[:], logit_all[:, t, :], top8_all[:, t, k:k + 1], ALU.is_equal, [b_la, b_g], [b_g])
                TT("dve", prod[:], oh[:], destf[:], ALU.mult, [b_g], [b_g])
                RED(dcol[:, t * 4 + k: t * 4 + k + 1], prod[:], ALU.add, [b_g], [b_g])
                TT("dve", prod[:], oh[:], Wt_all[:, t, :], ALU.mult, [b_g, b_la], [b_g])
                RED(wcol[:, t * 4 + k: t * 4 + k + 1], prod[:], ALU.add, [b_g], [b_rt])
        CP("dve", dest_i[:], dcol[:], [b_g], [b_rt])
        for j in range(NBLK):
            TS("dve", oh[:], bend[:], float(j), ALU.is_le, [b_g], [b_g])
            RED(ej[:, j:j + 1], oh[:], ALU.add, [b_g], [b_g])
        TS("dve", ej[:], ej[:], float(NE - 1), ALU.min, [b_g], [b_g])
        CP("dve", ej_i[:], ej[:], [b_g], [b_rt])
        TS("dve", t1[:], ej[:], 512.0, ALU.mult, [b_g, b_c], [b_g], s2=cf_t[:, C_IOTA:C_IOTA + 1], op1=ALU.add)
        idxv = idxf[:].rearrange("p (j q) -> p j q", q=4)
        for q in range(4):
            TS("dve", idxv[:, :, q], t1[:], float(q * 128), ALU.add, [b_g], [b_g])
        CP("dve", idxW_i[:], idxf[:], [b_g], [b_rt])
        if dbg:
            dr = L["d_rt"]
            DMA("sp", dr[:, 0:NT * 32], logit_all[:].rearrange("p t e -> p (t e)"), [b_la], [Buf()])
            DMA("sp", dr[:, NT * 32:NT * 36], dcol[:], [b_g], [Buf()])
            DMA("sp", dr[:, NT * 36:NT * 40], wcol[:], [b_rt], [Buf()])
        b_xs = Buf("Xs")
        for j in range(NBLK):
            DMA("sp", Xs[j * BLK:(j + 1) * BLK, :].rearrange("(i p) d -> p i d", p=128), zt[:], [b_cnt], [b_xs])
        for t in range(NT):
            for k in range(4):
                S.dma("pool", (lambda t, k: lambda h: h.indirect_dma_start(
                    out=Xs[:, :], out_offset=bass.IndirectOffsetOnAxis(ap=dest_i[:, t * 4 + k: t * 4 + k + 1], axis=0),
                    in_=h2all[:, t, :], in_offset=None))(t, k), [b_rt, b_h2[t]], [b_xs])
        S.barrier()

    b_ys = Buf("Ys")
    if "D" in phases:
        with ExitStack() as sd:
            def T(name, shape, dt):
                return sb(name, shape, dt, sd)

            Xb = T("Xb", [128, 4, D], BF16)
            b_Xb = Buf()
            XbT = [T("XbT%d" % i, [128, 16, BLK], BF16) for i in range(2)]
            b_XbT = [Buf() for _ in range(2)]
            NWQ = 4
            wq = [T("wq%d" % i, [128, 8192], BF16) for i in range(NWQ)]
            b_wq = [Buf() for _ in range(NWQ)]
            actT = T("actT", [128, 16, BLK], BF16)
            b_act = Buf()
            ystage = [T("ystage%d" % i, [128, 4, 512], F32) for i in range(2)]
            b_yst = [Buf() for _ in range(2)]
            bgub = [T("bgub%d" % i, [128, 2 * D], BF16) for i in range(2)]
            b_bgu = [Buf() for _ in range(2)]
            a_t = [T("a_t%d" % i, [128, BLK], F32) for i in range(2)]
            sg_t = [T("sg_t%d" % i, [128, BLK], F32) for i in range(2)]
            u_t = [T("u_t%d" % i, [128, BLK], F32) for i in range(2)]
            b_sw = [Buf() for _ in range(2)]
            swc = [0]

            pieces = []
            for j in range(NBLK):
                for q in range(4):
                    pieces.append((wg_l, j * 4 + q))
                    pieces.append((wu_l, j * 4 + q))
                for q in range(4):
                    pieces.append((wd_l, j * 4 + q))
            issued = [0]
            PRE = 3

            def issue_upto(n):
                while issued[0] <= min(n, len(pieces) - 1):
                    i = issued[0]
                    src, col = pieces[i]
                    S.dma("pool", (lambda i, src, col: lambda h: h.indirect_dma_start(
                        out=wq[i % NWQ][:, :], out_offset=None, in_=src[:, :],
                        in_offset=bass.IndirectOffsetOnAxis(ap=idxW_i[:, col:col + 1], axis=0)))(i, src, col),
                        [b_rt], [b_wq[i % NWQ]])
                    issued[0] += 1

            def piece(n):
                issue_upto(n + PRE)
                return wq[n % NWQ], b_wq[n % NWQ]

            def prep_block(j):
                xt = XbT[j % 2]
                bxt = b_XbT[j % 2]
                DMA("sp", Xb[:], Xs[j * BLK:(j + 1) * BLK, :].rearrange("(i p) d -> p i d", p=128), [b_xs], [b_Xb])
                S.dma("pool", (lambda j: lambda h: h.indirect_dma_start(
                    out=bgub[j % 2][:, :], out_offset=None, in_=bgu[:, :],
                    in_offset=bass.IndirectOffsetOnAxis(ap=ej_i[:, j:j + 1], axis=0)))(j), [b_rt], [b_bgu[j % 2]])
                for k in range(16):
                    hf = k % 2
                    for i in range(4):
                        TR(PT[hf][:, i * 128:(i + 1) * 128], Xb[:, i, k * 128:(k + 1) * 128], ident_b,
                           [b_Xb, b_c], [PTb[hf]])
                    CP("dve", xt[:, k, :], PT[hf][:, 0:512], [PTb[hf]], [bxt])

            gu_banks = [(PB[0], PBb[0], PB[1], PBb[1]), (PB[2], PBb[2], PB[3], PBb[3])]
            dn_banks = [(PB[4], PBb[4]), (PB[5], PBb[5])]
            guc = [0]
            dnc = [0]
            pn = 0
            prep_block(0)
            for j in range(NBLK):
                xt = XbT[j % 2]
                bxt = b_XbT[j % 2]
                bg_t, bbg = bgub[j % 2], b_bgu[j % 2]
                for q in range(4):
                    issue_upto(pn + 3)
                    wg, bwg = wq[pn % NWQ], b_wq[pn % NWQ]
                    wu, bwu = wq[(pn + 1) % NWQ], b_wq[(pn + 1) % NWQ]
                    pn += 2
                    for fc in range(4):
                        f0 = (q * 4 + fc) * 128
                        pg, bpg, pu, bpu = gu_banks[guc[0] % 2]
                        guc[0] += 1
                        MM(pg[:, :], bg_t[0:1, f0:f0 + 128], cb_t[0:1, B_ONE:B_ONE + 512], True, False, [bbg, b_c], [bpg])
                        for k in range(16):
                            MM(pg[:, :], wg[:, k * 512 + fc * 128: k * 512 + (fc + 1) * 128], xt[:, k, :], False, k == 15,
                               [bwg, bxt], [bpg])
                        MM(pu[:, :], bg_t[0:1, D + f0: D + f0 + 128], cb_t[0:1, B_ONE:B_ONE + 512], True, False,
                           [bbg, b_c], [bpu])
                        for k in range(16):
                            MM(pu[:, :], wu[:, k * 512 + fc * 128: k * 512 + (fc + 1) * 128], xt[:, k, :], False, k == 15,
                               [bwu, bxt], [bpu])
                        s_ = swc[0] % 2
                        swc[0] += 1
                        TS("dve", a_t[s_][:], pg[:, :], 7.0, ALU.min, [bpg], [b_sw[s_]])
                        ACT(sg_t[s_][:], a_t[s_][:], AF.Silu, [b_sw[s_]], [b_sw[s_]], scale=1.702)
                        TS("dve", u_t[s_][:], pu[:, :], 7.0, ALU.min, [bpu], [b_sw[s_]], s2=-7.0, op1=ALU.max)
                        STT("dve", actT[:, q * 4 + fc, :], u_t[s_][:], 1.0, sg_t[s_][:], ALU.add, ALU.mult, [b_sw[s_]],
                            [b_act])
                if j + 1 < NBLK:
                    prep_block(j + 1)
                for q in range(4):
                    wd, bwd = piece(pn)
                    pn += 1
                    for i in range(4):
                        pd, bpd = dn_banks[dnc[0] % 2]
                        dnc[0] += 1
                        for fc in range(16):
                            MM(pd[:, :], actT[:, fc, i * 128:(i + 1) * 128], wd[:, fc * 512:(fc + 1) * 512], fc == 0,
                               fc == 15, [b_act, bwd], [bpd])
                        TS("dve", ystage[q % 2][:, i, :], pd[:, :], 1.0 / 1.702, ALU.mult, [bpd], [b_yst[q % 2]])
                    DMA("sp", Ys[j * BLK:(j + 1) * BLK, q * 512:(q + 1) * 512].rearrange("(i p) d -> p i d", p=128),
                        ystage[q % 2][:], [b_yst[q % 2]], [b_ys])
            S.barrier()

    if "E" in phases:
        with ExitStack() as se:
            def T(name, shape, dt):
                return sb(name, shape, dt, se)

            yk = [T("yk%d" % i, [128, D], F32) for i in range(4)]
            b_yk = [Buf() for _ in range(4)]
            acc = T("acc", [128, D], F32)
            b_acc = Buf()
            x1t = T("x1e", [128, D], F32)
            b_x1 = Buf()
            outt = T("outt", [128, D], F32)
            b_ot = Buf()
            ln2g = T("ln2g", [128, D], F32)
            ln2b = T("ln2b", [128, D], F32)
            st6 = T("st6e", [128, 24], F32)
            mv = T("mve", [128, 2], F32)
            rstd = T("rstde", [128, 1], F32)
            nmr = T("nmre", [128, 1], F32)
            b_stat = Buf()
            b_out = Buf()
            DMA("sp", ln2g[:], ln_bc[:, 2 * D:3 * D], [], [b_c])
            DMA("sp", ln2b[:], ln_bc[:, 3 * D:4 * D], [], [b_c])
            for t in range(NT):
                rows = slice(t * 128, (t + 1) * 128)
                for k in range(4):
                    S.dma("pool", (lambda t, k: lambda h: h.indirect_dma_start(
                        out=yk[k][:, :], out_offset=None, in_=Ys[:, :],
                        in_offset=bass.IndirectOffsetOnAxis(ap=dest_i[:, t * 4 + k: t * 4 + k + 1], axis=0)))(t, k),
                        [b_rt, b_ys], [b_yk[k]])
                DMA("sp", x1t[:], x1_d[rows, :], [], [b_x1])
                TS("dve", acc[:], yk[0][:], wcol[:, t * 4: t * 4 + 1], ALU.mult, [b_yk[0], b_rt], [b_acc])
                for k in range(1, 4):
                    STT("dve", acc[:], yk[k][:], wcol[:, t * 4 + k: t * 4 + k + 1], acc[:], ALU.mult, ALU.add,
                        [b_yk[k], b_rt, b_acc], [b_acc])
                for pc in range(4):
                    bank, bb = PB[pc % 2], PBb[pc % 2]
                    MM(bank[:, :], WtT[:, t * 128:(t + 1) * 128], bd_bf[:, pc * 512:(pc + 1) * 512], True, True, [b_rt], [bb])
                    TT("dve", acc[:, pc * 512:(pc + 1) * 512], acc[:, pc * 512:(pc + 1) * 512], bank[:, :], ALU.add,
                       [bb, b_acc], [b_acc])
                TT("dve", acc[:], acc[:], g2_bc[:], ALU.mult, [b_acc, b_g2], [b_acc])
                STT("dve", acc[:], x1t[:], ALPHA, acc[:], ALU.mult, ALU.add, [b_x1, b_acc], [b_acc])
                for c4 in range(4):
                    S.op("dve", (lambda c4: lambda h: h.bn_stats(st6[:, c4 * 6:(c4 + 1) * 6], acc[:, c4 * 512:(c4 + 1) * 512]))(c4),
                         [b_acc], [b_stat])
                S.op("dve", lambda h: h.bn_aggr(mv[:], st6[:]), [b_stat], [b_stat])
                ACT(rstd[:], mv[:, 1:2], AF.Sqrt, [b_stat], [b_stat], bias=EPS)
                S.op("dve", lambda h: h.reciprocal(rstd[:], rstd[:]), [b_stat], [b_stat])
                STT("dve", nmr[:], mv[:, 0:1], -1.0, rstd[:], ALU.mult, ALU.mult, [b_stat], [b_stat])
                ACT(outt[:], acc[:], AF.Identity, [b_acc, b_stat], [b_ot], bias=nmr[:], scale=rstd[:])
                TT("dve", outt[:], outt[:], ln2g[:], ALU.mult, [b_ot, b_c], [b_ot])
                TT("dve", outt[:], outt[:], ln2b[:], ALU.add, [b_ot, b_c], [b_ot])
                DMA("sp", out[rows, :], outt[:], [b_ot], [b_out])
            S.barrier()
    sp_.close()


def prep_shared(inp):
    f = lambda a: np.ascontiguousarray(a, dtype=np.float32)
    b_in = np.asarray(inp["b_in"][0], np.float32)
    pad = np.zeros(41 * 128, np.float32)
    pad[:DIN] = b_in
    col8 = lambda v: np.asarray(v, np.float32).reshape(8, 128).T

    def relayout(w):
        w = np.asarray(w, np.float32).reshape(NE, 16, 128, 4, 512)
        return np.ascontiguousarray(w.transpose(0, 3, 2, 1, 4)).reshape(NE * 4 * 128, 16 * 512)

    cfc, cbc = make_consts()
    sh = {
        "w_ada": f(inp["w_ada"][0]),
        "b_ada": f(inp["b_ada"][0][None, :]),
        "w_in": f(inp["w_in"][0]),
        "b_in_col": f(pad.reshape(41, 128).T),
        "b_kv_bc": f(np.tile(b_in[2560:4096][None, :], (128, 1))),
        "dw_col": f(np.asarray(inp["dw_w"][0], np.float32).reshape(31, 8, 128).transpose(2, 1, 0).reshape(128, 248)),
        "cv_col": f(np.concatenate([col8(inp["dw_b"][0]), col8(inp["conv_ln_g"][0]), col8(inp["conv_ln_b"][0]),
                                    col8(inp["mh_g"][0])], axis=1)),
        "w_out": f(inp["w_out"][0]),
        "ln_bc": f(np.tile(np.concatenate([inp["ln1_g"][0], inp["ln1_b"][0], inp["ln2_g"][0], inp["ln2_b"][0]])[None, :],
                           (128, 1))),
        "router_w": f(inp["router_w"][0]),
        "rb_bc": f(np.tile(np.asarray(inp["router_b"][0], np.float32)[None, :], (128, 1))),
        "wg_l": relayout(inp["w_gate"][0]),
        "wu_l": relayout(inp["w_up"][0]),
        "wd_l": relayout(inp["w_down"][0]),
        "bgu": f(np.concatenate([inp["b_gate"][0], inp["b_up"][0]], axis=1)),
        "b_down": f(inp["b_down"][0]),
        "cf": cfc,
        "cb": cbc,
    }
    return sh


def core_inputs(inp, sh, b):
    m = dict(sh)
    m["x"] = np.ascontiguousarray(inp["x"][b], dtype=np.float32)
    m["c_col"] = np.ascontiguousarray(np.asarray(inp["c"][b], np.float32).reshape(16, 128).T)
    return m


_NC_CACHE = {}


def kernel(**inputs):
    sh = prep_shared(inputs)
    if "nc" not in _NC_CACHE:
        _NC_CACHE["nc"] = build()
    nc = _NC_CACHE["nc"]
    in_maps = [core_inputs(inputs, sh, b) for b in range(8)]
    res = run_bass_kernel_spmd(nc, in_maps, core_ids=list(range(8)))
    return np.stack([np.asarray(r["out"], dtype=np.float32) for r in res.results], axis=0)
```

```python
import numpy as np
from contextlib import ExitStack
import concourse.bass as bass
import concourse.mybir as mybir
from concourse.bass_utils import run_bass_kernel_spmd

F32 = mybir.dt.float32
BF16 = mybir.dt.bfloat16
I32 = mybir.dt.int32
AF = mybir.ActivationFunctionType
ALU = mybir.AluOpType
AX = mybir.AxisListType

NRING = 8
MIXSTOP = 0
EVAC_DVE = 1
B1STOP = 0


class Buf:
    __slots__ = ("name", "w", "rs")

    def __init__(self, name=""):
        self.name = name
        self.w = None
        self.rs = {}


class _Op:
    __slots__ = ("waits", "fn", "signal", "is_dma", "dma_sem")


class _Eng:
    def __init__(self, name):
        self.name = name
        self.ops = []
        self.waited = {}
        self.ndma = 0
        self.ring_tok = [None] * NRING


class Sched:
    def __init__(self, nc):
        self.nc = nc
        self.engs = {n: _Eng(n) for n in ("pe", "act", "dve", "pool", "sp")}
        self.order = []

    def _add_wait(self, eng, op, tok):
        if tok is None:
            return
        if tok[0] == "c":
            if tok[1] == eng.name and eng.name == "pe":
                return
            key = ("c", tok[1])
            v = tok[2]
        else:
            key = ("d", tok[1], tok[2])
            v = tok[3]
        if eng.waited.get(key, -1) >= v:
            return
        eng.waited[key] = v
        op.waits.append(tok)
        if tok[0] == "c":
            self.engs[tok[1]].ops[tok[2]].signal = True

    def _deps(self, eng, op, reads, writes):
        for b in reads:
            self._add_wait(eng, op, b.w)
        for b in writes:
            self._add_wait(eng, op, b.w)
            for t in list(b.rs.values()):
                self._add_wait(eng, op, t)

    def _commit(self, tok, reads, writes):
        key = tok[:2] if tok[0] == "c" else tok[:3]
        for b in reads:
            b.rs[key] = tok
        for b in writes:
            b.w = tok
            b.rs = {}

    def _new(self, engname, fn, is_dma):
        eng = self.engs[engname]
        o = _Op()
        o.waits = []
        o.fn = fn
        o.signal = False
        o.is_dma = is_dma
        o.dma_sem = None
        return eng, o

    def op(self, engname, fn, reads=(), writes=()):
        eng, o = self._new(engname, fn, False)
        self._deps(eng, o, reads, writes)
        idx = len(eng.ops)
        eng.ops.append(o)
        self.order.append((engname, idx))
        tok = ("c", engname, idx)
        self._commit(tok, reads, writes)
        return tok

    def dma(self, engname, fn, reads=(), writes=()):
        eng, o = self._new(engname, fn, True)
        slot = eng.ndma % NRING
        val = 16 * (eng.ndma // NRING + 1)
        eng.ndma += 1
        self._add_wait(eng, o, eng.ring_tok[slot])
        self._deps(eng, o, reads, writes)
        o.dma_sem = slot
        idx = len(eng.ops)
        eng.ops.append(o)
        self.order.append((engname, idx))
        tok = ("d", engname, slot, val)
        eng.ring_tok[slot] = tok
        self._commit(tok, reads, writes)
        return tok

    def all_tokens(self):
        toks = []
        for n, e in self.engs.items():
            if n != "sp":
                for i in range(len(e.ops) - 1, -1, -1):
                    if (not e.ops[i].is_dma) and e.ops[i].fn is not None:
                        toks.append(("c", n, i))
                        break
            toks.extend(t for t in e.ring_tok if t is not None)
        return toks

    def barrier(self):
        toks = self.all_tokens()
        for n in self.engs:
            eng, o = self._new(n, None, False)
            for t in toks:
                self._add_wait(eng, o, t)
            idx = len(eng.ops)
            eng.ops.append(o)
            self.order.append((n, idx))

    def emit(self, stack):
        nc = self.nc
        csem = {n: stack.enter_context(nc.semaphore("c_" + n)) for n in self.engs}
        dsem = {}
        for n in ("sp", "pool"):
            for s in range(NRING):
                dsem[(n, s)] = stack.enter_context(nc.semaphore("d_%s_%d" % (n, s)))
        pref = {}
        for n, e in self.engs.items():
            c = 0
            arr = []
            for o in e.ops:
                if o.signal and not o.is_dma and o.fn is not None:
                    c += 1
                arr.append(c)
            pref[n] = arr
        hs = {"pe": nc.tensor, "act": nc.scalar, "dve": nc.vector, "pool": nc.gpsimd, "sp": nc.sync}
        for (n, i) in self.order:
            o = self.engs[n].ops[i]
            h = hs[n]
            for t in o.waits:
                if t[0] == "c":
                    h.wait_ge(csem[t[1]], pref[t[1]][t[2]])
                else:
                    h.wait_ge(dsem[(t[1], t[2])], t[3])
            if o.fn is None:
                continue
            inst = o.fn(h)
            if o.is_dma:
                inst.then_inc(dsem[(n, o.dma_sem)], 16)
            elif o.signal:
                inst.then_inc(csem[n], 1)


S_TOK = 4096
D = 2048
NT = 32
ST = 256
NST = S_TOK // ST
TPS = ST // 128
CPS = ST // 64
DIN = 5128
NE = 32
BLK = 512
NBLK = 63
NSLOT = NBLK * BLK
ALPHA = 2.0 ** 0.25
EPS = 1e-5
QS = 128.0 ** -0.5

C_ID = 0
C_MASK = 128
C_IOTA = 192
C_SELI = 193
C_SELF = 197
C_NEGI = 201
C_SELB = 205
C_ONE = 717
C_IOE = 1229
C_IO16 = 1292
CF_W = 1296
B_ID = 0
B_ONE = 128
B_TRI = 640
CB_W = 768


def make_consts():
    import ml_dtypes
    cf = np.zeros((128, CF_W), np.float32)
    cf[:, C_ID:C_ID + 128] = np.eye(128, dtype=np.float32)
    p = np.arange(128)
    cf[:, C_MASK:C_MASK + 64] = ((p % 64)[:, None] <= np.arange(64)[None, :]).astype(np.float32)
    cf[:, C_IOTA] = p
    for h in range(4):
        cf[h, C_SELI + h] = 1.0
        cf[h + 4, C_SELF + h] = 1.0
        cf[h, C_NEGI + h] = -1.0
        cf[h, C_SELB + h * 128:C_SELB + (h + 1) * 128] = 1.0
    cf[:, C_ONE:C_ONE + 512] = 1.0
    cf[:, C_IOE:C_IOE + 63] = np.arange(63, dtype=np.float32)[None, :]
    cf[:, C_IO16:C_IO16 + 4] = (np.arange(4, dtype=np.float32) * 128.0)[None, :]
    cb = np.zeros((128, CB_W), np.float32)
    cb[:, B_ID:B_ID + 128] = np.eye(128)
    cb[:, B_ONE:B_ONE + 512] = 1.0
    cb[:, B_TRI:B_TRI + 128] = (p[:, None] < p[None, :]).astype(np.float32)
    return cf, cb.astype(ml_dtypes.bfloat16)


def build(dbg=False, phases=("A", "B", "C", "D", "E")):
    nc = bass.Bass("TRN2", target_bir_lowering=False)

    def din(name, shape, dt=F32):
        return nc.dram_tensor(name, list(shape), dt, kind="ExternalInput").ap()

    x = din("x", [S_TOK, D])
    c_col = din("c_col", [128, 16])
    w_ada = din("w_ada", [D, 6 * D])
    b_ada = din("b_ada", [1, 6 * D])
    w_in = din("w_in", [D, DIN])
    b_in_col = din("b_in_col", [128, 41])
    b_kv_bc = din("b_kv_bc", [128, 1536])
    dw_col = din("dw_col", [128, 8 * 31])
    cv_col = din("cv_col", [128, 32])
    w_out = din("w_out", [D, D])
    ln_bc = din("ln_bc", [128, 4 * D])
    router_w = din("router_w", [D, NE])
    rb_bc = din("rb_bc", [128, NE])
    if "C" in phases:
        wg_l = din("wg_l", [16384, 8192])
        wu_l = din("wu_l", [16384, 8192])
        wd_l = din("wd_l", [16384, 8192])
    else:
        wg_l = wu_l = wd_l = None
    bgu = din("bgu", [NE, 2 * D])
    b_down = din("b_down", [NE, D])
    cf = din("cf", [128, CF_W])
    cb = din("cb", [128, CB_W], BF16)
    out = nc.dram_tensor("out", [S_TOK, D], F32, kind="ExternalOutput").ap()
    ikind = "ExternalOutput" if dbg else "Internal"
    x1_d = nc.dram_tensor("x1_d", [S_TOK, D], F32, kind=ikind).ap()
    h2_d = nc.dram_tensor("h2_d", [S_TOK, D], BF16, kind=ikind).ap()
    Xs = nc.dram_tensor("Xs", [NSLOT, D], BF16, kind="Internal").ap()
    Ys = nc.dram_tensor("Ys", [NSLOT, D], F32, kind="Internal").ap()
    diag_d = nc.dram_tensor("diag_d", [8, 128, 31 * 128], BF16, kind="Internal").ap()
    if dbg:
        d_ymix = nc.dram_tensor("d_ymix", [128, 16 * ST], BF16, kind="ExternalOutput").ap()
        d_hT = nc.dram_tensor("d_hT", [128, 16 * ST], BF16, kind="ExternalOutput").ap()
        d_rt = nc.dram_tensor("d_rt", [128, NT * 40], F32, kind="ExternalOutput").ap()

    gs = ExitStack()
    S = Sched(nc)

    def sb(name, shape, dt, st=None):
        return (st or gs).enter_context(nc.sbuf_tensor(name, list(shape), dt))

    def MM(o, lhsT, rhs, start, stop, reads, writes, **kw):
        S.op("pe", lambda h: h.matmul(o, lhsT, rhs, start=start, stop=stop, **kw), reads, writes)

    def TR(o, in_, ident, reads, writes):
        S.op("pe", lambda h: h.transpose(o, in_, ident), reads, writes)

    def ACT(o, in_, func, reads, writes, bias=None, scale=None, eng="act"):
        kw = {}
        if bias is not None:
            kw["bias"] = bias
        if scale is not None:
            kw["scale"] = scale
        S.op("act", lambda h: h.activation(out=o, in_=in_, func=func, **kw), reads, writes)

    def TT(eng, o, a, b, op, reads, writes):
        S.op(eng, lambda h: h.tensor_tensor(out=o, in0=a, in1=b, op=op), reads, writes)

    def TS(eng, o, a, s1, op0, reads, writes, s2=None, op1=None):
        if op1 is None:
            S.op(eng, lambda h: h.tensor_scalar(out=o, in0=a, scalar1=s1, scalar2=None, op0=op0), reads, writes)
        else:
            S.op(eng, lambda h: h.tensor_scalar(out=o, in0=a, scalar1=s1, scalar2=s2, op0=op0, op1=op1), reads, writes)

    def STT(eng, o, a, sc, b, op0, op1, reads, writes):
        S.op(eng, lambda h: h.scalar_tensor_tensor(out=o, in0=a, scalar=sc, in1=b, op0=op0, op1=op1), reads, writes)

    def CP(eng, o, a, reads, writes):
        if eng == "act":
            S.op("act", lambda h: h.activation(out=o, in_=a, func=AF.Copy), reads, writes)
        else:
            S.op(eng, lambda h: h.tensor_copy(o, a), reads, writes)

    def DMA(q, o, a, reads, writes):
        return S.dma(q, lambda h: h.dma_start(out=o, in_=a), reads, writes)

    def RED(o, a, op, reads, writes):
        S.op("dve", lambda h: h.tensor_reduce(out=o, in_=a, axis=AX.X, op=op), reads, writes)

    PB = [gs.enter_context(nc.psum_tensor("pb%d" % i, [128, 512], F32)) for i in range(6)]
    PBb = [Buf("pb%d" % i) for i in range(6)]
    PTS = [gs.enter_context(nc.psum_tensor("pt%d" % i, [128, 1024], BF16)) for i in range(2)]
    PT = PTS
    PTb = [Buf("pt0"), Buf("pt1")]
    bigc = [0]

    def nextbank():
        i = bigc[0] % 3
        bigc[0] += 1
        return PB[i], PBb[i]

    xc = [0]
    yc = [0]

    xmode = [0]

    def xbank():
        if xmode[0] == 0:
            i = xc[0] % 2
            xc[0] += 1
            return PB[i], PBb[i]
        return PB[0], PBb[0]

    def ybank():
        i = (2, 4)[yc[0] % 2]
        yc[0] += 1
        return PB[i], PBb[i]

    cf_t = sb("cf_t", [128, CF_W], F32)
    cb_t = sb("cb_t", [128, CB_W], BF16)
    bcol = sb("bcol", [128, 41], F32)
    bqs = sb("bqs", [128, 4], F32)
    dwc = sb("dwc", [128, 248], F32)
    cvc = sb("cvc", [128, 32], F32)
    mod_col = sb("mod_col", [128, 96], F32)
    sc1p = sb("sc1p", [128, 16], F32)
    g2_bc = sb("g2_bc", [128, D], F32)
    b_c = Buf("consts")
    b_g2 = Buf("g2")
    DMA("sp", cf_t[:], cf[:], [], [b_c])
    DMA("sp", cb_t[:], cb[:], [], [b_c])
    DMA("sp", bcol[:], b_in_col[:], [], [b_c])
    DMA("sp", dwc[:], dw_col[:], [], [b_c])
    DMA("sp", cvc[:], cv_col[:], [], [b_c])
    TS("dve", bqs[:], bcol[:, 16:20], QS, ALU.mult, [b_c], [b_c])
    S.barrier()
    ident_b = cb_t[:, B_ID:B_ID + 128]

    sab = ExitStack()
    g1_bc = sb("g1_bc", [128, D], F32, sab)
    sc2p_bc = sb("sc2p_bc", [128, D], F32, sab)
    sh2_bc = sb("sh2_bc", [128, D], F32, sab)
    b_rows = Buf("rows")

    with ExitStack() as sa:
        mod_row = sb("mod_row", [1, 6 * D], F32, sa)
        b_mr = Buf("mr")
        ccol = sb("ccol", [128, 16], F32, sa)
        scb = sb("scb", [128, 16], BF16, sa)
        b_cc = Buf()
        b_scb = Buf()
        DMA("sp", ccol[:], c_col[:], [], [b_cc])
        ACT(scb[:], ccol[:], AF.Silu, [b_cc], [b_scb])
        wa = [sb("wa%d" % i, [128, 16, 512], BF16, sa) for i in range(3)]
        b_wa = [Buf() for _ in range(3)]
        bar = [sb("bar%d" % i, [1, 512], F32, sa) for i in range(2)]
        b_bar = [Buf() for _ in range(2)]
        w_ada_v = w_ada.rearrange("(k p) n -> p k n", p=128)
        for pc in range(24):
            i = pc % 3
            DMA("pool", wa[i][:], w_ada_v[:, :, pc * 512:(pc + 1) * 512], [], [b_wa[i]])
            DMA("sp", bar[pc % 2][:], b_ada[0:1, pc * 512:(pc + 1) * 512], [], [b_bar[pc % 2]])
            bank, bb = PB[pc % 2], PBb[pc % 2]
            for k in range(16):
                MM(bank[0:1, :], scb[:, k:k + 1], wa[i][:, k, :], k == 0, k == 15, [b_scb, b_wa[i]], [bb])
            TT("dve", mod_row[0:1, pc * 512:(pc + 1) * 512], bank[0:1, :], bar[pc % 2][0:1, :], ALU.add,
               [bb, b_bar[pc % 2]], [b_mr])
        for j in range(96):
            MM(PB[2][:, j:j + 1], mod_row[0:1, j * 128:(j + 1) * 128], cf_t[0:1, C_ONE:C_ONE + 1], True, True,
               [b_mr, b_c], [PBb[2]])
        CP("dve", mod_col[:], PB[2][:, 0:96], [PBb[2]], [b_c])
        TS("dve", sc1p[:], mod_col[:, 16:32], 1.0, ALU.add, [b_c], [b_c])
        for (dst, off, plus1, bdst) in ((g1_bc, 2 * D, False, b_rows), (sh2_bc, 3 * D, False, b_rows),
                                        (sc2p_bc, 4 * D, True, b_rows), (g2_bc, 5 * D, False, b_g2)):
            for i in range(4):
                bank, bb = PB[i % 2], PBb[i % 2]
                MM(bank[:, :], cf_t[0:1, C_ONE:C_ONE + 128], mod_row[0:1, off + i * 512: off + (i + 1) * 512],
                   True, True, [b_mr, b_c], [bb])
                if plus1:
                    ACT(dst[:, i * 512:(i + 1) * 512], bank[:, :], AF.Identity, [bb], [bdst], bias=1.0)
                else:
                    CP("dve", dst[:, i * 512:(i + 1) * 512], bank[:, :], [bb], [bdst])
        S.barrier()
    sh1 = mod_col[:, 0:16]

    if "B" in phases:
        build_mixer(nc, S, locals())
    S.barrier()
    sab.close()

    if "C" in phases:
        build_moe(nc, S, locals())

    S.barrier()
    S.emit(gs)
    gs.close()
    return nc


def build_mixer(nc, S, L):
    (MM, TR, ACT, TT, TS, STT, CP, DMA, RED, sb, PB, PBb, PT, PTb, nextbank) = (
        L[k] for k in ("MM", "TR", "ACT", "TT", "TS", "STT", "CP", "DMA", "RED", "sb", "PB", "PBb", "PT", "PTb",
                       "nextbank"))
    xbank, ybank, xmode = L["xbank"], L["ybank"], L["xmode"]
    cf_t, cb_t, bcol, bqs, dwc, cvc, mod_col, sc1p, sh1, b_c = (
        L[k] for k in ("cf_t", "cb_t", "bcol", "bqs", "dwc", "cvc", "mod_col", "sc1p", "sh1", "b_c"))
    g1_bc, sc2p_bc, sh2_bc, b_rows = (L[k] for k in ("g1_bc", "sc2p_bc", "sh2_bc", "b_rows"))
    x, w_in, w_out, b_kv_bc, ln_bc, x1_d, h2_d = (L[k] for k in ("x", "w_in", "w_out", "b_kv_bc", "ln_bc", "x1_d", "h2_d"))
    dbg = L["dbg"]
    ident_b = cb_t[:, B_ID:B_ID + 128]
    sm = ExitStack()

    def T(name, shape, dt):
        return sb(name, shape, dt, sm)

    xall = T("xall", [128, TPS, D], F32)
    b_x = [Buf() for _ in range(TPS)]
    xn = T("xn", [128, D], BF16)
    b_xn = Buf()
    st6 = T("st6", [128, 24], F32)
    mv = T("mv", [128, 2], F32)
    rstd = T("rstd", [128, 1], F32)
    nmr = T("nmr", [128, 1], F32)
    b_stat = Buf()
    hT = T("hT", [128, 16, ST], BF16)
    b_hT = Buf()
    wgb = [T("wgb%d" % i, [128, 16, 512], BF16) for i in range(2)]
    b_wg = [Buf() for _ in range(2)]
    wgt = T("wgt", [128, 16, 8], BF16)
    b_wgt = Buf()
    sg = T("sg", [128, 8, ST], BF16)
    b_sg = Buf()
    uT = T("uT", [128, 8, 30 + ST], BF16)
    b_uT = Buf()
    qT = T("qT", [128, 4, ST], BF16)
    kT = T("kT", [128, 4, ST], BF16)
    b_qk = Buf()
    sgo = T("sgo", [128, 8, ST], BF16)
    b_sgo = Buf()
    ktok = T("ktok", [64, CPS, 512], BF16)
    vtok = T("vtok", [64, CPS, 1024], BF16)
    b_kv = Buf()
    bkv = T("bkv", [128, 1536], F32)
    ymixT = T("ymixT", [128, 16, ST], BF16)
    b_ym = Buf()
    ycv = T("ycv", [128, 8, ST], F32)
    b_ycv = Buf()
    diag = [T("diag%d" % i, [128, 31, 128], BF16) for i in range(2)]
    b_diag = [Buf() for _ in range(2)]
    diag_d = L["diag_d"]
    b_dd = Buf()
    ybq = T("ybq", [128, 2 * ST], BF16)
    b_yb = Buf()
    mean = T("mean", [128, ST], F32)
    rstc = T("rstc", [128, ST], F32)
    msq = T("msq", [128, ST], F32)
    b_cst = Buf()
    ctmp = T("ctmp", [128, ST], F32)
    b_ctmp = Buf()
    gsb = T("gsb", [8, ST], F32)
    expg = T("expg", [8, ST], F32)
    lfn = T("lfn", [8, ST], F32)
    bneg = T("bneg", [8, ST], F32)
    A_sb = T("A_sb", [4, ST], F32)
    G_sb = T("G_sb", [4, ST], F32)
    E_sb = T("E_sb", [4, ST], F32)
    b_gt = Buf()
    bcar = T("bcar", [8, 1], F32)
    gcar = T("gcar", [4, 1], F32)
    G_bc = T("G_bc", [128, 4, ST], F32)
    gprev = T("gprev", [128, 4], F32)
    b_gbc = Buf()
    colsc = T("colsc", [64, CPS, 8], F32)
    b_cols = Buf()
    Cst = T("Cst", [128, 4, 256], F32)
    nst = T("nst", [128, 4], F32)
    Cbf = T("Cbf", [128, 4, 256], BF16)
    nbf = T("nbf", [128, 4], BF16)
    b_C = Buf()
    b_Cbf = Buf()
    DTt = [T("DT%d" % i, [64, 64], F32) for i in range(2)]
    Dm = [T("Dm%d" % i, [64, 64], F32) for i in range(2)]
    b_DT = [Buf() for _ in range(2)]
    b_Dm = [Buf() for _ in range(2)]
    sT = T("sT", [64, 4, 64], BF16)
    b_sT = [Buf() for _ in range(4)]
    wi = [T("wi%d" % i, [128, 64], F32) for i in range(2)]
    b_wi = [Buf() for _ in range(2)]
    qs = T("qs", [128, 4, 64], BF16)
    b_qs = [Buf() for _ in range(4)]
    kw = [T("kw%d" % i, [64, 4, 128], BF16) for i in range(2)]
    b_kw = [[Buf() for _ in range(4)] for _ in range(2)]
    wkc = [T("wkc%d" % i, [64, 4], F32) for i in range(2)]
    dec = [T("dec%d" % i, [128, 4], F32) for i in range(2)]
    b_sm = [Buf() for _ in range(2)]
    dabs = T("dabs", [64, 4], F32)
    dd = T("dd", [64, 4], F32)
    rr = T("rr", [64, 4], F32)
    ssq = T("ssq", [64, 4], F32)
    tcol = T("tcol", [64, 4], F32)
    fcol = T("fcol", [64, 4], F32)
    b_nrm = Buf()
    sqt = T("sqt", [64, 256], F32)
    b_sqt = Buf()
    hn = T("hn", [64, CPS, 4, 256], BF16)
    b_hn = Buf()
    tmpw = T("tmpw", [128, 512], F32)
    b_tmpw = Buf()
    x1t = ycv[:].rearrange("p c n -> p (c n)")
    b_x1t = b_ycv
    h2b = xn[:]
    b_h2b = b_xn
    ln1g = hT[:].bitcast(F32).rearrange("p k n -> p (k n)")
    ln1b = ymixT[:].bitcast(F32).rearrange("p k n -> p (k n)")
    b_dx = Buf()
    b_dh = Buf()

    DMA("sp", bkv[:], b_kv_bc[:], [], [b_c])
    S.op("pool", lambda h: h.memset(uT[:, :, 0:30], 0.0), [], [b_uT])
    S.op("pool", lambda h: h.memset(Cst[:], 0.0), [], [b_C])
    S.op("pool", lambda h: h.memset(nst[:], 0.0), [], [b_C])
    S.op("pool", lambda h: h.memset(Cbf[:], 0.0), [], [b_Cbf])
    S.op("pool", lambda h: h.memset(nbf[:], 0.0), [], [b_Cbf])
    S.op("pool", lambda h: h.memset(bcar[:], 0.0), [], [b_gt])
    S.op("pool", lambda h: h.memset(gcar[:], 0.0), [], [b_gt])
    S.op("pool", lambda h: h.memset(gprev[:], 0.0), [], [b_gbc])
    for c in range(8):
        for j in range(31):
            ACT(diag[c % 2][:, j, :], ident_b, AF.Identity, [b_c], [b_diag[c % 2]], scale=dwc[:, c * 31 + j: c * 31 + j + 1])
        DMA("sp", diag_d[c], diag[c % 2][:].rearrange("p j n -> p (j n)"), [b_diag[c % 2]], [b_dd])
    S.barrier()
    DMA("sp", diag[0][:].rearrange("p j n -> p (j n)"), diag_d[0], [b_dd], [b_diag[0]])
    gconv = [0]

    w_in_v = w_in.rearrange("(k p) n -> p k n", p=128)
    w_out_v = w_out.rearrange("(k p) n -> p k n", p=128)
    wcnt = [0]

    def load_w(src):
        i = wcnt[0] % 2
        wcnt[0] += 1
        DMA("pool", wgb[i][:], src, [], [b_wg[i]])
        return wgb[i], b_wg[i]

    def ln_stats(src, rd):
        for c4 in range(4):
            S.op("dve", (lambda c4: lambda h: h.bn_stats(st6[:, c4 * 6:(c4 + 1) * 6], src[:, c4 * 512:(c4 + 1) * 512]))(c4),
                 rd, [b_stat])
        S.op("dve", lambda h: h.bn_aggr(mv[:], st6[:]), [b_stat], [b_stat])
        ACT(rstd[:], mv[:, 1:2], AF.Sqrt, [b_stat], [b_stat], bias=EPS)
        S.op("dve", lambda h: h.reciprocal(rstd[:], rstd[:]), [b_stat], [b_stat])
        STT("dve", nmr[:], mv[:, 0:1], -1.0, rstd[:], ALU.mult, ALU.mult, [b_stat], [b_stat])

    for st in range(NST):
        t0 = st * ST
        for i in range(TPS):
            DMA("sp", xall[:, i, :], x[t0 + i * 128: t0 + (i + 1) * 128, :], [], [b_x[i]])
            ln_stats(xall[:, i, :], [b_x[i]])
            if B1STOP == 1:
                continue
            ACT(xn[:], xall[:, i, :], AF.Identity, [b_x[i], b_stat], [b_xn], bias=nmr[:], scale=rstd[:])
            if B1STOP == 2:
                continue
            for kg in range(4):
                hf = kg % 2
                for kk in range(4):
                    k = kg * 4 + kk
                    TR(PT[hf][:, kk * 128:(kk + 1) * 128], xn[:, k * 128:(k + 1) * 128], ident_b,
                       [b_xn, b_c], [PTb[hf]])
                for kk in range(4):
                    if B1STOP == 3:
                        continue
                    k = kg * 4 + kk
                    if EVAC_DVE:
                        TS("dve", hT[:, k, i * 128:(i + 1) * 128], PT[hf][:, kk * 128:(kk + 1) * 128],
                           sc1p[:, k:k + 1], ALU.mult, [PTb[hf], b_c], [b_hT], s2=sh1[:, k:k + 1], op1=ALU.add)
                    else:
                        ACT(hT[:, k, i * 128:(i + 1) * 128], PT[hf][:, kk * 128:(kk + 1) * 128],
                            AF.Identity, [PTb[hf], b_c], [b_hT], bias=sh1[:, k:k + 1], scale=sc1p[:, k:k + 1])
        if dbg and st == 0:
            DMA("sp", L["d_hT"][:], hT[:].rearrange("p k n -> p (k n)"), [b_hT], [Buf()])

        if MIXSTOP == 1:
            continue
        def fm_chunk(wt, bw, c, evac):
            bank, bb = nextbank()
            for k in range(16):
                MM(bank[:, 0:ST], wt[:, k, c * 128:(c + 1) * 128], hT[:, k, :], k == 0, k == 15, [bw, b_hT], [bb])
            evac(bank[:, 0:ST], bb)

        wt, bw = load_w(w_in_v[:, :, 2048:2560])
        for c in range(4):
            fm_chunk(wt, bw, c, (lambda c: lambda ps, bb: ACT(qT[:, c, :], ps, AF.Identity, [bb, b_c], [b_qk],
                                                            bias=bqs[:, c:c + 1], scale=QS))(c))
        wt, bw = load_w(w_in_v[:, :, 2560:3072])
        for c in range(4):
            fm_chunk(wt, bw, c, (lambda c: lambda ps, bb: ACT(kT[:, c, :], ps, AF.Identity, [bb, b_c], [b_qk],
                                                            bias=bcol[:, 20 + c: 21 + c]))(c))
        for ch in range(CPS):
            bank, bb = nextbank()
            for k in range(16):
                MM(bank[0:64, :], hT[:, k, ch * 64:(ch + 1) * 64], wt[:, k, :], k == 0, k == 15, [bw, b_hT], [bb])
            TT("dve", ktok[:, ch, :], bank[0:64, :], bkv[0:64, 0:512], ALU.add, [bb, b_c], [b_kv])
        for g in range(2):
            wt, bw = load_w(w_in_v[:, :, 3072 + g * 512: 3072 + (g + 1) * 512])
            for ch in range(CPS):
                bank, bb = nextbank()
                for k in range(16):
                    MM(bank[0:64, :], hT[:, k, ch * 64:(ch + 1) * 64], wt[:, k, :], k == 0, k == 15, [bw, b_hT], [bb])
                TT("dve", vtok[:, ch, g * 512:(g + 1) * 512], bank[0:64, :], bkv[0:64, 512 + g * 512: 1024 + g * 512],
                   ALU.add, [bb, b_c], [b_kv])
        DMA("pool", wgt[:], w_in_v[:, :, 5120:5128], [], [b_wgt])
        for k in range(16):
            MM(PB[3][0:8, 0:ST], wgt[:, k, :], hT[:, k, :], k == 0, k == 15, [b_wgt, b_hT], [PBb[3]])
        ACT(gsb[:], PB[3][0:8, 0:ST], AF.Identity, [PBb[3], b_c], [b_gt], bias=bcol[0:8, 40:41])

        if MIXSTOP == 2:
            continue
        def streamX():
            xmode[0] = 0
            def fm_chunk_x(wt, bw, c, evac):
                bank, bb = xbank()
                for k in range(16):
                    MM(bank[:, 0:ST], wt[:, k, c * 128:(c + 1) * 128], hT[:, k, :], k == 0, k == 15, [bw, b_hT], [bb])
                evac(bank[:, 0:ST], bb)
            for g in range(2):
                wt, bw = load_w(w_in_v[:, :, 4096 + g * 512: 4096 + (g + 1) * 512])
                for c in range(4):
                    yield
                    cc = g * 4 + c
                    fm_chunk_x(wt, bw, c, (lambda cc: lambda ps, bb: ACT(sgo[:, cc, :], ps, AF.Sigmoid, [bb, b_c], [b_sgo],
                                                                     bias=bcol[:, 32 + cc: 33 + cc]))(cc))
            for g in range(2):
                wt, bw = load_w(w_in_v[:, :, 1024 + g * 512: 1024 + (g + 1) * 512])
                for c in range(4):
                    yield
                    cc = g * 4 + c
                    fm_chunk_x(wt, bw, c, (lambda cc: lambda ps, bb: ACT(sg[:, cc, :], ps, AF.Sigmoid, [bb, b_c], [b_sg],
                                                                     bias=bcol[:, 8 + cc: 9 + cc]))(cc))
            for g in range(2):
                wt, bw = load_w(w_in_v[:, :, g * 512:(g + 1) * 512])
                for c in range(4):
                    yield
                    cc = g * 4 + c
                    fm_chunk_x(wt, bw, c, (lambda cc: lambda ps, bb: STT("dve", uT[:, cc, 30:30 + ST], ps, bcol[:, cc:cc + 1],
                                                                     sg[:, cc, :], ALU.add, ALU.mult, [bb, b_sg, b_c],
                                                                     [b_uT]))(cc))
            xmode[0] = 1
            MM_sum, bsum = PB[1], PBb[1]
            MM_sq, bsq = PB[1], PBb[1]
            for c in range(8):
                yield
                g_ = gconv[0]
                gconv[0] += 1
                if g_ + 1 < NST * 8:
                    DMA("sp", diag[(g_ + 1) % 2][:].rearrange("p j n -> p (j n)"), diag_d[(g_ + 1) % 8], [b_dd],
                        [b_diag[(g_ + 1) % 2]])
                dg, bdg = diag[g_ % 2], b_diag[g_ % 2]
                bank, bb = xbank()
                for j in range(31):
                    MM(bank[:, 0:ST], dg[:, j, :], uT[:, c, j:j + ST], j == 0, j == 30, [bdg, b_uT], [bb])
                ACT(ycv[:, c, :], bank[:, 0:ST], AF.Identity, [bb, b_c], [b_ycv], bias=cvc[:, c:c + 1])
                ACT(ybq[:, 0:ST], bank[:, 0:ST], AF.Identity, [bb, b_c], [b_yb], bias=cvc[:, c:c + 1])
                ACT(ybq[:, ST:2 * ST], bank[:, 0:ST], AF.Square, [bb, b_c], [b_yb], bias=cvc[:, c:c + 1])
                MM(MM_sum[:, 0:2 * ST], cb_t[:, B_ONE:B_ONE + 128], ybq[:], c == 0, c == 7, [b_yb, b_c], [bsum])
            CP("pool", uT[:, :, 0:30], uT[:, :, ST:ST + 30], [b_uT], [b_uT])
            TS("dve", mean[:], MM_sum[:, 0:ST], 1.0 / 1024, ALU.mult, [bsum], [b_cst])
            TT("dve", msq[:], mean[:], mean[:], ALU.mult, [b_cst], [b_cst])
            STT("dve", rstc[:], MM_sq[:, ST:2 * ST], 1.0 / 1024, msq[:], ALU.mult, ALU.subtract, [bsq, b_cst], [b_cst])
            ACT(rstc[:], rstc[:], AF.Sqrt, [b_cst], [b_cst], bias=EPS)
            S.op("dve", lambda h: h.reciprocal(rstc[:], rstc[:]), [b_cst], [b_cst])
            for c in range(8):
                yield
                TT("dve", ctmp[:], ycv[:, c, :], mean[:], ALU.subtract, [b_ycv, b_cst], [b_ctmp])
                TT("dve", ctmp[:], ctmp[:], rstc[:], ALU.mult, [b_ctmp, b_cst], [b_ctmp])
                ACT(ymixT[:, c, :], ctmp[:], AF.Silu, [b_ctmp, b_c], [b_ym], bias=cvc[:, 16 + c: 17 + c],
                    scale=cvc[:, 8 + c: 9 + c])

        def streamY():
            ACT(expg[:], gsb[:], AF.Exp, [b_gt], [b_gt], scale=-1.0)
            ACT(lfn[:], expg[:], AF.Ln, [b_gt], [b_gt], bias=1.0)
            S.op("dve", lambda h: h.tensor_tensor_scan(out=bneg[:], data0=cf_t[0:8, C_ONE:C_ONE + ST], data1=lfn[:],
                                                      initial=bcar[:, 0:1], op0=ALU.mult, op1=ALU.add), [b_gt, b_c], [b_gt])
            CP("dve", bcar[:], bneg[:, ST - 1:ST], [b_gt], [b_gt])
            MM(PB[4][0:4, 0:ST], cf_t[0:8, C_SELI:C_SELI + 4], gsb[:], True, False, [b_gt, b_c], [PBb[4]])
            MM(PB[4][0:4, 0:ST], cf_t[0:8, C_SELF:C_SELF + 4], bneg[:], False, True, [b_gt, b_c], [PBb[4]])
            CP("dve", A_sb[:], PB[4][0:4, 0:ST], [PBb[4]], [b_gt])
            S.op("dve", lambda h: h.tensor_tensor_scan(out=G_sb[:], data0=cf_t[0:4, C_ONE:C_ONE + ST], data1=A_sb[:],
                                                      initial=gcar[:, 0:1], op0=ALU.mult, op1=ALU.max), [b_gt, b_c], [b_gt])
            CP("dve", gcar[:], G_sb[:, ST - 1:ST], [b_gt], [b_gt])
            MM(PB[4][0:4, 0:ST], cf_t[0:8, C_SELF:C_SELF + 4], bneg[:], True, False, [b_gt, b_c], [PBb[4]])
            MM(PB[4][0:4, 0:ST], cf_t[0:4, C_NEGI:C_NEGI + 4], G_sb[:], False, True, [b_gt, b_c], [PBb[4]])
            ACT(E_sb[:], PB[4][0:4, 0:ST], AF.Exp, [PBb[4]], [b_gt])
            if st > 0:
                CP("dve", gprev[:], G_bc[:, :, ST - 1], [b_gbc], [b_gbc])
            for h_ in range(4):
                yield
                MM(PB[3][:, 0:ST], cf_t[0:4, C_SELB + h_ * 128: C_SELB + (h_ + 1) * 128], G_sb[:], True, True,
                   [b_gt, b_c], [PBb[3]])
                CP("dve", G_bc[:, h_, :], PB[3][:, 0:ST], [PBb[3]], [b_gbc])
            for ch in range(CPS):
                S.op("pe", (lambda ch: lambda h: h.transpose(PB[4][0:64, 0:4], A_sb[0:4, ch * 64:(ch + 1) * 64],
                                                             cf_t[0:4, C_ID:C_ID + 4]))(ch), [b_gt, b_c], [PBb[4]])
                S.op("pe", (lambda ch: lambda h: h.transpose(PB[4][0:64, 4:8], E_sb[0:4, ch * 64:(ch + 1) * 64],
                                                             cf_t[0:4, C_ID:C_ID + 4]))(ch), [b_gt, b_c], [PBb[4]])
                CP("dve", colsc[:, ch, :], PB[4][0:64, 0:8], [PBb[4]], [b_cols])

            S32 = PT[1][:].bitcast(F32)
            bS32 = PTb[1]

            def front(ch):
                cs = slice(ch * 64, (ch + 1) * 64)
                last = ch * 64 + 63
                par = ch % 2
                for h_ in range(4):
                    yield
                    gp = G_bc[:, h_, ch * 64 - 1: ch * 64] if ch > 0 else gprev[:, h_:h_ + 1]
                    i2 = h_ % 2
                    MM(S32[0:64, h_ * 64:(h_ + 1) * 64], kT[:, h_, cs], qT[:, h_, cs], True, True, [b_qk], [bS32])
                    ACT(DTt[i2][:], G_bc[0:64, h_, cs], AF.Exp, [b_gbc, b_cols], [b_DT[i2]], bias=colsc[:, ch, h_:h_ + 1],
                        scale=-1.0)
                    TT("dve", Dm[i2][:], DTt[i2][:], cf_t[0:64, C_MASK:C_MASK + 64], ALU.mult, [b_DT[i2], b_c], [b_Dm[i2]])
                    TT("dve", sT[:, h_, :], S32[0:64, h_ * 64:(h_ + 1) * 64], Dm[i2][:], ALU.mult, [bS32, b_Dm[i2]],
                       [b_sT[h_]])
                    ACT(wi[i2][:], G_bc[:, h_, cs], AF.Exp, [b_gbc], [b_wi[i2]], bias=gp, scale=-1.0)
                    TT("dve", qs[:, h_, :], qT[:, h_, cs], wi[i2][:], ALU.mult, [b_qk, b_wi[i2]], [b_qs[h_]])
                    ACT(wkc[par][:, h_:h_ + 1], G_bc[0:64, h_, last:last + 1], AF.Exp, [b_gbc, b_cols], [b_sm[par]],
                        bias=colsc[:, ch, h_:h_ + 1], scale=-1.0)
                    ACT(dec[par][:, h_:h_ + 1], G_bc[:, h_, last:last + 1], AF.Exp, [b_gbc], [b_sm[par]], bias=gp, scale=-1.0)
                    TS("dve", kw[par][:, h_, :], ktok[:, ch, h_ * 128:(h_ + 1) * 128], wkc[par][:, h_:h_ + 1], ALU.mult,
                       [b_kv, b_sm[par]], [b_kw[par][h_]])

            yield from front(0)
            for ch in range(CPS):
                cs = slice(ch * 64, (ch + 1) * 64)
                par = ch % 2
                nbanks = []
                for pr in range(2):
                    yield
                    bank, bb = ybank()
                    nbanks.append((bank, bb))
                    for hh in range(2):
                        h_ = pr * 2 + hh
                        MM(bank[0:64, hh * 256:(hh + 1) * 256], sT[:, h_, :], vtok[:, ch, h_ * 256:(h_ + 1) * 256], True, False,
                           [b_sT[h_], b_kv], [bb])
                        MM(bank[0:64, hh * 256:(hh + 1) * 256], qs[:, h_, :], Cbf[:, h_, :], False, True, [b_qs[h_], b_Cbf], [bb])
                for h_ in range(4):
                    MM(PB[3][0:64, h_:h_ + 1], sT[:, h_, :], cb_t[0:64, B_ONE:B_ONE + 1], True, False, [b_sT[h_], b_c], [PBb[3]])
                    MM(PB[3][0:64, h_:h_ + 1], qs[:, h_, :], nbf[:, h_:h_ + 1], False, True, [b_qs[h_], b_Cbf], [PBb[3]])
                CP("dve", dd[:], PB[3][0:64, 0:4], [PBb[3]], [b_nrm])
                for pr in range(2):
                    yield
                    bank, bb = PB[5], PBb[5]
                    for hh in range(2):
                        h_ = pr * 2 + hh
                        MM(bank[:, hh * 256:(hh + 1) * 256], kw[par][:, h_, :], vtok[:, ch, h_ * 256:(h_ + 1) * 256], True, True,
                           [b_kw[par][h_], b_kv], [bb])
                        MM(PB[3][:, 8 + h_: 9 + h_], kw[par][:, h_, :], cb_t[0:64, B_ONE:B_ONE + 1], True, True,
                           [b_kw[par][h_], b_c], [PBb[3]])
                    for hh in range(2):
                        h_ = pr * 2 + hh
                        STT("dve", Cst[:, h_, :], Cst[:, h_, :], dec[par][:, h_:h_ + 1], bank[:, hh * 256:(hh + 1) * 256],
                            ALU.mult, ALU.add, [bb, b_sm[par], b_C], [b_C])
                        STT("dve", nst[:, h_:h_ + 1], nst[:, h_:h_ + 1], dec[par][:, h_:h_ + 1], PB[3][:, 8 + h_: 9 + h_],
                            ALU.mult, ALU.add, [PBb[3], b_sm[par], b_C], [b_C])
                CP("act", Cbf[:].rearrange("p h v -> p (h v)"), Cst[:].rearrange("p h v -> p (h v)"), [b_C], [b_Cbf])
                CP("act", nbf[:], nst[:], [b_C], [b_Cbf])
                if ch + 1 < CPS:
                    yield from front(ch + 1)
                STT("dve", dabs[:], dd[:], -1.0, dd[:], ALU.mult, ALU.max, [b_nrm], [b_nrm])
                TT("dve", dd[:], dabs[:], colsc[:, ch, 4:8], ALU.max, [b_nrm, b_cols], [b_nrm])
                S.op("dve", lambda h: h.reciprocal(rr[:], dd[:]), [b_nrm], [b_nrm])
                for h_ in range(4):
                    yield
                    bank, bb = nbanks[h_ // 2]
                    hh = h_ % 2
                    ACT(sqt[:], bank[0:64, hh * 256:(hh + 1) * 256], AF.Square, [bb], [b_sqt])
                    RED(ssq[:, h_:h_ + 1], sqt[:], ALU.add, [b_sqt], [b_nrm])
                TT("dve", tcol[:], rr[:], rr[:], ALU.mult, [b_nrm], [b_nrm])
                TT("dve", tcol[:], tcol[:], ssq[:], ALU.mult, [b_nrm], [b_nrm])
                TS("dve", tcol[:], tcol[:], 1.0 / 256, ALU.mult, [b_nrm], [b_nrm], s2=EPS, op1=ALU.add)
                ACT(tcol[:], tcol[:], AF.Sqrt, [b_nrm], [b_nrm])
                S.op("dve", lambda h: h.reciprocal(tcol[:], tcol[:]), [b_nrm], [b_nrm])
                TT("dve", fcol[:], rr[:], tcol[:], ALU.mult, [b_nrm], [b_nrm])
                for h_ in range(4):
                    yield
                    bank, bb = nbanks[h_ // 2]
                    hh = h_ % 2
                    ACT(hn[:, ch, h_, :], bank[0:64, hh * 256:(hh + 1) * 256], AF.Identity, [bb, b_nrm], [b_hn],
                        scale=fcol[:, h_:h_ + 1])
                for idx in range(8):
                    h_, half = idx // 2, idx % 2
                    TR(PT[0][:, idx * 64:(idx + 1) * 64], hn[:, ch, h_, half * 128:(half + 1) * 128],
                       cb_t[0:64, B_ID:B_ID + 64], [b_hn, b_c], [PTb[0]])
                for idx in range(8):
                    STT("dve", ymixT[:, 8 + idx, cs], PT[0][:, idx * 64:(idx + 1) * 64],
                        cvc[:, 24 + idx: 25 + idx], sgo[:, idx, cs], ALU.mult, ALU.mult, [PTb[0], b_sgo, b_c], [b_ym])
        gx, gy = streamX(), streamY()
        alive = [gx, gy]
        if MIXSTOP == 3:
            alive = [gx]
        if MIXSTOP == 4:
            alive = [gy]
        while alive:
            for g_ in list(alive):
                try:
                    next(g_)
                except StopIteration:
                    alive.remove(g_)

        if dbg and st == 0:
            DMA("sp", L["d_ymix"][:], ymixT[:].rearrange("p k n -> p (k n)"), [b_ym], [Buf()])

        if MIXSTOP == 5:
            continue
        for pc in range(4):
            wt, bw = load_w(w_out_v[:, :, pc * 512:(pc + 1) * 512])
            for i in range(TPS):
                bank, bb = nextbank()
                for k in range(16):
                    MM(bank[:, :], ymixT[:, k, i * 128:(i + 1) * 128], wt[:, k, :], k == 0, k == 15, [bw, b_ym], [bb])
                TT("dve", tmpw[:], bank[:, :], g1_bc[:, pc * 512:(pc + 1) * 512], ALU.mult, [bb, b_rows], [b_tmpw])
                STT("dve", xall[:, i, pc * 512:(pc + 1) * 512], xall[:, i, pc * 512:(pc + 1) * 512], ALPHA, tmpw[:],
                    ALU.mult, ALU.add, [b_tmpw, b_x[i]], [b_x[i]])
        if MIXSTOP == 6:
            continue
        DMA("sp", ln1g, ln_bc[:, 0:D], [], [b_hT])
        DMA("sp", ln1b, ln_bc[:, D:2 * D], [], [b_ym])
        for i in range(TPS):
            rows = slice(t0 + i * 128, t0 + (i + 1) * 128)
            ln_stats(xall[:, i, :], [b_x[i]])
            ACT(x1t, xall[:, i, :], AF.Identity, [b_x[i], b_stat], [b_x1t], bias=nmr[:], scale=rstd[:])
            TT("dve", x1t, x1t, ln1g, ALU.mult, [b_x1t, b_hT], [b_x1t])
            TT("dve", x1t, x1t, ln1b, ALU.add, [b_x1t, b_ym], [b_x1t])
            DMA("sp", x1_d[rows, :], x1t, [b_x1t], [b_dx])
            ln_stats(x1t, [b_x1t])
            ACT(xall[:, i, :], x1t, AF.Identity, [b_x1t, b_stat], [b_x[i]], bias=nmr[:], scale=rstd[:])
            TT("dve", xall[:, i, :], xall[:, i, :], sc2p_bc[:], ALU.mult, [b_x[i], b_rows], [b_x[i]])
            TT("dve", h2b, xall[:, i, :], sh2_bc[:], ALU.add, [b_x[i], b_rows], [b_h2b])
            DMA("sp", h2_d[rows, :], h2b, [b_h2b], [b_dh])
    S.barrier()
    sm.close()


def build_moe(nc, S, L):
    (MM, TR, ACT, TT, TS, STT, CP, DMA, RED, sb, PB, PBb, PT, PTb) = (
        L[k] for k in ("MM", "TR", "ACT", "TT", "TS", "STT", "CP", "DMA", "RED", "sb", "PB", "PBb", "PT", "PTb"))
    cf_t, cb_t, b_c, g2_bc, b_g2 = (L[k] for k in ("cf_t", "cb_t", "b_c", "g2_bc", "b_g2"))
    x1_d, h2_d, Xs, Ys, out, ln_bc = (L[k] for k in ("x1_d", "h2_d", "Xs", "Ys", "out", "ln_bc"))
    router_w, rb_bc, wg_l, wu_l, wd_l, bgu, b_down = (L[k] for k in ("router_w", "rb_bc", "wg_l", "wu_l", "wd_l", "bgu",
                                                                      "b_down"))
    dbg = L["dbg"]
    phases = L["phases"]
    ident_b = cb_t[:, B_ID:B_ID + 128]
    sp_ = ExitStack()

    def P(name, shape, dt):
        return sb(name, shape, dt, sp_)

    dest_i = P("dest_i", [128, NT * 4], I32)
    wcol = P("wcol", [128, NT * 4], F32)
    WtT = P("WtT", [32, S_TOK], BF16)
    idxW_i = P("idxW_i", [128, NBLK * 4], I32)
    ej_i = P("ej_i", [128, NBLK], I32)
    bd_bf = P("bd_bf", [32, D], BF16)
    b_rt = Buf("routing")
    DMA("pool", bd_bf[:], b_down[:, :], [], [b_rt])

    with ExitStack() as sc:
        def T(name, shape, dt):
            return sb(name, shape, dt, sc)

        h2all = T("h2all", [128, NT, D], BF16)
        b_h2 = [Buf() for _ in range(NT)]
        rwb = T("rwb", [128, 16, NE], BF16)
        rbb = T("rbb", [128, NE], F32)
        h2T = [T("h2T%d" % i, [128, 16, 128], BF16) for i in range(2)]
        b_h2T = [Buf() for _ in range(2)]
        logit_all = T("logit_all", [128, NT, NE], F32)
        top8_all = T("top8_all", [128, NT, 8], F32)
        Wt_all = T("Wt_all", [128, NT, NE], F32)
        pos_all = T("pos_all", [128, NT, NE], F32)
        b_la = Buf()
        maskf = T("maskf", [128, NE], F32)
        mask_bf = T("mask_bf", [128, NE], BF16)
        nmx = T("nmx", [128, 1], F32)
        ex = T("ex", [128, NE], F32)
        rs = T("rs", [128, 1], F32)
        b_tl = Buf()
        cnt = T("cnt", [128, NE], F32)
        b_cnt = Buf()
        zero1 = T("zero1", [128, 1], F32)
        yv = T("yv", [128, NE], F32)
        fr = T("fr", [128, NE], F32)
        nb = T("nb", [128, NE], F32)
        bend = T("bend", [128, NE], F32)
        base = T("base", [128, NE], F32)
        destf = T("destf", [128, NE], F32)
        oh = T("oh", [128, NE], F32)
        prod = T("prod", [128, NE], F32)
        dcol = T("dcol", [128, NT * 4], F32)
        ej = T("ej", [128, NBLK], F32)
        t1 = T("t1", [128, NBLK], F32)
        idxf = T("idxf", [128, NBLK * 4], F32)
        b_g = Buf()

        rw_v = router_w.rearrange("(k p) e -> p k e", p=128)
        DMA("pool", rwb[:], rw_v, [], [b_rt])
        DMA("sp", rbb[:], rb_bc[:], [], [b_rt])
        S.op("pool", lambda h: h.memset(cnt[:], 0.0), [], [b_cnt])
        S.op("pool", lambda h: h.memset(zero1[:], 0.0), [], [b_cnt])
        zt = T("zt", [128, 4, D], BF16)
        S.op("pool", lambda h: h.memset(zt[:], 0.0), [], [b_cnt])
        for t in range(NT):
            DMA("sp", h2all[:, t, :], h2_d[t * 128:(t + 1) * 128, :], [], [b_h2[t]])
        for t in range(NT):
            j = t % 2
            for kg in range(4):
                hf = kg % 2
                for kk in range(4):
                    k = kg * 4 + kk
                    TR(PT[hf][:, kk * 128:(kk + 1) * 128], h2all[:, t, k * 128:(k + 1) * 128], ident_b,
                       [b_h2[t], b_c], [PTb[hf]])
                CP("act" if kg % 2 else "dve", h2T[j][:, kg * 4:(kg + 1) * 4, :].rearrange("p k n -> p (k n)"),
                   PT[hf][:, 0:512], [PTb[hf]], [b_h2T[j]])
            for k in range(16):
                MM(PB[3][:, 0:NE], h2T[j][:, k, :], rwb[:, k, :], k == 0, k == 15, [b_h2T[j], b_rt], [PBb[3]])
            lg = logit_all[:, t, :]
            TT("dve", lg, PB[3][:, 0:NE], rbb[:], ALU.add, [PBb[3], b_rt], [b_la])
            S.op("dve", (lambda t: lambda h: h.max(out=top8_all[:, t, :], in_=logit_all[:, t, :]))(t), [b_la], [b_la])
            TS("dve", maskf[:], lg, top8_all[:, t, 3:4], ALU.is_ge, [b_la], [b_tl])
            TS("dve", nmx[:], top8_all[:, t, 0:1], -1.0, ALU.mult, [b_la], [b_tl])
            ACT(ex[:], lg, AF.Exp, [b_la, b_tl], [b_tl], bias=nmx[:])
            TT("dve", ex[:], ex[:], maskf[:], ALU.mult, [b_tl], [b_tl])
            RED(rs[:], ex[:], ALU.add, [b_tl], [b_tl])
            S.op("dve", lambda h: h.reciprocal(rs[:], rs[:]), [b_tl], [b_tl])
            TS("dve", Wt_all[:, t, :], ex[:], rs[:], ALU.mult, [b_tl], [b_la])
            CP("dve", mask_bf[:], maskf[:], [b_tl], [b_tl])
            MM(PB[4][:, 0:NE], cb_t[:, B_TRI:B_TRI + 128], mask_bf[:], True, True, [b_tl, b_c], [PBb[4]])
            MM(PB[4][:, NE:2 * NE], cb_t[:, B_ONE:B_ONE + 128], mask_bf[:], True, True, [b_tl, b_c], [PBb[4]])
            TT("dve", pos_all[:, t, :], PB[4][:, 0:NE], cnt[:], ALU.add, [PBb[4], b_cnt], [b_la])
            TT("dve", cnt[:], cnt[:], PB[4][:, NE:2 * NE], ALU.add, [PBb[4], b_cnt], [b_cnt])
            TR(PB[5][0:32, 0:128], Wt_all[:, t, :], cf_t[:, C_ID:C_ID + 128], [b_la, b_c], [PBb[5]])
            CP("act", WtT[:, t * 128:(t + 1) * 128], PB[5][0:32, 0:128], [PBb[5]], [b_rt])
        TS("dve", nb[:], cnt[:], 0.0, ALU.is_gt, [b_cnt], [b_g])
        for m_ in range(1, 8):
            TS("dve", fr[:], cnt[:], float(m_ * BLK), ALU.is_gt, [b_cnt, b_g], [b_g])
            TT("dve", nb[:], nb[:], fr[:], ALU.add, [b_g], [b_g])
        S.op("dve", lambda h: h.tensor_tensor_scan(out=bend[:], data0=cf_t[:, C_ONE:C_ONE + NE], data1=nb[:],
                                                  initial=zero1[:, 0:1], op0=ALU.mult, op1=ALU.add), [b_g, b_c, b_cnt], [b_g])
        TT("dve", base[:], bend[:], nb[:], ALU.subtract, [b_g], [b_g])
        TS("dve", base[:], base[:], 512.0, ALU.mult, [b_g], [b_g])
        for t in range(NT):
            TT("dve", destf[:], pos_all[:, t, :], base[:], ALU.add, [b_la, b_g], [b_g])
            for k in range(4):
                TS("dve", oh[:], logit_all[:, t, :], top8_all[:, t, k:k + 1], ALU.is_equal, [b_la, b_g], [b_g])
                TT("dve", prod[:], oh[:], destf[:], ALU.mult, [b_g], [b_g])
                RED(dcol[:, t * 4 + k: t * 4 + k + 1], prod[:], ALU.add, [b_g], [b_g])
                TT("dve", prod[:], oh[:], Wt_all[:, t, :], ALU.mult, [b_g, b_la], [b_g])
                RED(wcol[:, t * 4 + k: t * 4 + k + 1], prod[:], ALU.add, [b_g], [b_rt])
        CP("dve", dest_i[:], dcol[:], [b_g], [b_rt])
        for j in range(NBLK):
            TS("dve", oh[:], bend[:], float(j), ALU.is_le, [b_g], [b_g])
            RED(ej[:, j:j + 1], oh[:], ALU.add, [b_g], [b_g])
        TS("dve", ej[:], ej[:], float(NE - 1), ALU.min, [b_g], [b_g])
        CP("dve", ej_i[:], ej[:], [b_g], [b_rt])
        TS("dve", t1[:], ej[:], 512.0, ALU.mult, [b_g, b_c], [b_g], s2=cf_t[:, C_IOTA:C_IOTA + 1], op1=ALU.add)
        idxv = idxf[:].rearrange("p (j q) -> p j q", q=4)
        for q in range(4):
            TS("dve", idxv[:, :, q], t1[:], float(q * 128), ALU.add, [b_g], [b_g])
        CP("dve", idxW_i[:], idxf[:], [b_g], [b_rt])
        if dbg:
            dr = L["d_rt"]
            DMA("sp", dr[:, 0:NT * 32], logit_all[:].rearrange("p t e -> p (t e)"), [b_la], [Buf()])
            DMA("sp", dr[:, NT * 32:NT * 36], dcol[:], [b_g], [Buf()])
            DMA("sp", dr[:, NT * 36:NT * 40], wcol[:], [b_rt], [Buf()])
        b_xs = Buf("Xs")
        for j in range(NBLK):
            DMA("sp", Xs[j * BLK:(j + 1) * BLK, :].rearrange("(i p) d -> p i d", p=128), zt[:], [b_cnt], [b_xs])
        for t in range(NT):
            for k in range(4):
                S.dma("pool", (lambda t, k: lambda h: h.indirect_dma_start(
                    out=Xs[:, :], out_offset=bass.IndirectOffsetOnAxis(ap=dest_i[:, t * 4 + k: t * 4 + k + 1], axis=0),
                    in_=h2all[:, t, :], in_offset=None))(t, k), [b_rt, b_h2[t]], [b_xs])
        S.barrier()

    b_ys = Buf("Ys")
    if "D" in phases:
        with ExitStack() as sd:
            def T(name, shape, dt):
                return sb(name, shape, dt, sd)

            Xb = T("Xb", [128, 4, D], BF16)
            b_Xb = Buf()
            XbT = [T("XbT%d" % i, [128, 16, BLK], BF16) for i in range(2)]
            b_XbT = [Buf() for _ in range(2)]
            NWQ = 4
            wq = [T("wq%d" % i, [128, 8192], BF16) for i in range(NWQ)]
            b_wq = [Buf() for _ in range(NWQ)]
            actT = T("actT", [128, 16, BLK], BF16)
            b_act = Buf()
            ystage = [T("ystage%d" % i, [128, 4, 512], F32) for i in range(2)]
            b_yst = [Buf() for _ in range(2)]
            bgub = [T("bgub%d" % i, [128, 2 * D], BF16) for i in range(2)]
            b_bgu = [Buf() for _ in range(2)]
            a_t = [T("a_t%d" % i, [128, BLK], F32) for i in range(2)]
            sg_t = [T("sg_t%d" % i, [128, BLK], F32) for i in range(2)]
            u_t = [T("u_t%d" % i, [128, BLK], F32) for i in range(2)]
            b_sw = [Buf() for _ in range(2)]
            swc = [0]

            pieces = []
            for j in range(NBLK):
                for q in range(4):
                    pieces.append((wg_l, j * 4 + q))
                    pieces.append((wu_l, j * 4 + q))
                for q in range(4):
                    pieces.append((wd_l, j * 4 + q))
            issued = [0]
            PRE = 3

            def issue_upto(n):
                while issued[0] <= min(n, len(pieces) - 1):
                    i = issued[0]
                    src, col = pieces[i]
                    S.dma("pool", (lambda i, src, col: lambda h: h.indirect_dma_start(
                        out=wq[i % NWQ][:, :], out_offset=None, in_=src[:, :],
                        in_offset=bass.IndirectOffsetOnAxis(ap=idxW_i[:, col:col + 1], axis=0)))(i, src, col),
                        [b_rt], [b_wq[i % NWQ]])
                    issued[0] += 1

            def piece(n):
                issue_upto(n + PRE)
                return wq[n % NWQ], b_wq[n % NWQ]

            def prep_block(j):
                xt = XbT[j % 2]
                bxt = b_XbT[j % 2]
                DMA("sp", Xb[:], Xs[j * BLK:(j + 1) * BLK, :].rearrange("(i p) d -> p i d", p=128), [b_xs], [b_Xb])
                S.dma("pool", (lambda j: lambda h: h.indirect_dma_start(
                    out=bgub[j % 2][:, :], out_offset=None, in_=bgu[:, :],
                    in_offset=bass.IndirectOffsetOnAxis(ap=ej_i[:, j:j + 1], axis=0)))(j), [b_rt], [b_bgu[j % 2]])
                for k in range(16):
                    hf = k % 2
                    for i in range(4):
                        TR(PT[hf][:, i * 128:(i + 1) * 128], Xb[:, i, k * 128:(k + 1) * 128], ident_b,
                           [b_Xb, b_c], [PTb[hf]])
                    CP("dve", xt[:, k, :], PT[hf][:, 0:512], [PTb[hf]], [bxt])

            gu_banks = [(PB[0], PBb[0], PB[1], PBb[1]), (PB[2], PBb[2], PB[3], PBb[3])]
            dn_banks = [(PB[4], PBb[4]), (PB[5], PBb[5])]
            guc = [0]
            dnc = [0]
            pn = 0
            prep_block(0)
            for j in range(NBLK):
                xt = XbT[j % 2]
                bxt = b_XbT[j % 2]
                bg_t, bbg = bgub[j % 2], b_bgu[j % 2]
                for q in range(4):
                    issue_upto(pn + 3)
                    wg, bwg = wq[pn % NWQ], b_wq[pn % NWQ]
                    wu, bwu = wq[(pn + 1) % NWQ], b_wq[(pn + 1) % NWQ]
                    pn += 2
                    for fc in range(4):
                        f0 = (q * 4 + fc) * 128
                        pg, bpg, pu, bpu = gu_banks[guc[0] % 2]
                        guc[0] += 1
                        MM(pg[:, :], bg_t[0:1, f0:f0 + 128], cb_t[0:1, B_ONE:B_ONE + 512], True, False, [bbg, b_c], [bpg])
                        for k in range(16):
                            MM(pg[:, :], wg[:, k * 512 + fc * 128: k * 512 + (fc + 1) * 128], xt[:, k, :], False, k == 15,
                               [bwg, bxt], [bpg])
                        MM(pu[:, :], bg_t[0:1, D + f0: D + f0 + 128], cb_t[0:1, B_ONE:B_ONE + 512], True, False,
                           [bbg, b_c], [bpu])
                        for k in range(16):
                            MM(pu[:, :], wu[:, k * 512 + fc * 128: k * 512 + (fc + 1) * 128], xt[:, k, :], False, k == 15,
                               [bwu, bxt], [bpu])
                        s_ = swc[0] % 2
                        swc[0] += 1
                        TS("dve", a_t[s_][:], pg[:, :], 7.0, ALU.min, [bpg], [b_sw[s_]])
                        ACT(sg_t[s_][:], a_t[s_][:], AF.Silu, [b_sw[s_]], [b_sw[s_]], scale=1.702)
                        TS("dve", u_t[s_][:], pu[:, :], 7.0, ALU.min, [bpu], [b_sw[s_]], s2=-7.0, op1=ALU.max)
                        STT("dve", actT[:, q * 4 + fc, :], u_t[s_][:], 1.0, sg_t[s_][:], ALU.add, ALU.mult, [b_sw[s_]],
                            [b_act])
                if j + 1 < NBLK:
                    prep_block(j + 1)
                for q in range(4):
                    wd, bwd = piece(pn)
                    pn += 1
                    for i in range(4):
                        pd, bpd = dn_banks[dnc[0] % 2]
                        dnc[0] += 1
                        for fc in range(16):
                            MM(pd[:, :], actT[:, fc, i * 128:(i + 1) * 128], wd[:, fc * 512:(fc + 1) * 512], fc == 0,
                               fc == 15, [b_act, bwd], [bpd])
                        TS("dve", ystage[q % 2][:, i, :], pd[:, :], 1.0 / 1.702, ALU.mult, [bpd], [b_yst[q % 2]])
                    DMA("sp", Ys[j * BLK:(j + 1) * BLK, q * 512:(q + 1) * 512].rearrange("(i p) d -> p i d", p=128),
                        ystage[q % 2][:], [b_yst[q % 2]], [b_ys])
            S.barrier()

    if "E" in phases:
        with ExitStack() as se:
            def T(name, shape, dt):
                return sb(name, shape, dt, se)

            yk = [T("yk%d" % i, [128, D], F32) for i in range(4)]
            b_yk = [Buf() for _ in range(4)]
            acc = T("acc", [128, D], F32)
            b_acc = Buf()
            x1t = T("x1e", [128, D], F32)
            b_x1 = Buf()
            outt = T("outt", [128, D], F32)
            b_ot = Buf()
            ln2g = T("ln2g", [128, D], F32)
            ln2b = T("ln2b", [128, D], F32)
            st6 = T("st6e", [128, 24], F32)
            mv = T("mve", [128, 2], F32)
            rstd = T("rstde", [128, 1], F32)
            nmr = T("nmre", [128, 1], F32)
            b_stat = Buf()
            b_out = Buf()
            DMA("sp", ln2g[:], ln_bc[:, 2 * D:3 * D], [], [b_c])
            DMA("sp", ln2b[:], ln_bc[:, 3 * D:4 * D], [], [b_c])
            for t in range(NT):
                rows = slice(t * 128, (t + 1) * 128)
                for k in range(4):
                    S.dma("pool", (lambda t, k: lambda h: h.indirect_dma_start(
                        out=yk[k][:, :], out_offset=None, in_=Ys[:, :],
                        in_offset=bass.IndirectOffsetOnAxis(ap=dest_i[:, t * 4 + k: t * 4 + k + 1], axis=0)))(t, k),
                        [b_rt, b_ys], [b_yk[k]])
                DMA("sp", x1t[:], x1_d[rows, :], [], [b_x1])
                TS("dve", acc[:], yk[0][:], wcol[:, t * 4: t * 4 + 1], ALU.mult, [b_yk[0], b_rt], [b_acc])
                for k in range(1, 4):
                    STT("dve", acc[:], yk[k][:], wcol[:, t * 4 + k: t * 4 + k + 1], acc[:], ALU.mult, ALU.add,
                        [b_yk[k], b_rt, b_acc], [b_acc])
                for pc in range(4):
                    bank, bb = PB[pc % 2], PBb[pc % 2]
                    MM(bank[:, :], WtT[:, t * 128:(t + 1) * 128], bd_bf[:, pc * 512:(pc + 1) * 512], True, True, [b_rt], [bb])
                    TT("dve", acc[:, pc * 512:(pc + 1) * 512], acc[:, pc * 512:(pc + 1) * 512], bank[:, :], ALU.add,
                       [bb, b_acc], [b_acc])
                TT("dve", acc[:], acc[:], g2_bc[:], ALU.mult, [b_acc, b_g2], [b_acc])
                STT("dve", acc[:], x1t[:], ALPHA, acc[:], ALU.mult, ALU.add, [b_x1, b_acc], [b_acc])
                for c4 in range(4):
                    S.op("dve", (lambda c4: lambda h: h.bn_stats(st6[:, c4 * 6:(c4 + 1) * 6], acc[:, c4 * 512:(c4 + 1) * 512]))(c4),
                         [b_acc], [b_stat])
                S.op("dve", lambda h: h.bn_aggr(mv[:], st6[:]), [b_stat], [b_stat])
                ACT(rstd[:], mv[:, 1:2], AF.Sqrt, [b_stat], [b_stat], bias=EPS)
                S.op("dve", lambda h: h.reciprocal(rstd[:], rstd[:]), [b_stat], [b_stat])
                STT("dve", nmr[:], mv[:, 0:1], -1.0, rstd[:], ALU.mult, ALU.mult, [b_stat], [b_stat])
                ACT(outt[:], acc[:], AF.Identity, [b_acc, b_stat], [b_ot], bias=nmr[:], scale=rstd[:])
                TT("dve", outt[:], outt[:], ln2g[:], ALU.mult, [b_ot, b_c], [b_ot])
                TT("dve", outt[:], outt[:], ln2b[:], ALU.add, [b_ot, b_c], [b_ot])
                DMA("sp", out[rows, :], outt[:], [b_ot], [b_out])
            S.barrier()
    sp_.close()


def prep_shared(inp):
    f = lambda a: np.ascontiguousarray(a, dtype=np.float32)
    b_in = np.asarray(inp["b_in"][0], np.float32)
    pad = np.zeros(41 * 128, np.float32)
    pad[:DIN] = b_in
    col8 = lambda v: np.asarray(v, np.float32).reshape(8, 128).T

    def relayout(w):
        w = np.asarray(w, np.float32).reshape(NE, 16, 128, 4, 512)
        return np.ascontiguousarray(w.transpose(0, 3, 2, 1, 4)).reshape(NE * 4 * 128, 16 * 512)

    cfc, cbc = make_consts()
    sh = {
        "w_ada": f(inp["w_ada"][0]),
        "b_ada": f(inp["b_ada"][0][None, :]),
        "w_in": f(inp["w_in"][0]),
        "b_in_col": f(pad.reshape(41, 128).T),
        "b_kv_bc": f(np.tile(b_in[2560:4096][None, :], (128, 1))),
        "dw_col": f(np.asarray(inp["dw_w"][0], np.float32).reshape(31, 8, 128).transpose(2, 1, 0).reshape(128, 248)),
        "cv_col": f(np.concatenate([col8(inp["dw_b"][0]), col8(inp["conv_ln_g"][0]), col8(inp["conv_ln_b"][0]),
                                    col8(inp["mh_g"][0])], axis=1)),
        "w_out": f(inp["w_out"][0]),
        "ln_bc": f(np.tile(np.concatenate([inp["ln1_g"][0], inp["ln1_b"][0], inp["ln2_g"][0], inp["ln2_b"][0]])[None, :],
                           (128, 1))),
        "router_w": f(inp["router_w"][0]),
        "rb_bc": f(np.tile(np.asarray(inp["router_b"][0], np.float32)[None, :], (128, 1))),
        "wg_l": relayout(inp["w_gate"][0]),
        "wu_l": relayout(inp["w_up"][0]),
        "wd_l": relayout(inp["w_down"][0]),
        "bgu": f(np.concatenate([inp["b_gate"][0], inp["b_up"][0]], axis=1)),
        "b_down": f(inp["b_down"][0]),
        "cf": cfc,
        "cb": cbc,
    }
    return sh


def core_inputs(inp, sh, b):
    m = dict(sh)
    m["x"] = np.ascontiguousarray(inp["x"][b], dtype=np.float32)
    m["c_col"] = np.ascontiguousarray(np.asarray(inp["c"][b], np.float32).reshape(16, 128).T)
    return m


_NC_CACHE = {}


def kernel(**inputs):
    sh = prep_shared(inputs)
    if "nc" not in _NC_CACHE:
        _NC_CACHE["nc"] = build()
    nc = _NC_CACHE["nc"]
    in_maps = [core_inputs(inputs, sh, b) for b in range(8)]
    res = run_bass_kernel_spmd(nc, in_maps, core_ids=list(range(8)))
    return np.stack([np.asarray(r["out"], dtype=np.float32) for r in res.results], axis=0)
```
